# Optimizing a Trainium2 kernel written in Bass

```python
import jax, jax.numpy as jnp
from jax import lax
import numpy as np


D_MODEL = 2048
BATCH = 4
SEQ = 4096
DEPTH = 1

N_META = 16
BLOCK = 128
NORM_EPS = 1e-6

RWKV_HEADS = 16
RWKV_HEAD = 64
RWKV_DIM = RWKV_HEADS * RWKV_HEAD
DECAY_LORA = 64
ICLR_LORA = 64
GATE_LORA = 160
GN_EPS = 64e-5

MLA_HEADS = 16
Q_LORA = 512
KV_LORA = 512
QK_NOPE = 128
QK_ROPE = 64
V_HEAD = 128
MLA_DIM = MLA_HEADS * V_HEAD
ROPE_THETA = 10000.0

N_KEYS = 128
N_EXPERTS = N_KEYS * N_KEYS
PEER_HEADS = 8
PEER_QDIM = 256
PEER_HALF = PEER_QDIM // 2
PEER_TOPK = 16

RWKV_SPLIT = (RWKV_DIM, RWKV_DIM, RWKV_DIM, GATE_LORA, DECAY_LORA, DECAY_LORA, ICLR_LORA, ICLR_LORA)
RWKV_COLS = sum(RWKV_SPLIT)
IN_SPLIT = (RWKV_COLS, Q_LORA, KV_LORA, QK_ROPE, D_MODEL, D_MODEL)
IN_COLS = sum(IN_SPLIT)

kernel_name = 'hybrid_rwkv7_mla_peer_encoder_block'


def split_cols(t, sizes):
    return jnp.split(t, np.cumsum(sizes)[:-1].tolist(), axis=-1)


def rmsnorm(x, g):
    xf = x.astype(jnp.float32)
    y = xf * lax.rsqrt(jnp.mean(xf * xf, axis=-1, keepdims=True) + NORM_EPS)
    return (y * g.astype(jnp.float32)).astype(x.dtype)


def centred_shift(p, c):
    prev = jnp.pad(p, ((0, 0), (1, 0), (0, 0)))[:, :-1]
    nxt = jnp.pad(p, ((0, 0), (0, 1), (0, 0)))[:, 1:]
    return c[0] * prev + c[1] * p + c[2] * nxt


def wkv7_scan(r, w, k, v, a, b):
    Bsz, L, H, N = r.shape

    def step(S, inp):
        r_t, w_t, k_t, v_t, a_t, b_t = inp
        sa = jnp.einsum('bhij,bhj->bhi', S, a_t)
        S = S * w_t[:, :, None, :] + sa[..., None] * b_t[:, :, None, :] + v_t[..., None] * k_t[:, :, None, :]
        return S, jnp.einsum('bhij,bhj->bhi', S, r_t)

    xs = tuple(jnp.moveaxis(t, 1, 0) for t in (r, w, k, v, a, b))
    S0 = jnp.zeros((Bsz, H, N, N), jnp.float32)
    _, y = lax.scan(step, S0, xs)
    return jnp.moveaxis(y, 0, 1)


def rwkv7_branch(cols, shift_c, w_up, w0, a_up, a0, g_up, k_k, k_a, r_k, ln_g, ln_b):
    f32 = jnp.float32
    Bsz, L, _ = cols.shape
    xs = centred_shift(cols.astype(f32), shift_c.astype(f32))
    r, k, v, g_d, wd_f, wd_b, ad_f, ad_b = split_cols(xs, RWKV_SPLIT)
    heads = lambda t: t.reshape(t.shape[:-1] + (RWKV_HEADS, RWKV_HEAD))
    g = jax.nn.sigmoid(g_d) @ g_up.astype(f32)
    kk = heads(k * k_k.astype(f32))
    kk = kk / jnp.maximum(jnp.linalg.norm(kk, axis=-1, keepdims=True), 1e-12)
    rh, kh, vh = heads(r), heads(k), heads(v)

    def direction(wd, ad, d, reverse):
        w_log = -jax.nn.softplus(-(w0[d].astype(f32) + jnp.tanh(wd) @ w_up[d].astype(f32))) - 0.5
        decay = jnp.exp(-jnp.exp(heads(w_log)))
        iclr = jax.nn.sigmoid(heads(a0[d].astype(f32) + ad @ a_up[d].astype(f32)))
        kt = kh * (1.0 + (iclr - 1.0) * heads(k_a.astype(f32)))
        ins = (rh, decay, kt, vh, -kk, kk * iclr)
        if reverse:
            ins = tuple(jnp.flip(t, 1) for t in ins)
        y = wkv7_scan(*ins)
        if reverse:
            y = jnp.flip(y, 1)
        return y, kt

    y_f, kt_f = direction(wd_f, ad_f, 0, False)
    y_b, kt_b = direction(wd_b, ad_b, 1, True)
    y = y_f + y_b
    mu = jnp.mean(y, axis=-1, keepdims=True)
    var = jnp.mean(jnp.square(y - mu), axis=-1, keepdims=True)
    y = ((y - mu) * lax.rsqrt(var + GN_EPS)).reshape(Bsz, L, RWKV_DIM) * ln_g.astype(f32) + ln_b.astype(f32)
    bonus = jnp.sum(rh * (0.5 * (kt_f + kt_b)) * r_k.astype(f32), axis=-1, keepdims=True) * vh
    return ((y + bonus.reshape(Bsz, L, RWKV_DIM)) * g).astype(cols.dtype)


def apply_rope(x, cos, sin):
    x1, x2 = jnp.split(x, 2, axis=-1)
    return jnp.concatenate([x1 * cos - x2 * sin, x1 * sin + x2 * cos], axis=-1)


def mla_branch(q_d, kv_d, k_rope_raw, q_norm_g, w_uq, kv_norm_g, w_ukv, cos, sin):
    Bsz, L, _ = q_d.shape
    q = (rmsnorm(q_d, q_norm_g) @ w_uq).reshape(Bsz, L, MLA_HEADS, QK_NOPE + QK_ROPE)
    q_nope = q[..., :QK_NOPE]
    q_rope = apply_rope(q[..., QK_NOPE:], cos[:, None, :], sin[:, None, :])
    kv = (rmsnorm(kv_d, kv_norm_g) @ w_ukv).reshape(Bsz, L, MLA_HEADS, QK_NOPE + V_HEAD)
    k_nope, v = kv[..., :QK_NOPE], kv[..., QK_NOPE:]
    k_rope = apply_rope(k_rope_raw, cos, sin)
    pad = (-N_META) % BLOCK
    Lq = L + pad
    nblk = Lq // BLOCK
    to_blocks = lambda t: jnp.pad(t, ((0, 0), (pad, 0), (0, 0), (0, 0))).reshape(
        Bsz, nblk, BLOCK, MLA_HEADS, t.shape[-1]).transpose(1, 0, 2, 3, 4)
    qn_blk, qr_blk = to_blocks(q_nope), to_blocks(q_rope)
    scale = (QK_NOPE + QK_ROPE) ** -0.5

    def attend(blk):
        qn_b, qr_b = blk
        s = jnp.einsum('bqhd,bkhd->bhqk', qn_b, k_nope) + jnp.einsum('bqhd,bkd->bhqk', qr_b, k_rope)
        p = jax.nn.softmax(s.astype(jnp.float32) * scale, axis=-1).astype(v.dtype)
        return jnp.einsum('bhqk,bkhd->bqhd', p, v)

    o = lax.map(attend, (qn_blk, qr_blk))
    return o.transpose(1, 0, 2, 3, 4).reshape(Bsz, Lq, MLA_DIM)[:, pad:]


def peer_ffn(h, w_q, sub_keys, u_tab, v_tab):
    Bsz, L, D = h.shape
    q = (h @ w_q).reshape(Bsz, L, PEER_HEADS, 2, PEER_HALF)
    s = jnp.einsum('blhpd,hpnd->blhpn', q, sub_keys)
    top_s, top_i = lax.top_k(s, PEER_TOPK)
    cand = top_s[..., 0, :, None] + top_s[..., 1, None, :]
    best_s, best_c = lax.top_k(cand.reshape(Bsz, L, PEER_HEADS, PEER_TOPK * PEER_TOPK), PEER_TOPK)
    i1 = jnp.take_along_axis(top_i[..., 0, :], best_c // PEER_TOPK, axis=-1)
    i2 = jnp.take_along_axis(top_i[..., 1, :], best_c % PEER_TOPK, axis=-1)
    expert = i1 * N_KEYS + i2
    gate = jax.nn.softmax(best_s.astype(jnp.float32), axis=-1).astype(h.dtype)
    pad = (-N_META) % BLOCK
    Lp = L + pad
    nblk = Bsz * Lp // BLOCK
    padt = lambda t: jnp.pad(t, ((0, 0), (pad, 0)) + ((0, 0),) * (t.ndim - 2))
    h_blk = padt(h).reshape(nblk, BLOCK, D)
    e_blk = padt(expert).reshape(nblk, BLOCK, PEER_HEADS, PEER_TOPK)
    g_blk = padt(gate).reshape(nblk, BLOCK, PEER_HEADS, PEER_TOPK)

    def experts(blk):
        h_b, e_b, g_b = blk
        u = jnp.take(u_tab, e_b, axis=0)
        act = jax.nn.gelu(jnp.einsum('td,thkd->thk', h_b, u), approximate=False) * g_b
        return jnp.einsum('thk,thkd->td', act, jnp.take(v_tab, e_b, axis=0))

    out = lax.map(experts, (h_blk, e_blk, g_blk))
    return out.reshape(Bsz, Lp, D)[:, pad:]


def setup_inputs(seed: int = 0) -> dict:
    key = jax.random.key(seed)
    ks = jax.random.split(key, 32)
    nrm = lambda k, shape, scale: jax.random.normal(k, shape, jnp.float32) * scale
    n_idx = jnp.arange(RWKV_DIM, dtype=jnp.float32) / (RWKV_DIM - 1)
    decay_base = -6.5 + 5.0 * n_idx ** 0.85
    shift_base = jnp.array([0.25, 0.5, 0.25], jnp.float32)[:, None]
    return {
        'x': nrm(ks[0], (BATCH, SEQ, D_MODEL), 1.0),
        'meta_tokens': nrm(ks[1], (N_META, D_MODEL), 1.0),
        'norm_mix_g': 1.0 + nrm(ks[2], (DEPTH, D_MODEL), 0.02),
        'w_in': nrm(ks[3], (DEPTH, D_MODEL, IN_COLS), D_MODEL ** -0.5),
        'b_gate': nrm(ks[4], (DEPTH, 2 * D_MODEL), 0.02),
        'shift_c': shift_base + nrm(ks[5], (DEPTH, 3, RWKV_COLS), 0.05),
        'w_up': nrm(ks[6], (DEPTH, 2, DECAY_LORA, RWKV_DIM), 0.1 * DECAY_LORA ** -0.5),
        'w0': decay_base + nrm(ks[7], (DEPTH, 2, RWKV_DIM), 0.1),
        'a_up': nrm(ks[8], (DEPTH, 2, ICLR_LORA, RWKV_DIM), 0.5 * ICLR_LORA ** -0.5),
        'a0': nrm(ks[9], (DEPTH, 2, RWKV_DIM), 0.1),
        'g_up': nrm(ks[10], (DEPTH, GATE_LORA, RWKV_DIM), GATE_LORA ** -0.5),
        'k_k': 0.85 + nrm(ks[11], (DEPTH, RWKV_DIM), 0.02),
        'k_a': 1.0 + nrm(ks[12], (DEPTH, RWKV_DIM), 0.02),
        'r_k': nrm(ks[13], (DEPTH, RWKV_HEADS, RWKV_HEAD), 0.1),
        'ln_x_g': 1.0 + nrm(ks[14], (DEPTH, RWKV_DIM), 0.02),
        'ln_x_b': nrm(ks[15], (DEPTH, RWKV_DIM), 0.02),
        'q_norm_g': 1.0 + nrm(ks[16], (DEPTH, Q_LORA), 0.02),
        'w_uq': nrm(ks[17], (DEPTH, Q_LORA, MLA_HEADS * (QK_NOPE + QK_ROPE)), Q_LORA ** -0.5),
        'kv_norm_g': 1.0 + nrm(ks[18], (DEPTH, KV_LORA), 0.02),
        'w_ukv': nrm(ks[19], (DEPTH, KV_LORA, MLA_HEADS * (QK_NOPE + V_HEAD)), KV_LORA ** -0.5),
        'p_rwkv': nrm(ks[20], (DEPTH, RWKV_DIM, D_MODEL), RWKV_DIM ** -0.5),
        'p_mla': nrm(ks[21], (DEPTH, MLA_DIM, D_MODEL), MLA_DIM ** -0.5),
        'w_o': nrm(ks[22], (DEPTH, D_MODEL, D_MODEL), D_MODEL ** -0.5),
        'norm_ffn_g': 1.0 + nrm(ks[23], (DEPTH, D_MODEL), 0.02),
        'peer_wq': nrm(ks[24], (DEPTH, D_MODEL, PEER_HEADS * PEER_QDIM), D_MODEL ** -0.5),
        'peer_keys': nrm(ks[25], (DEPTH, PEER_HEADS, 2, N_KEYS, PEER_HALF), PEER_HALF ** -0.5),
        'peer_u': nrm(ks[26], (DEPTH, N_EXPERTS, D_MODEL), D_MODEL ** -0.5),
        'peer_v': nrm(ks[27], (DEPTH, N_EXPERTS, D_MODEL), PEER_HEADS ** -0.5),
        'final_norm_g': 1.0 + nrm(ks[28], (D_MODEL,), 0.02),
    }


def reference(x, meta_tokens, norm_mix_g, w_in, b_gate, shift_c, w_up, w0, a_up, a0, g_up, k_k, k_a, r_k,
              ln_x_g, ln_x_b, q_norm_g, w_uq, kv_norm_g, w_ukv, p_rwkv, p_mla, w_o, norm_ffn_g,
              peer_wq, peer_keys, peer_u, peer_v, final_norm_g):
    Bsz = x.shape[0]
    meta = jnp.broadcast_to(meta_tokens.astype(x.dtype)[None], (Bsz, N_META, D_MODEL))
    h = jnp.concatenate([meta, x], axis=1)
    L = h.shape[1]
    pos = jnp.arange(L, dtype=jnp.float32)
    inv_freq = ROPE_THETA ** (-jnp.arange(0, QK_ROPE, 2, dtype=jnp.float32) / QK_ROPE)
    ang = pos[:, None] * inv_freq[None, :]
    cos, sin = jnp.cos(ang).astype(x.dtype), jnp.sin(ang).astype(x.dtype)
    for l in range(DEPTH):
        n = rmsnorm(h, norm_mix_g[l])
        proj = n @ w_in[l]
        rw_cols, q_d, kv_d, k_rope_raw, gr_pre, gm_pre = split_cols(proj, IN_SPLIT)
        y_r = rwkv7_branch(rw_cols, shift_c[l], w_up[l], w0[l], a_up[l], a0[l], g_up[l],
                           k_k[l], k_a[l], r_k[l], ln_x_g[l], ln_x_b[l])
        y_m = mla_branch(q_d, kv_d, k_rope_raw, q_norm_g[l], w_uq[l], kv_norm_g[l], w_ukv[l], cos, sin)
        gates = jax.nn.sigmoid(jnp.concatenate([gr_pre, gm_pre], axis=-1) + b_gate[l])
        g_r, g_m = jnp.split(gates, 2, axis=-1)
        merged = g_r * (y_r @ p_rwkv[l]) + g_m * (y_m @ p_mla[l])
        h = h + merged @ w_o[l]
        h = h + peer_ffn(rmsnorm(h, norm_ffn_g[l]), peer_wq[l], peer_keys[l], peer_u[l], peer_v[l])
    return rmsnorm(h, final_norm_g)[:, N_META:]
```

```python
import numpy as np
from contextlib import ExitStack
import concourse.bass as bass
import concourse.mybir as mybir
from concourse.bass_utils import run_bass_kernel_spmd

F32 = mybir.dt.float32
BF16 = mybir.dt.bfloat16
I32 = mybir.dt.int32
U32 = mybir.dt.uint32
AF = mybir.ActivationFunctionType
ALU = mybir.AluOpType
AX = mybir.AxisListType

D = 2048
NTOK = 4224
OWN0, OWN1 = 64, 2112
NOWN = 2048
HALF = 2112
NCH = 66
C = 64
RW = 3488
EPS = 1e-6
C0 = float(np.exp(-0.5))

R_R, R_K, R_V, R_GD, R_WDF, R_WDB, R_ADF, R_ADB = 0, 1024, 2048, 3072, 3232, 3296, 3360, 3424
R_QD, R_KVD, R_KR, R_KRR, R_GR, R_GM = 3488, 4000, 4512, 4576, 4640, 6688
NROWS = 8736


class Ctx:
    def __init__(self, nc, es):
        self.nc = nc
        self.E = {"pe": nc.tensor, "dve": nc.vector, "act": nc.scalar, "pool": nc.gpsimd, "sp": nc.sync}
        self.psem = {k: es.enter_context(nc.semaphore("prog_" + k)) for k in ["pe", "dve", "act", "pool"]}
        self.pcnt = {k: 0 for k in self.psem}
        self.seen = {}
        self.buf = {}
        self.dq = {}
        for q in ["sp", "pool", "act"]:
            sems = [es.enter_context(nc.semaphore("dq_%s_%d" % (q, i))) for i in range(20)]
            self.dq[q] = {"sems": sems, "cnt": [0] * len(sems), "i": 0}
        self.ninst = 0
        self.pend = {}

    def wait(self, eng, tok):
        key, sem, val = tok
        k = (eng, key)
        if self.seen.get(k, 0) >= val:
            return
        self.E[eng].wait_ge(sem, val)
        self.seen[k] = val

    def _deps(self, reads, writes):
        deps = []
        for k in reads:
            st = self.buf.get(k)
            if st and st["w"]:
                deps.append(st["w"])
        for k in writes:
            st = self.buf.get(k)
            if st:
                if st["w"]:
                    deps.append(st["w"])
                deps.extend(st["r"].values())
        return deps

    def _commit(self, tok, reads, writes):
        for k in reads:
            st = self.buf.setdefault(k, {"w": None, "r": {}})
            old = st["r"].get(tok[0])
            if old is None or old[2] < tok[2]:
                st["r"][tok[0]] = tok
        for k in writes:
            self.buf[k] = {"w": tok, "r": {}}

    def op(self, eng, fn, reads=(), writes=(), last=True):
        for d in self._deps(reads, writes):
            self.wait(eng, d)
        ins = fn(self.E[eng])
        self.ninst += 1
        pend = self.pend.setdefault(eng, [])
        pend.append((list(reads), list(writes)))
        if not last:
            return None
        self.pcnt[eng] += 1
        ins.then_inc(self.psem[eng], 1)
        tok = (eng, self.psem[eng], self.pcnt[eng])
        for r, w in pend:
            self._commit(tok, r, w)
        del pend[:]
        return tok

    def dma(self, q, out, in_, reads=(), writes=(), indirect=None, slow=False):
        dq = self.dq[q]
        i = dq["i"]
        dq["i"] = (i + 1) % len(dq["sems"])
        sem = dq["sems"][i]
        key = "dq_%s_%d" % (q, i)
        if dq["cnt"][i] > 0:
            self.wait(q, (key, sem, dq["cnt"][i]))
        for d in self._deps(reads, writes):
            self.wait(q, d)
        if indirect is None:
            ins = self.E[q].dma_start(out=out, in_=in_, allow_slow_non_contiguous=slow)
        else:
            ins = self.E[q].indirect_dma_start(out=out, out_offset=None, in_=in_, in_offset=indirect)
        dq["cnt"][i] += 16
        ins.then_inc(sem, 16)
        tok = (key, sem, dq["cnt"][i])
        self._commit(tok, reads, writes)
        self.ninst += 1
        return tok

    def barrier(self):
        toks = [(k, self.psem[k], self.pcnt[k]) for k in self.psem if self.pcnt[k] > 0]
        for q, dq in self.dq.items():
            for i, sem in enumerate(dq["sems"]):
                if dq["cnt"][i] > 0:
                    toks.append(("dq_%s_%d" % (q, i), sem, dq["cnt"][i]))
        for eng in ["pe", "dve", "act", "pool", "sp"]:
            for t in toks:
                self.wait(eng, t)
        self.buf = {}


def stage_inproj(cx, nc, T, PS):
    with ExitStack() as es:
        def sb(name, shape, dt):
            return es.enter_context(nc.sbuf_tensor(name, shape, dt))
        nT = sb("nT", [128, 16, HALF], BF16)
        xt = [sb("xt%d" % i, [128, D], F32) for i in range(2)]
        xs = [sb("xs%d" % i, [128, D], F32) for i in range(2)]
        ssq = sb("ssq", [128, 40], F32)
        rstd = sb("rstd", [128, 40], F32)
        gS = sb("gS", [128, 16], F32)
        ident = sb("identA", [128, 128], F32)
        wb = [sb("wb%d" % i, [128, 16, 256], BF16) for i in range(2)]
        wrot = sb("wrot", [128, 16, 64], BF16)
        stg = [sb("stg%d" % i, [128, 512], F32) for i in range(4)]
        zero = sb("zeroA", [128, 2], F32)

        cx.dma("sp", ident[:], T["ident"], writes=["ident"])
        cx.dma("sp", gS[:], T["norm_mix_g"], writes=["gS"])
        epsc = sb("epsc", [128, 1], F32)
        cx.op("dve", lambda e: e.memset(epsc[:], EPS), writes=["epsc"])
        cx.op("dve", lambda e: e.memset(zero[:], 0.0), writes=["zero"])
        for r0 in range(0, NROWS, 128):
            m = min(128, NROWS - r0)
            cx.dma("sp", T["PRT"][r0:r0 + m, 0:1], zero[0:m, 0:1], reads=["zero"], slow=True)
            cx.dma("sp", T["PRT"][r0:r0 + m, NTOK + 1:NTOK + 2], zero[0:m, 1:2], reads=["zero"], slow=True)

        units = []
        for u0 in range(0, 3072, 256):
            units.append((T["w_in"][:, u0:u0 + 256], 256, [(0, 128, u0, False), (128, 128, u0 + 128, False)], False))
        units.append((T["w_lora"][:, 0:160], 160, [(0, 128, R_GD, False), (128, 32, R_GD + 128, False)], False))
        units.append((T["w_lora"][:, 160:416], 256, [(0, 64, R_WDF, False), (64, 64, R_WDB, False),
                                                      (128, 64, R_ADF, False), (192, 64, R_ADB, False)], False))
        for u0 in range(0, 512, 256):
            units.append((T["w_in"][:, 4000 + u0:4000 + u0 + 256], 256,
                          [(0, 128, R_KVD + u0, False), (128, 128, R_KVD + u0 + 128, False)], False))
        units.append((T["w_in"][:, 4512:4576], 64, [(0, 64, R_KR, False), (0, 64, R_KRR, True)], False))
        for u0 in range(0, 512, 256):
            units.append((T["w_in"][:, 3488 + u0:3488 + u0 + 256], 256,
                          [(0, 128, R_QD + u0, False), (128, 128, R_QD + u0 + 128, False)], True))
        for u0 in range(0, 4096, 256):
            units.append((T["w_in"][:, 4576 + u0:4576 + u0 + 256], 256,
                          [(0, 128, R_GR + u0, False), (128, 128, R_GR + u0 + 128, False)], True))

        tcb = [sb("tcb%d" % i, [128, 2048], BF16) for i in range(4)]

        def tabconv_gen():
            k = 0
            for ti in range(128):
                for tab, src in enumerate(["peer_u", "peer_v"]):
                    b_ = k % 4
                    cx.dma("pool", tcb[b_][:], T[src][ti * 128:(ti + 1) * 128, :], writes=[("tcb", b_)])
                    cx.dma("act", T["UVB"][ti * 128:(ti + 1) * 128, tab * 2048:(tab + 1) * 2048], tcb[b_][:],
                           reads=[("tcb", b_)])
                    k += 1
                    yield

        tcg = tabconv_gen()
        ev = [0]
        for half in range(2):
            tbase = half * HALF
            for ti in range(HALF // 128 + 1):
                t0 = tbase + ti * 128
                n = min(128, tbase + HALF - t0)
                if n <= 0:
                    continue
                b = ti % 2
                tt = half * 17 + ti
                cx.dma("sp", xt[b][0:n, :], T["xp"][t0:t0 + n, :], writes=[("xt", b)])
                cx.op("act", lambda e: e.activation(out=xs[b][0:n, :], in_=xt[b][0:n, :], func=AF.Square,
                                                    accum_out=ssq[0:n, tt:tt + 1]),
                      reads=[("xt", b)], writes=[("xs", b), ("ssq", tt)])
                cx.op("act", lambda e: e.activation(out=ssq[0:n, tt:tt + 1], in_=ssq[0:n, tt:tt + 1], func=AF.Sqrt,
                                                    scale=1.0 / D, bias=epsc[0:n, 0:1]),
                      reads=[("ssq", tt), "epsc"], writes=[("ssq", tt)])
                cx.op("dve", lambda e: e.reciprocal(out=rstd[0:n, tt:tt + 1], in_=ssq[0:n, tt:tt + 1]),
                      reads=[("ssq", tt)], writes=[("rstd", tt)])
                cx.op("act", lambda e: e.activation(out=xs[b][0:n, :], in_=xt[b][0:n, :], func=AF.Copy,
                                                    scale=rstd[0:n, tt:tt + 1]),
                      reads=[("xt", b), ("rstd", tt)], writes=[("xs", b)])
                for cg in range(4):
                    pb = PS[(tt * 4 + cg) % 2]
                    for ci in range(4):
                        c = cg * 4 + ci
                        cx.op("pe", lambda e: e.transpose(out=pb[:, ci * 128:ci * 128 + n],
                                                          in_=xs[b][0:n, c * 128:(c + 1) * 128], identity=ident[0:n, 0:n]),
                              reads=[("xs", b), "ident"], writes=[("ps", (tt * 4 + cg) % 2, ci)], last=(ci == 3))
                    tl = t0 - tbase
                    cx.op("dve", lambda e: e.tensor_tensor(
                        out=nT[:, cg * 4:cg * 4 + 4, tl:tl + n],
                        in0=pb[:, 0:512].rearrange("p (c t) -> p c t", c=4)[:, :, 0:n],
                        in1=gS[:, cg * 4:cg * 4 + 4].unsqueeze(2).to_broadcast([128, 4, n]), op=ALU.mult),
                        reads=[("ps", (tt * 4 + cg) % 2, i) for i in range(4)] + ["gS"],
                        writes=[("nT", ti)])
            if half == 0:
                blocks = [(0, 64, False)] + [(64 + 512 * i, 512, True) for i in range(4)]
            else:
                blocks = [(HALF + 512 * i, 512, False) for i in range(4)] + [(4160, 64, False)]
            nTreads = [("nT", i) for i in range(17)]
            active = [u for u in units if not (u[3] and half == 1)]

            def issue_load(idx):
                src_, U_, segs_, _ = active[idx]
                wb_ = wb[idx % 2]
                cx.dma("pool", wb_[:, :, 0:U_], src_.rearrange("(c p) u -> p c u", p=128), writes=[("wb", idx % 2)])
                if any(s_[3] for s_ in segs_):
                    cx.op("pool", lambda e: e.tensor_scalar(out=wrot[:, :, 0:32], in0=wb_[:, :, 32:64], scalar1=-1.0,
                                                            scalar2=None, op0=ALU.mult),
                          reads=[("wb", idx % 2)], writes=["wrot"])
                    cx.op("pool", lambda e: e.tensor_copy(out=wrot[:, :, 32:64], in_=wb_[:, :, 0:32]),
                          reads=[("wb", idx % 2)], writes=["wrot"])

            issue_load(0)
            for ui, (src, U, segs, own_only) in enumerate(active):
                if ui + 1 < len(active):
                    issue_load(ui + 1)
                wbuf = ui % 2
                for _ in range(5):
                    next(tcg, None)
                for (off, M, drow, rot) in segs:
                    for (t0, n, isown) in blocks:
                        if own_only and not isown:
                            continue
                        k = ev[0]
                        ev[0] += 1
                        pbk = 2 + (k % 4)
                        pb = PS[pbk]
                        tl = t0 - tbase
                        for c in range(16):
                            lhsT = wrot[:, c, 0:64] if rot else wb[wbuf][:, c, off:off + M]
                            cx.op("pe", lambda e: e.matmul(out=pb[0:M, 0:n], lhsT=lhsT, rhs=nT[:, c, tl:tl + n],
                                                           start=(c == 0), stop=(c == 15)),
                                  reads=[("wb", wbuf), "wrot"] + (nTreads if c == 0 else []),
                                  writes=[("psb", pbk)], last=(c == 15))
                        sg = stg[k % 4]
                        if k % 2 == 0:
                            cx.op("act", lambda e: e.copy(out=sg[0:M, 0:n], in_=pb[0:M, 0:n]),
                                  reads=[("psb", pbk)], writes=[("stg", k % 4)])
                        else:
                            cx.op("dve", lambda e: e.tensor_copy(out=sg[0:M, 0:n], in_=pb[0:M, 0:n]),
                                  reads=[("psb", pbk)], writes=[("stg", k % 4)])
                        cx.dma("sp", T["PRT"][drow:drow + M, 1 + t0:1 + t0 + n], sg[0:M, 0:n],
                               reads=[("stg", k % 4)])
        for _ in tcg:
            pass
    cx.barrier()


IN_SPECS = [
    ("xp", [NTOK, D]), ("ident", [128, 128]),
    ("norm_mix_g", [128, 16]), ("w_in", [D, 8672]), ("w_lora", [D, 416]), ("shiftc_fm", [128, 28, 3]), ("ones64", [64, 64]), ("rmask", [64, 16, 64]),
    ("maskG0", [128, 128]), ("maskG1", [128, 128]), ("maskN0", [64, 64]), ("maskN1", [64, 64]),
    ("wup0", [65, 1024]), ("wup1", [65, 1024]), ("aup0", [65, 1024]), ("aup1", [65, 1024]),
    ("kk_fm", [64, 16]), ("ka_fm", [64, 16]), ("rk_fm", [64, 16]),
    ("lng_b", [128, 1024]), ("lnb_b", [128, 1024]), ("g_up", [160, 1024]), ("invf", [64, 1]),
    ("gq_fm", [128, 4]), ("gkv_fm", [128, 4]), ("valid_tm", [128, 33]), ("pos64", [64, NTOK]),
    ("w_uq", [512, 3072]), ("w_ukv", [512, 4096]), ("p_rwkv", [1024, 2048]), ("p_mla", [2048, 2048]),
    ("w_o", [2048, 2048]), ("bgate_fm", [128, 32]), ("peer_wq", [2048, 2048]), ("peer_keys", [16, 128, 128]),
    ("peer_u", [16384, 2048]), ("peer_v", [16384, 2048]), ("gffn_b", [128, 2048]), ("gfin_b", [128, 2048]), ("iota256", [128, 256]),
]


def build(upto="all", debug_out=()):
    nc = bass.Bass("TRN2", target_bir_lowering=False)
    T = {}
    for name, shape in IN_SPECS:
        T[name] = nc.dram_tensor(name, shape, F32, kind="ExternalInput").ap()
    def scratch(name, shape, dt=F32):
        kind = "ExternalOutput" if name in debug_out else "Internal"
        T[name] = nc.dram_tensor(name, shape, dt, kind=kind).ap()
    scratch("PRT", [NROWS, NTOK + 2])
    scratch("XSQ", [3, NCH, 64, 16, 64])
    scratch("XSL", [416, NTOK])
    scratch("YF", [NOWN, 1040])
    scratch("YB", [NOWN, 1040])
    scratch("VTM", [NOWN, 1024])
    scratch("YRT", [1024, NOWN], BF16)
    scratch("YMT", [2048, NOWN], BF16)
    scratch("MGT", [2048, NOWN], BF16)
    scratch("H2", [NOWN, D])
    scratch("UVB", [16384, 4096], BF16)
    T["out"] = nc.dram_tensor("out", [NOWN, D], F32, kind="ExternalOutput").ap()
    with ExitStack() as es:
        PQ = [es.enter_context(nc.psum_tensor("pq%d" % i, [128, 1024], F32)) for i in range(4)]
        PS = [PQ[i // 2][:, (i % 2) * 512:(i % 2) * 512 + 512] for i in range(8)]
        cx = Ctx(nc, es)
        stage_inproj(cx, nc, T, PS)
        if upto == "inproj":
            print("instructions:", cx.ninst)
            return nc
        stage_shift(cx, nc, T)
        stage_rwkv(cx, nc, T, PQ, PS)
        if upto == "rwkv":
            print("instructions:", cx.ninst)
            return nc
        stage_post(cx, nc, T, PQ)
        stage_mla(cx, nc, T, PQ, PS)
        if upto == "mla":
            print("instructions:", cx.ninst)
            return nc
        stage_merge(cx, nc, T, PQ, PS)
        stage_peer(cx, nc, T, PQ, PS)
        print("instructions:", cx.ninst)
    return nc


def prep_core(inputs, b, s):
    x = inputs["x"]
    meta = inputs["meta_tokens"]
    xp = np.zeros((NTOK, D), np.float32)
    posv = np.zeros((1, NTOK), np.float32)
    valid = np.zeros((1, NTOK), np.float32)
    if s == 0:
        xp[48:64] = meta
        xp[64:64 + 4096] = x[b]
        posv[0, 48:64] = np.arange(16)
        posv[0, 64:64 + 4096] = 16 + np.arange(4096)
        valid[0, 48:64 + 4096] = 1
    else:
        xp[64:64 + 4096] = x[b, ::-1]
        xp[4160:4176] = meta[::-1]
        posv[0, 64:64 + 4096] = 16 + np.arange(4095, -1, -1)
        posv[0, 4160:4176] = np.arange(15, -1, -1)
        valid[0, 64:4176] = 1
    w_in = inputs["w_in"][0]
    sc = inputs["shift_c"][0]
    lo = R_GD
    if s == 0:
        order = [(R_GD, 160), (R_WDF, 64), (R_WDB, 64), (R_ADF, 64), (R_ADB, 64)]
    else:
        order = [(R_GD, 160), (R_WDB, 64), (R_WDF, 64), (R_ADB, 64), (R_ADF, 64)]
    cols = np.concatenate([np.arange(a, a + n) for a, n in order])
    w_lora = np.ascontiguousarray(w_in[:, cols])
    shiftc = sc.copy()
    shiftc[:, lo:RW] = sc[:, cols]
    if s == 1:
        shiftc = shiftc[::-1]
    m = {
        "xp": xp, "posv": posv, "valid": valid, "ident": np.eye(128, dtype=np.float32),
        "norm_mix_g": np.ascontiguousarray(inputs["norm_mix_g"][0].reshape(16, 128).T), "w_in": w_in, "w_lora": w_lora,
    }
    scp = np.zeros((3, 28 * 128), np.float32)
    scp[:, :RW] = shiftc
    m["shiftc_fm"] = np.ascontiguousarray(scp.reshape(3, 28, 128).transpose(2, 1, 0))
    m["ones64"] = np.ones((64, 64), np.float32)
    rm = np.ones((64, 16, 64), np.float32)
    rm[:, :, 0] = 0
    m["rmask"] = rm
    tt = np.arange(64)
    for di in range(2):
        if di == 0:
            strict = (tt[:, None] < tt[None, :]); incl = (tt[:, None] <= tt[None, :])
        else:
            strict = (tt[:, None] > tt[None, :]); incl = (tt[:, None] >= tt[None, :])
        m["maskG%d" % di] = np.block([[strict, incl], [strict, incl]]).astype(np.float32)
        m["maskN%d" % di] = np.ascontiguousarray(strict.T).astype(np.float32)
        d = di if s == 0 else 1 - di
        m["wup%d" % di] = np.concatenate([inputs["w_up"][0, d], inputs["w0"][0, d][None]], 0)
        m["aup%d" % di] = np.concatenate([inputs["a_up"][0, d], inputs["a0"][0, d][None]], 0)
    m["kk_fm"] = np.ascontiguousarray(inputs["k_k"][0].reshape(16, 64).T)
    m["ka_fm"] = np.ascontiguousarray(inputs["k_a"][0].reshape(16, 64).T)
    m["rk_fm"] = np.ascontiguousarray(inputs["r_k"][0].T)
    rep = lambda v, n: np.ascontiguousarray(np.broadcast_to(v[None, :], (n, v.shape[0])))
    m["lng_b"] = rep(inputs["ln_x_g"][0], 128)
    m["lnb_b"] = rep(inputs["ln_x_b"][0], 128)
    m["g_up"] = inputs["g_up"][0]
    invf = (10000.0 ** (-np.arange(0, 64, 2, dtype=np.float32) / 64)).astype(np.float32)
    m["invf"] = np.concatenate([invf, invf])[:, None]
    m["gq_fm"] = np.ascontiguousarray(inputs["q_norm_g"][0].reshape(4, 128).T)
    m["gkv_fm"] = np.ascontiguousarray(inputs["kv_norm_g"][0].reshape(4, 128).T)
    m["valid_tm"] = np.ascontiguousarray(valid[0].reshape(33, 128).T)
    m["pos64"] = rep(posv[0], 64)
    for k in ["w_uq", "w_ukv", "p_rwkv", "p_mla", "w_o", "peer_wq", "peer_u", "peer_v"]:
        m[k] = inputs[k][0]
    m["bgate_fm"] = np.ascontiguousarray(inputs["b_gate"][0].reshape(32, 128).T)
    m["peer_keys"] = inputs["peer_keys"][0].reshape(16, 128, 128)
    m["gffn_b"] = rep(inputs["norm_ffn_g"][0], 128)
    m["gfin_b"] = rep(inputs["final_norm_g"], 128)
    m["iota256"] = np.ascontiguousarray(np.broadcast_to(np.arange(256, dtype=np.float32)[None, :], (128, 256)))
    del m["posv"], m["valid"]
    return m


def stage_shift(cx, nc, T):
    with ExitStack() as es:
        def sb(name, shape, dt):
            return es.enter_context(nc.sbuf_tensor(name, shape, dt))
        sc = sb("shc", [128, 28, 3], F32)
        raw = [sb("shraw%d" % i, [128, 514], F32) for i in range(4)]
        xo = [sb("shxo%d" % i, [128, 512], F32) for i in range(4)]
        cx.dma("sp", sc[:], T["shiftc_fm"], writes=["sc"])
        k = 0
        for rt in range(28):
            r0 = rt * 128
            m = min(128, RW - r0)
            for t0 in range(0, NTOK, 512):
                n = min(512, NTOK - t0)
                b = k % 4
                k += 1
                cx.dma("sp", raw[b][0:m, 0:n + 2], T["PRT"][r0:r0 + m, t0:t0 + n + 2], writes=[("raw", b)])
                cx.op("act", lambda e: e.activation(out=xo[b][0:m, 0:n], in_=raw[b][0:m, 1:n + 1], func=AF.Copy,
                                                    scale=sc[0:m, rt, 1:2]),
                      reads=[("raw", b), "sc"], writes=[("xo", b)])
                cx.op("dve", lambda e: e.scalar_tensor_tensor(out=xo[b][0:m, 0:n], in0=raw[b][0:m, 0:n],
                                                              scalar=sc[0:m, rt, 0:1], in1=xo[b][0:m, 0:n],
                                                              op0=ALU.mult, op1=ALU.add),
                      reads=[("raw", b), ("xo", b), "sc"], writes=[("xo", b)])
                cx.op("dve", lambda e: e.scalar_tensor_tensor(out=xo[b][0:m, 0:n], in0=raw[b][0:m, 2:n + 2],
                                                              scalar=sc[0:m, rt, 2:3], in1=xo[b][0:m, 0:n],
                                                              op0=ALU.mult, op1=ALU.add),
                      reads=[("raw", b), ("xo", b), "sc"], writes=[("xo", b)])
                if rt < 24:
                    q, hp = rt // 8, rt % 8
                    c0 = t0 // 64
                    ncs = n // 64
                    for hh in range(2):
                        h = 2 * hp + hh
                        cx.dma("pool", T["XSQ"][q, c0:c0 + ncs, :, h, :].rearrange("c j t -> j c t"),
                               xo[b][hh * 64:hh * 64 + 64, 0:n].rearrange("j (c t) -> j c t", t=64),
                               reads=[("xo", b)])
                else:
                    cx.dma("pool", T["XSL"][r0 - 3072:r0 - 3072 + m, t0:t0 + n], xo[b][0:m, 0:n], reads=[("xo", b)])
    cx.barrier()


def stage_rwkv(cx, nc, T, PQ, PS):
    F32R = mybir.dt.float32r
    HG = 4
    with ExitStack() as es:
        def sb(name, shape, dt=F32):
            return es.enter_context(nc.sbuf_tensor(name, shape, dt))
        identf = sb("identRf", [128, 128])
        ident = sb("identR", [128, 128], F32R)
        ones64 = sb("ones64s", [64, 64], F32R)
        rmask = sb("rmasks", [64, HG, 64])
        maskG = [sb("maskGs%d" % i, [128, 128]) for i in range(2)]
        maskN = [sb("maskNs%d" % i, [64, 64]) for i in range(2)]
        wup = [sb("wups%d" % i, [65, 1024], F32R) for i in range(2)]
        aup = [sb("aups%d" % i, [65, 1024], F32R) for i in range(2)]
        kkp = sb("kkp", [64, 16])
        kap = sb("kap", [64, 16])
        omka = sb("omka", [64, 16])
        rkp = sb("rkp", [64, 16])
        wstg = sb("wstg", [65, 1024])
        cx.dma("sp", identf[:], T["ident"], writes=["identf"])
        cx.op("dve", lambda e: e.tensor_copy(out=ident[:], in_=identf[:]), reads=["identf"], writes=["ident"])
        onesf = sb("onesf", [65, 64])
        cx.op("dve", lambda e: e.memset(onesf[:], 1.0), writes=["onesf"])
        cx.op("dve", lambda e: e.tensor_copy(out=ones64[:], in_=onesf[0:64, :]), reads=["onesf"], writes=["ones64"])
        cx.dma("sp", rmask[:], T["rmask"][:, 0:HG, :], writes=["rmask"])
        for i in range(2):
            cx.dma("sp", maskG[i][:], T["maskG%d" % i], writes=["maskG%d" % i])
            cx.dma("sp", maskN[i][:], T["maskN%d" % i], writes=["maskN%d" % i])
            for (dst, src, nm) in [(wup[i], "wup%d" % i, "wup%d" % i), (aup[i], "aup%d" % i, "aup%d" % i)]:
                cx.dma("sp", wstg[:], T[src], writes=["wstg"])
                cx.op("dve", lambda e: e.tensor_copy(out=dst[:], in_=wstg[:]), reads=["wstg"], writes=[nm])
        cx.dma("sp", kkp[:], T["kk_fm"], writes=["kkp"])
        cx.dma("sp", kap[:], T["ka_fm"], writes=["kap"])
        cx.dma("sp", rkp[:], T["rk_fm"], writes=["rkp"])
        cx.op("dve", lambda e: e.tensor_scalar(out=omka[:], in0=kap[:], scalar1=-1.0, scalar2=1.0, op0=ALU.mult,
                                               op1=ALU.add), reads=["kap"], writes=["omka"])

        bank = [0]

        def newbank():
            i = bank[0] % 8
            bank[0] += 1
            return PS[i], ("ps", i)

        def make_chain(cid):
            h0 = cid * HG
            A = {}
            for n in ["rT", "kT", "sg", "ic", "PF", "Pex", "Pin", "ea", "er", "ei", "em", "kx", "t1", "kk", "kt", "kki",
                      "Q0", "YO", "ST"]:
                A[n] = sb("c%d_%s" % (cid, n), [64, HG, 64])
            for n in ["t2", "Nk0", "Nk1", "Mk0", "Mk1", "TT0", "TT1", "X", "STr"]:
                A[n] = sb("c%d_%s" % (cid, n), [64, HG, 64], F32R)
            A["vpad"] = sb("c%d_vpad" % cid, [64, HG, 128])
            A["LBp"] = sb("c%d_LBp" % cid, [64, HG, 128])
            A["LB"] = sb("c%d_LB" % cid, [64, HG, 128], F32R)
            A["RA"] = sb("c%d_RA" % cid, [64, HG, 128], F32R)
            A["GM"] = sb("c%d_GM" % cid, [128, HG, 128], F32R)
            A["UV"] = sb("c%d_UV" % cid, [128, HG, 64], F32R)
            A["BK"] = sb("c%d_BK" % cid, [128, HG, 64], F32R)
            A["wdraw"] = sb("c%d_wdraw" % cid, [64, 64])
            A["adraw"] = sb("c%d_adraw" % cid, [64, 64])
            A["wd"] = sb("c%d_wd" % cid, [65, 64], F32R)
            A["ad"] = sb("c%d_ad" % cid, [65, 64], F32R)
            A["tot"] = sb("c%d_tot" % cid, [64, HG])
            A["etot"] = sb("c%d_etot" % cid, [64, HG])
            A["rkr"] = sb("c%d_rkr" % cid, [64, HG])
            return A

        chains = [make_chain(cid) for cid in range(16 // HG)]

        def run_chain(cid, di, chunks, ychunks, ydst):
            A = chains[cid]
            h0 = cid * HG

            def K(*names):
                return [(cid, n) for n in names]

            def bc(p):
                return p[:, h0:h0 + HG].unsqueeze(2).to_broadcast([64, HG, 64])

            def bcl(p):
                return p[:].unsqueeze(2).to_broadcast([64, HG, 64])

            def tt(eng, out, a, b, op, reads, writes):
                return cx.op(eng, lambda e: e.tensor_tensor(out=out, in0=a, in1=b, op=op), reads=reads, writes=writes)

            def act(out, in_, func, reads, writes, **kw):
                return cx.op("act", lambda e: e.activation(out=out, in_=in_, func=func, **kw), reads=reads, writes=writes)

            def hmm(pb, pk, lhsf, rhsf, reads, w=64):
                for h in range(HG):
                    cx.op("pe", lambda e: e.matmul(out=pb[0:64, h * w:h * w + w], lhsT=lhsf(h), rhs=rhsf(h), start=True,
                                                   stop=True), reads=reads, writes=[pk], last=(h == HG - 1))

            def p3(pb, rows=64, w=64):
                return pb[0:rows, 0:HG * w].rearrange("p (h t) -> p h t", h=HG)

            def loads(c):
                t0 = c * 64
                cx.dma("sp", A["rT"][:], T["XSQ"][0, c, :, h0:h0 + HG, :], writes=K("rT"))
                cx.dma("sp", A["kT"][:], T["XSQ"][1, c, :, h0:h0 + HG, :], writes=K("kT"))
                cx.dma("sp", A["vpad"][:, :, 64:128], T["XSQ"][2, c, :, h0:h0 + HG, :], writes=K("vpad"))
                lw0 = (R_WDF if di == 0 else R_WDB) - 3072
                la0 = (R_ADF if di == 0 else R_ADB) - 3072
                cx.dma("sp", A["wdraw"][:], T["XSL"][lw0:lw0 + 64, t0:t0 + 64], writes=K("wdraw"))
                cx.dma("sp", A["adraw"][:], T["XSL"][la0:la0 + 64, t0:t0 + 64], writes=K("adraw"))

            cx.op("dve", lambda e: e.memset(A["ST"][:], 0.0), writes=K("ST"))
            cx.op("dve", lambda e: e.tensor_copy(out=A["STr"][:], in_=A["ST"][:]), reads=K("ST"), writes=K("STr"))
            cx.op("dve", lambda e: e.tensor_copy(out=A["wd"][64:65, :], in_=onesf[64:65, :]), reads=["onesf"], writes=K("wd1"))
            cx.op("dve", lambda e: e.tensor_copy(out=A["ad"][64:65, :], in_=onesf[64:65, :]), reads=["onesf"], writes=K("ad1"))
            cx.op("dve", lambda e: e.memset(A["vpad"][:], 0.0), writes=K("vpad"))
            loads(chunks[0])
            yield
            for ci, c in enumerate(chunks):
                t0 = c * 64
                want_y = c in ychunks
                act(A["wd"][0:64, :], A["wdraw"][:], AF.Tanh, K("wdraw"), K("wd"))
                act(A["ad"][0:64, :], A["adraw"][:], AF.Copy, K("adraw"), K("ad"))
                pb, pk = newbank()
                hmm(pb, pk, lambda h: wup[di][0:65, (h0 + h) * 64:(h0 + h) * 64 + 64], lambda h: A["wd"][0:65, :],
                    K("wd", "wd1") + ["wup%d" % di])
                act(A["sg"][:], p3(pb), AF.Sigmoid, [pk], K("sg"))
                pb, pk = newbank()
                hmm(pb, pk, lambda h: aup[di][0:65, (h0 + h) * 64:(h0 + h) * 64 + 64], lambda h: A["ad"][0:65, :],
                    K("ad", "ad1") + ["aup%d" % di])
                act(A["ic"][:], p3(pb), AF.Sigmoid, [pk], K("ic"))
                yield
                cx.op("dve", lambda e: e.tensor_tensor_scan(out=A["PF"][:].rearrange("p h t -> p (h t)"),
                                                            data0=rmask[:].rearrange("p h t -> p (h t)"),
                                                            data1=A["sg"][:].rearrange("p h t -> p (h t)"),
                                                            initial=0.0, op0=ALU.mult, op1=ALU.add),
                      reads=["rmask"] + K("sg"), writes=K("PF"))
                cx.op("dve", lambda e: e.tensor_copy(out=A["tot"][:], in_=A["PF"][:, :, 63]), reads=K("PF"), writes=K("tot"))
                if di == 0:
                    tt("dve", A["Pex"][:], A["PF"][:], A["sg"][:], ALU.subtract, K("PF", "sg"), K("Pex"))
                    pin = "PF"
                else:
                    tt("dve", A["Pex"][:], bcl(A["tot"]), A["PF"][:], ALU.subtract, K("PF", "tot"), K("Pex"))
                    tt("dve", A["Pin"][:], A["Pex"][:], A["sg"][:], ALU.add, K("Pex", "sg"), K("Pin"))
                    pin = "Pin"
                tt("dve", A["em"][:], bcl(A["tot"]), A[pin][:], ALU.subtract, K(pin, "tot"), K("em"))
                tt("pool", A["kx"][:], A["kT"][:], bc(kkp), ALU.mult, K("kT") + ["kkp"], K("kx"))
                tt("pool", A["t2"][:], A["kx"][:], A["kx"][:], ALU.mult, K("kx"), K("t2"))
                yield
                act(A["ea"][:], A["Pex"][:], AF.Exp, K("Pex"), K("ea"), scale=-C0)
                act(A["er"][:], A[pin][:], AF.Exp, K(pin), K("er"), scale=-C0)
                act(A["ei"][:], A[pin][:], AF.Exp, K(pin), K("ei"), scale=C0)
                act(A["em"][:], A["em"][:], AF.Exp, K("em"), K("em"), scale=-C0)
                act(A["etot"][:], A["tot"][:], AF.Exp, K("tot"), K("etot"), scale=-C0)
                pb, pk = newbank()
                cx.op("pe", lambda e: e.matmul(out=pb[0:64, 0:HG * 64], lhsT=ones64[:, :], rhs=A["t2"][:, :, :], start=True,
                                               stop=True), reads=K("t2") + ["ones64"], writes=[pk])
                act(A["kk"][:], p3(pb), AF.Sqrt, [pk], K("kk"))
                tt("pool", A["t1"][:], A["ic"][:], bc(kap), ALU.mult, K("ic") + ["kap"], K("t1"))
                tt("pool", A["t1"][:], A["t1"][:], bc(omka), ALU.add, K("t1") + ["omka"], K("t1"))
                tt("pool", A["kt"][:], A["kT"][:], A["t1"][:], ALU.mult, K("kT", "t1"), K("kt"))
                yield
                cx.op("dve", lambda e: e.tensor_scalar(out=A["kk"][:], in0=A["kk"][:], scalar1=1e-12, scalar2=None,
                                                       op0=ALU.max), reads=K("kk"), writes=K("kk"))
                cx.op("dve", lambda e: e.reciprocal(out=A["kk"][:], in_=A["kk"][:]), reads=K("kk"), writes=K("kk"))
                tt("dve", A["kk"][:], A["kk"][:], A["kx"][:], ALU.mult, K("kk", "kx"), K("kk"))
                tt("pool", A["kki"][:], A["kk"][:], A["ic"][:], ALU.mult, K("kk", "ic"), K("kki"))
                LB4 = A["LB"][:].rearrange("p h (s t) -> p h s t", s=2)
                RA4 = A["RA"][:].rearrange("p h (s t) -> p h s t", s=2)
                LP4 = A["LBp"][:].rearrange("p h (s t) -> p h s t", s=2)
                tt("pool", LB4[:, :, 0, :], A["kki"][:], A["ei"][:], ALU.mult, K("kki", "ei"), K("LB"))
                tt("pool", LB4[:, :, 1, :], A["kt"][:], A["ei"][:], ALU.mult, K("kt", "ei", "LB"), K("LB"))
                cx.op("dve", lambda e: e.scalar_tensor_tensor(out=RA4[:, :, 0, :], in0=A["kk"][:], scalar=-1.0,
                                                              in1=A["ea"][:], op0=ALU.mult, op1=ALU.mult),
                      reads=K("kk", "ea"), writes=K("RA"))
                tt("dve", RA4[:, :, 1, :], A["rT"][:], A["er"][:], ALU.mult, K("rT", "er", "RA"), K("RA"))
                tt("pool", LP4[:, :, 0, :], A["kki"][:], A["em"][:], ALU.mult, K("kki", "em"), K("LBp"))
                tt("pool", LP4[:, :, 1, :], A["kt"][:], A["em"][:], ALU.mult, K("kt", "em", "LBp"), K("LBp"))
                if want_y:
                    tt("pool", A["t1"][:], A["rT"][:], A["kt"][:], ALU.mult, K("rT", "kt"), K("t1"))
                    tt("pool", A["t2"][:], A["t1"][:], bc(rkp), ALU.mult, K("t1") + ["rkp"], K("t2"))
                yield
                if want_y:
                    pb, pk = newbank()
                    hmm(pb, pk, lambda h: A["t2"][:, h, :], lambda h: ones64[:, 0:2], K("t2") + ["ones64"], w=2)
                    cx.op("act", lambda e: e.copy(out=A["rkr"][:], in_=pb[0:64, 0:2 * HG].rearrange("p (h t) -> p h t", t=2)[:, :, 0]),
                          reads=[pk], writes=K("rkr"))
                pb, pk = newbank()
                for h in range(HG):
                    cx.op("pe", lambda e: e.matmul(out=pb[:, h * 128:h * 128 + 128], lhsT=A["LB"][:, h, :], rhs=A["RA"][:, h, :],
                                                   start=True, stop=True), reads=K("LB", "RA"), writes=[pk], last=(h == HG - 1))
                tt("dve", A["GM"][:], pb[:, 0:HG * 128].rearrange("p (h t) -> p h t", h=HG),
                   maskG[di][:].unsqueeze(1).to_broadcast([128, HG, 128]), ALU.mult, [pk, "maskG%d" % di], K("GM"))
                pb, pk = newbank()
                hmm(pb, pk, lambda h: A["RA"][:, h, 0:64], lambda h: A["LB"][:, h, 0:64], K("RA", "LB"))
                tt("dve", A["Nk0"][:], p3(pb), maskN[di][:].unsqueeze(1).to_broadcast([64, HG, 64]), ALU.mult,
                   [pk, "maskN%d" % di], K("Nk0"))
                yield
                cx.op("act", lambda e: e.copy(out=A["Mk0"][:], in_=A["GM"][0:64, :, 0:64]), reads=K("GM"), writes=K("Mk0"))
                tt("pool", A["TT0"][:], A["GM"][0:64, :, 0:64], identf[0:64, 0:64].unsqueeze(1).to_broadcast([64, HG, 64]),
                   ALU.add, K("GM") + ["identf"], K("TT0"))
                pb, pk = newbank()
                for h in range(HG):
                    cx.op("pe", lambda e: e.transpose(out=pb[:, h * 64:h * 64 + 64], in_=A["vpad"][:, h, :],
                                                      identity=identf[0:64, 0:64]), reads=K("vpad") + ["identf"], writes=[pk], last=(h == HG - 1))
                cx.op("act", lambda e: e.copy(out=A["UV"][64:128, :, :], in_=pb[64:128, 0:HG * 64].rearrange("p (h t) -> p h t", h=HG)),
                      reads=[pk], writes=K("UVv"))
                pb, pk = newbank()
                for h in range(HG):
                    cx.op("pe", lambda e: e.transpose(out=pb[:, h * 64:h * 64 + 64], in_=A["LBp"][:, h, :],
                                                      identity=identf[0:64, 0:64]), reads=K("LBp") + ["identf"], writes=[pk], last=(h == HG - 1))
                cx.op("act", lambda e: e.copy(out=A["BK"][:], in_=pb[:, 0:HG * 64].rearrange("p (h t) -> p h t", h=HG)),
                      reads=[pk], writes=K("BK"))
                if ci + 1 < len(chunks):
                    loads(chunks[ci + 1])
                yield
                cur = 0
                for lvl in range(5):
                    nxt = 1 - cur
                    Nc, Mc, Nn, Mn = "Nk%d" % cur, "Mk%d" % cur, "Nk%d" % nxt, "Mk%d" % nxt
                    pb, pk = newbank()
                    hmm(pb, pk, lambda h: A[Mc][:, h, :], lambda h: A[Nc][:, h, :], K(Mc, Nc))
                    cx.op("act", lambda e: e.copy(out=A[Nn][:], in_=p3(pb)), reads=[pk], writes=K(Nn))
                    if lvl < 4:
                        pb, pk = newbank()
                        hmm(pb, pk, lambda h: A[Nc][:, h, :], lambda h: A[Mc][:, h, :], K(Mc, Nc))
                        cx.op("dve", lambda e: e.tensor_copy(out=A[Mn][:], in_=p3(pb)), reads=[pk], writes=K(Mn))
                    yield
                    Tc, Tn = "TT%d" % cur, "TT%d" % nxt
                    pb, pk = newbank()
                    hmm(pb, pk, lambda h: A[Nn][:, h, :], lambda h: A[Tc][:, h, :], K(Nn, Tc))
                    tt("dve", A[Tn][:], p3(pb), A[Tc][:], ALU.add, [pk] + K(Tc), K(Tn))
                    cur = nxt
                    yield
                TTf = "TT%d" % cur
                pb, pk = newbank()
                hmm(pb, pk, lambda h: A["GM"][64:128, h, 0:64], lambda h: A["UV"][64:128, h, :], K("GM", "UVv"))
                cx.op("act", lambda e: e.copy(out=A["Q0"][:], in_=p3(pb)), reads=[pk], writes=K("Q0"))
                yield
                pb, pk = newbank()
                hmm(pb, pk, lambda h: A["RA"][:, h, 0:64], lambda h: A["STr"][:, h, :], K("RA", "STr"))
                tt("dve", A["X"][:], p3(pb), A["Q0"][:], ALU.add, [pk] + K("Q0"), K("X"))
                yield
                pb, pk = newbank()
                hmm(pb, pk, lambda h: A[TTf][:, h, :], lambda h: A["X"][:, h, :], K(TTf, "X"))
                cx.op("act", lambda e: e.copy(out=A["UV"][0:64, :, :], in_=p3(pb)), reads=[pk], writes=K("UVu"))
                yield
                if want_y:
                    pb, pk = newbank()
                    for h in range(HG):
                        cx.op("pe", lambda e: e.matmul(out=pb[0:64, h * 64:h * 64 + 64], lhsT=A["RA"][:, h, 64:128],
                                                       rhs=A["STr"][:, h, :], start=True, stop=False),
                              reads=K("RA", "STr"), writes=[pk], last=False)
                        cx.op("pe", lambda e: e.matmul(out=pb[0:64, h * 64:h * 64 + 64], lhsT=A["GM"][:, h, 64:128],
                                                       rhs=A["UV"][:, h, :], start=False, stop=True),
                              reads=K("GM", "UVu", "UVv"), writes=[pk], last=(h == HG - 1))
                    cx.op("act", lambda e: e.copy(out=A["YO"][:], in_=p3(pb)), reads=[pk], writes=K("YO"))
                    cx.dma("act", T[ydst][t0 - 64:t0, h0 * 64:(h0 + HG) * 64], A["YO"][:].rearrange("p h t -> p (h t)"),
                           reads=K("YO"))
                    cx.dma("act", T[ydst][t0 - 64:t0, 1024 + h0:1024 + h0 + HG], A["rkr"][:], reads=K("rkr"))
                    if di == 0:
                        cx.dma("act", T["VTM"][t0 - 64:t0, h0 * 64:(h0 + HG) * 64],
                               A["UV"][64:128, :, :].bitcast(F32).rearrange("p h t -> p (h t)"), reads=K("UVv"))
                pb, pk = newbank()
                hmm(pb, pk, lambda h: A["BK"][:, h, :], lambda h: A["UV"][:, h, :], K("BK", "UVu", "UVv"))
                tt("dve", A["ST"][:], A["ST"][:], bcl(A["etot"]), ALU.mult, K("ST", "etot"), K("ST"))
                tt("dve", A["ST"][:], A["ST"][:], p3(pb), ALU.add, K("ST") + [pk], K("ST"))
                cx.op("act", lambda e: e.copy(out=A["STr"][:], in_=A["ST"][:]), reads=K("ST"), writes=K("STr"))
                yield

        own = set(range(1, 33))
        for (di, chunks, ydst) in [(0, list(range(0, 33)), "YF"), (1, list(range(65, 0, -1)), "YB")]:
            gens = [run_chain(cid, di, chunks, own, ydst) for cid in range(16 // HG)]
            alive = list(gens)
            while alive:
                for g in list(alive):
                    try:
                        next(g)
                    except StopIteration:
                        alive.remove(g)
    cx.barrier()


def load_w_bf16(cx, nc, es, name, src, K, N, stg, stgkey):
    w = es.enter_context(nc.sbuf_tensor(name, [128, K // 128, N], BF16))
    for c in range(K // 128):
        for n0 in range(0, N, 2048):
            n = min(2048, N - n0)
            cx.dma("pool", w[:, c, n0:n0 + n], src[c * 128:(c + 1) * 128, n0:n0 + n], writes=[name])
    return w


def stage_post(cx, nc, T, PQ):
    with ExitStack() as es:
        def sb(name, shape, dt=F32):
            return es.enter_context(nc.sbuf_tensor(name, shape, dt))
        H = 16
        ident = sb("identP", [128, 128])
        lng = sb("lng", [128, 1024])
        lnb = sb("lnb", [128, 1024])
        gup0 = sb("gup0", [128, 1024])
        gup1 = sb("gup1", [32, 1024])
        gne = sb("gne", [128, 1])
        yf = sb("yf", [128, 1040])
        yb = sb("yb", [128, 1040])
        vt = sb("vt", [128, H, 64])
        y = sb("ypost", [128, H, 64])
        sq = sb("sqpost", [128, H, 64])
        mu = sb("mu", [128, H])
        var = sb("var", [128, H])
        bon = sb("bon", [128, H])
        sg0 = sb("sg0", [128, 128])
        sg1 = sb("sg1", [32, 128])
        yrt = sb("yrt", [128, 8, 128], BF16)
        cx.dma("sp", ident[:], T["ident"], writes=["ident"])
        cx.dma("sp", lng[:], T["lng_b"], writes=["lng"])
        cx.dma("sp", lnb[:], T["lnb_b"], writes=["lnb"])
        cx.dma("sp", gup0[:], T["g_up"][0:128, :], writes=["gup0"])
        cx.dma("sp", gup1[:], T["g_up"][128:160, :], writes=["gup1"])
        cx.op("dve", lambda e: e.memset(gne[:], 64e-5), writes=["gne"])
        for ti in range(16):
            t0 = ti * 128
            cx.dma("sp", yf[:], T["YF"][t0:t0 + 128, :], writes=["yf"])
            cx.dma("sp", yb[:], T["YB"][t0:t0 + 128, :], writes=["yb"])
            cx.dma("sp", vt[:].rearrange("p h t -> p (h t)"), T["VTM"][t0:t0 + 128, :], writes=["vt"])
            cx.dma("sp", sg0[:], T["XSL"][0:128, 64 + t0:64 + t0 + 128], writes=["sg0"])
            cx.dma("sp", sg1[:], T["XSL"][128:160, 64 + t0:64 + t0 + 128], writes=["sg1"])
            cx.op("act", lambda e: e.activation(out=sg0[:], in_=sg0[:], func=AF.Sigmoid), reads=["sg0"], writes=["sg0"])
            cx.op("act", lambda e: e.activation(out=sg1[:], in_=sg1[:], func=AF.Sigmoid), reads=["sg1"], writes=["sg1"])
            pq = PQ[ti % 2]
            pk = ("pq", ti % 2)
            for nb in range(2):
                cx.op("pe", lambda e: e.matmul(out=pq[:, nb * 512:nb * 512 + 512], lhsT=sg0[:, :],
                                               rhs=gup0[:, nb * 512:nb * 512 + 512], start=True, stop=False),
                      reads=["sg0", "gup0"], writes=[pk], last=False)
                cx.op("pe", lambda e: e.matmul(out=pq[:, nb * 512:nb * 512 + 512], lhsT=sg1[:, :],
                                               rhs=gup1[:, nb * 512:nb * 512 + 512], start=False, stop=True),
                      reads=["sg1", "gup1"], writes=[pk], last=(nb == 1))
            y3f = yf[:, 0:1024].rearrange("p (h t) -> p h t", h=H)
            y3b = yb[:, 0:1024].rearrange("p (h t) -> p h t", h=H)
            cx.op("dve", lambda e: e.tensor_tensor(out=y[:], in0=y3f, in1=y3b, op=ALU.add), reads=["yf", "yb"], writes=["y"])
            cx.op("dve", lambda e: e.tensor_reduce(out=mu[:], in_=y[:], axis=AX.X, op=ALU.add), reads=["y"], writes=["mu"])
            cx.op("dve", lambda e: e.tensor_scalar(out=mu[:], in0=mu[:], scalar1=1.0 / 64, scalar2=None, op0=ALU.mult),
                  reads=["mu"], writes=["mu"])
            cx.op("dve", lambda e: e.tensor_tensor(out=y[:], in0=y[:], in1=mu[:].unsqueeze(2).to_broadcast([128, H, 64]),
                                                   op=ALU.subtract), reads=["y", "mu"], writes=["y"])
            cx.op("dve", lambda e: e.tensor_tensor(out=sq[:], in0=y[:], in1=y[:], op=ALU.mult), reads=["y"], writes=["sq"])
            cx.op("dve", lambda e: e.tensor_reduce(out=var[:], in_=sq[:], axis=AX.X, op=ALU.add), reads=["sq"], writes=["var"])
            cx.op("act", lambda e: e.activation(out=var[:], in_=var[:], func=AF.Sqrt, scale=1.0 / 64, bias=gne[:, 0:1]),
                  reads=["var", "gne"], writes=["var"])
            cx.op("dve", lambda e: e.reciprocal(out=var[:], in_=var[:]), reads=["var"], writes=["var"])
            cx.op("dve", lambda e: e.tensor_tensor(out=y[:], in0=y[:], in1=var[:].unsqueeze(2).to_broadcast([128, H, 64]),
                                                   op=ALU.mult), reads=["y", "var"], writes=["y"])
            yfl = y[:].rearrange("p h t -> p (h t)")
            cx.op("dve", lambda e: e.tensor_tensor(out=yfl, in0=yfl, in1=lng[:], op=ALU.mult), reads=["y", "lng"], writes=["y"])
            cx.op("dve", lambda e: e.tensor_tensor(out=yfl, in0=yfl, in1=lnb[:], op=ALU.add), reads=["y", "lnb"], writes=["y"])
            cx.op("dve", lambda e: e.tensor_tensor(out=bon[:], in0=yf[:, 1024:1040], in1=yb[:, 1024:1040], op=ALU.add),
                  reads=["yf", "yb"], writes=["bon"])
            cx.op("dve", lambda e: e.scalar_tensor_tensor(out=sq[:], in0=vt[:], scalar=0.5,
                                                          in1=bon[:].unsqueeze(2).to_broadcast([128, H, 64]),
                                                          op0=ALU.mult, op1=ALU.mult), reads=["vt", "bon"], writes=["sq"])
            cx.op("dve", lambda e: e.tensor_tensor(out=y[:], in0=y[:], in1=sq[:], op=ALU.add), reads=["y", "sq"], writes=["y"])
            cx.op("dve", lambda e: e.tensor_tensor(out=yfl, in0=yfl, in1=pq[:, :], op=ALU.mult), reads=["y", pk], writes=["y"])
            pq2 = PQ[2 + ti % 2]
            pk2 = ("pq", 2 + ti % 2)
            for c in range(8):
                cx.op("pe", lambda e: e.transpose(out=pq2[:, c * 128:(c + 1) * 128], in_=yfl[:, c * 128:(c + 1) * 128],
                                                  identity=ident[:, :]), reads=["y", "ident"], writes=[pk2], last=(c == 7))
            cx.op("act", lambda e: e.copy(out=yrt[:], in_=pq2[:, :].rearrange("p (c t) -> p c t", c=8)),
                  reads=[pk2], writes=["yrt"])
            cx.dma("sp", T["YRT"][:, t0:t0 + 128].rearrange("(c p) t -> p c t", p=128), yrt[:], reads=["yrt"])
    cx.barrier()


def stage_mla(cx, nc, T, PQ, PS):
    with ExitStack() as es:
        def sb(name, shape, dt=F32):
            return es.enter_context(nc.sbuf_tensor(name, shape, dt))
        ident = sb("identM", [128, 128])
        ones = sb("onesM", [128, 128])
        stg = [sb("mstg%d" % i, [128, 2048]) for i in range(2)]
        epsc = sb("epscM", [128, 1])
        negpi = sb("negpi", [64, 1])
        invf = sb("invfs", [64, 1])
        gq = sb("gq", [128, 4])
        gkv = sb("gkv", [128, 4])
        kbias = sb("kbias", [128, 33])
        kvn = sb("kvnT", [128, 4, NTOK], BF16)
        qn = sb("qnT", [128, 4, NOWN], BF16)
        cos2 = sb("cos2", [64, NTOK])
        sin2 = sb("sin2", [64, NTOK])
        kr = sb("krT", [128, NTOK], BF16)
        cx.dma("sp", ident[:], T["ident"], writes=["ident"])
        cx.dma("sp", invf[:], T["invf"], writes=["invf"])
        cx.dma("sp", gq[:], T["gq_fm"], writes=["gq"])
        cx.dma("sp", gkv[:], T["gkv_fm"], writes=["gkv"])
        cx.dma("sp", kbias[:], T["valid_tm"], writes=["kbias"])
        cx.op("dve", lambda e: e.tensor_scalar(out=kbias[:], in0=kbias[:], scalar1=-1.0, scalar2=30000.0, op0=ALU.add,
                                               op1=ALU.mult), reads=["kbias"], writes=["kbias"])
        cx.op("dve", lambda e: e.memset(ones[:], 1.0), writes=["ones"])
        cx.op("dve", lambda e: e.memset(epsc[:], EPS), writes=["epsc"])
        cx.op("dve", lambda e: e.memset(negpi[:], -float(np.pi)), writes=["negpi"])
        wuq = load_w_bf16(cx, nc, es, "wuq", T["w_uq"], 512, 3072, stg, "mstg")
        wukv = load_w_bf16(cx, nc, es, "wukv", T["w_ukv"], 512, 4096, stg, "mstg")
        wqr = sb("wqrot", [128, 4, 16, 64], BF16)
        wuq4 = wuq[:].rearrange("p c (h e) -> p c h e", h=16)
        cx.op("pool", lambda e: e.tensor_scalar(out=wqr[:, :, :, 0:32], in0=wuq4[:, :, :, 160:192], scalar1=-1.0,
                                                scalar2=None, op0=ALU.mult), reads=["wuq"], writes=["wqr"])
        cx.op("pool", lambda e: e.tensor_copy(out=wqr[:, :, :, 32:64], in_=wuq4[:, :, :, 128:160]), reads=["wuq"],
              writes=["wqr"])
        cx.barrier()
        kf = stg[0][0:64, 0:1056]
        ki = stg[1][0:64, 0:1056].bitcast(I32)
        TWO_PI = float(2 * np.pi)
        for (tab, off, nm) in [(sin2, 0.0, "sin2"), (cos2, float(0.5 * np.pi), "cos2")]:
            for hb in range(4):
                cs = slice(hb * 1056, hb * 1056 + 1056)
                cx.dma("sp", tab[:, cs], T["pos64"][:, cs], writes=[nm])
                cx.op("dve", lambda e: e.tensor_scalar(out=tab[:, cs], in0=tab[:, cs], scalar1=invf[:, 0:1], scalar2=off,
                                                       op0=ALU.mult, op1=ALU.add), reads=[nm, "invf"], writes=[nm])
                cx.op("dve", lambda e: e.tensor_scalar(out=kf, in0=tab[:, cs], scalar1=1.0 / TWO_PI, scalar2=None,
                                                       op0=ALU.mult), reads=[nm], writes=["kf"])
                cx.op("dve", lambda e: e.tensor_copy(out=ki, in_=kf), reads=["kf"], writes=["ki"])
                cx.op("dve", lambda e: e.tensor_copy(out=kf, in_=ki), reads=["ki"], writes=["kf"])
                cx.op("dve", lambda e: e.scalar_tensor_tensor(out=tab[:, cs], in0=kf, scalar=-TWO_PI, in1=tab[:, cs],
                                                              op0=ALU.mult, op1=ALU.add), reads=["kf", nm], writes=[nm])
                cx.op("dve", lambda e: e.tensor_scalar(out=kf, in0=tab[:, cs], scalar1=float(np.pi), scalar2=None,
                                                       op0=ALU.is_gt), reads=[nm], writes=["kf"])
                cx.op("dve", lambda e: e.scalar_tensor_tensor(out=tab[:, cs], in0=kf, scalar=-TWO_PI, in1=tab[:, cs],
                                                              op0=ALU.mult, op1=ALU.add), reads=["kf", nm], writes=[nm])
                cx.op("dve", lambda e: e.tensor_scalar(out=kf, in0=tab[:, cs], scalar1=-float(np.pi), scalar2=None,
                                                       op0=ALU.is_lt), reads=[nm], writes=["kf"])
                cx.op("dve", lambda e: e.scalar_tensor_tensor(out=tab[:, cs], in0=kf, scalar=TWO_PI, in1=tab[:, cs],
                                                              op0=ALU.mult, op1=ALU.add), reads=["kf", nm], writes=[nm])
                cx.op("act", lambda e: e.activation(out=tab[:, cs], in_=tab[:, cs], func=AF.Sin),
                      reads=[nm], writes=[nm])
        cx.barrier()
        def latent_norm(row0, tok0, ntok, dst, g, nm):
            for b0 in range(0, ntok, 512):
                n = min(512, ntok - b0)
                xs_ = []
                pq, pk = PQ[0], ("pq", 0)
                for c in range(4):
                    st_ = stg[c % 2]
                    half = (c // 2) * 1024
                    cx.dma("sp", st_[:, half:half + n], T["PRT"][row0 + c * 128:row0 + c * 128 + 128,
                                                                  1 + tok0 + b0:1 + tok0 + b0 + n],
                           writes=[("mstg", c % 2, c // 2)])
                    cx.op("act", lambda e: e.activation(out=st_[:, half + 512:half + 512 + n], in_=st_[:, half:half + n],
                                                        func=AF.Square),
                          reads=[("mstg", c % 2, c // 2)], writes=[("msq", c % 2, c // 2)])
                    cx.op("pe", lambda e: e.matmul(out=pq[:, 0:n], lhsT=ones[:, :], rhs=st_[:, half + 512:half + 512 + n],
                                                   start=(c == 0), stop=(c == 3)),
                          reads=[("msq", c % 2, c // 2), "ones"], writes=[pk], last=(c == 3))
                cx.op("act", lambda e: e.activation(out=pq[:, 512:512 + n], in_=pq[:, 0:n], func=AF.Sqrt, scale=1.0 / 512,
                                                    bias=epsc[:, 0:1]), reads=[pk, "epsc"], writes=[("pqb", 0)])
                cx.op("dve", lambda e: e.reciprocal(out=pq[:, 512:512 + n], in_=pq[:, 512:512 + n]),
                      reads=[("pqb", 0)], writes=[("pqb", 0)])
                for c in range(4):
                    st_ = stg[c % 2]
                    half = (c // 2) * 1024
                    cx.op("dve", lambda e: e.scalar_tensor_tensor(out=dst[:, c, b0:b0 + n], in0=st_[:, half:half + n],
                                                                  scalar=g[:, c:c + 1], in1=pq[:, 512:512 + n],
                                                                  op0=ALU.mult, op1=ALU.mult),
                          reads=[("mstg", c % 2, c // 2), ("pqb", 0), nm], writes=[("dst", nm)])
                    cx.buf[("msq", c % 2, c // 2)] = cx.buf.get(("msq", c % 2, c // 2), {"w": None, "r": {}})
        latent_norm(R_KVD, 0, NTOK, kvn, gkv, "gkv")
        latent_norm(R_QD, OWN0, NOWN, qn, gq, "gq")
        for b0 in range(0, NTOK, 1024):
            n = min(1024, NTOK - b0)
            cx.dma("sp", stg[0][0:64, 0:n], T["PRT"][R_KR:R_KR + 64, 1 + b0:1 + b0 + n], writes=[("mstg", 0, 0), ("mstg", 0, 1)])
            cx.dma("sp", stg[1][0:64, 0:n], T["PRT"][R_KRR:R_KRR + 64, 1 + b0:1 + b0 + n], writes=[("mstg", 1, 0), ("mstg", 1, 1)])
            cx.op("dve", lambda e: e.tensor_tensor(out=stg[0][0:64, 0:n], in0=stg[0][0:64, 0:n], in1=cos2[:, b0:b0 + n],
                                                   op=ALU.mult), reads=[("mstg", 0, 0), ("mstg", 0, 1), "cos2"],
                  writes=[("mstg", 0, 0), ("mstg", 0, 1)])
            cx.op("dve", lambda e: e.tensor_tensor(out=stg[1][0:64, 0:n], in0=stg[1][0:64, 0:n], in1=sin2[:, b0:b0 + n],
                                                   op=ALU.mult), reads=[("mstg", 1, 0), ("mstg", 1, 1), "sin2"],
                  writes=[("mstg", 1, 0), ("mstg", 1, 1)])
            cx.op("dve", lambda e: e.tensor_tensor(out=kr[0:64, b0:b0 + n], in0=stg[0][0:64, 0:n], in1=stg[1][0:64, 0:n],
                                                   op=ALU.add), reads=[("mstg", 0, 0), ("mstg", 0, 1), ("mstg", 1, 0), ("mstg", 1, 1)],
                  writes=["kr"])
        cx.barrier()
        kT = sb("kTh", [128, NTOK], BF16)
        Vh = sb("Vh", [128, 33, 132], BF16)
        qT = sb("qTh", [128, NOWN], BF16)
        qr = sb("qrh", [128, NOWN], BF16)
        qa = sb("qra", [128, 512])
        qb_ = sb("qrb", [64, 512])
        PT = [sb("PT%d" % i, [128, 512], BF16) for i in range(2)]
        ymt = [sb("ymt%d" % i, [128, 512], BF16) for i in range(2)]
        rs = sb("rsum", [1, 512])
        bcs = qa
        ones1 = sb("ones1", [1, 128])
        onesb = sb("onesb", [128, 128], BF16)
        cx.op("dve", lambda e: e.memset(ones1[:], 1.0), writes=["ones1"])
        cx.op("dve", lambda e: e.memset(onesb[:], 1.0), writes=["onesb"])
        qi_box = [0]
        cx.op("dve", lambda e: e.memset(Vh[:, :, 128:129], 1.0), writes=["Vh1"])
        cx.op("pool", lambda e: e.memset(kr[64:128, :], 0.0), writes=["kr0"])
        cx.op("pool", lambda e: e.memset(qr[64:128, :], 0.0), writes=["qr0"])
        scale = float(192 ** -0.5)
        kk = 0
        kk_box = [0]
        for h in range(16):
            for b0 in range(0, NTOK, 512):
                n = min(512, NTOK - b0)
                pb, pbk = PS[kk % 2], ("ps", kk % 2)
                kk += 1
                for c in range(4):
                    cx.op("pe", lambda e: e.matmul(out=pb[:, 0:n], lhsT=wukv[:, c, h * 256:h * 256 + 128],
                                                   rhs=kvn[:, c, b0:b0 + n], start=(c == 0), stop=(c == 3)),
                          reads=["wukv", ("dst", "gkv")], writes=[pbk], last=(c == 3))
                cx.op("act", lambda e: e.copy(out=kT[:, b0:b0 + n], in_=pb[:, 0:n]), reads=[pbk], writes=["kT"])
            for kt in range(33):
                pb, pbk = PS[kk % 2], ("ps", kk % 2)
                kk += 1
                for c in range(4):
                    cx.op("pe", lambda e: e.matmul(out=pb[:, 0:128], lhsT=kvn[:, c, kt * 128:kt * 128 + 128],
                                                   rhs=wukv[:, c, h * 256 + 128:h * 256 + 256], start=(c == 0), stop=(c == 3)),
                          reads=["wukv", ("dst", "gkv")], writes=[pbk], last=(c == 3))
                cx.op("dve", lambda e: e.tensor_copy(out=Vh[:, kt, 0:128], in_=pb[:, 0:128]), reads=[pbk], writes=["Vh"])
            for b0 in range(0, NOWN, 512):
                pb, pbk = PS[kk % 2], ("ps", kk % 2)
                kk += 1
                for c in range(4):
                    cx.op("pe", lambda e: e.matmul(out=pb[:, 0:512], lhsT=wuq[:, c, h * 192:h * 192 + 128],
                                                   rhs=qn[:, c, b0:b0 + 512], start=(c == 0), stop=(c == 3)),
                          reads=["wuq", ("dst", "gq")], writes=[pbk], last=(c == 3))
                cx.op("act", lambda e: e.copy(out=qT[:, b0:b0 + 512], in_=pb[:, 0:512]), reads=[pbk], writes=["qT"])
                pb, pbk = PS[kk % 2], ("ps", kk % 2)
                kk += 1
                for c in range(4):
                    cx.op("pe", lambda e: e.matmul(out=pb[0:64, 0:512], lhsT=wuq[:, c, h * 192 + 128:h * 192 + 192],
                                                   rhs=qn[:, c, b0:b0 + 512], start=(c == 0), stop=(c == 3)),
                          reads=["wuq", ("dst", "gq")], writes=[pbk], last=(c == 3))
                cx.op("dve", lambda e: e.tensor_tensor(out=qa[0:64, :], in0=pb[0:64, 0:512], in1=cos2[:, OWN0 + b0:OWN0 + b0 + 512],
                                                       op=ALU.mult), reads=[pbk, "cos2"], writes=["qa"])
                pb, pbk = PS[kk % 2], ("ps", kk % 2)
                kk += 1
                for c in range(4):
                    cx.op("pe", lambda e: e.matmul(out=pb[0:64, 0:512], lhsT=wqr[:, c, h, :],
                                                   rhs=qn[:, c, b0:b0 + 512], start=(c == 0), stop=(c == 3)),
                          reads=["wqr", ("dst", "gq")], writes=[pbk], last=(c == 3))
                cx.op("dve", lambda e: e.tensor_tensor(out=qb_[:], in0=pb[0:64, 0:512], in1=sin2[:, OWN0 + b0:OWN0 + b0 + 512],
                                                       op=ALU.mult), reads=[pbk, "sin2"], writes=["qb"])
                cx.op("dve", lambda e: e.tensor_tensor(out=qr[0:64, b0:b0 + 512], in0=qa[0:64, :], in1=qb_[:], op=ALU.add),
                      reads=["qa", "qb"], writes=["qr"])
            for qblk in range(4):
                q0 = qblk * 512
                sbank = {}

                def emit_S(kt):
                    i = kk_box[0] % 2
                    kk_box[0] += 1
                    pb_, pbk_ = PS[i], ("ps", i)
                    cx.op("pe", lambda e: e.matmul(out=pb_[:, 0:512], lhsT=kT[:, kt * 128:kt * 128 + 128],
                                                   rhs=qT[:, q0:q0 + 512], start=True, stop=False),
                          reads=["kT", "qT"], writes=[pbk_], last=False)
                    cx.op("pe", lambda e: e.matmul(out=pb_[:, 0:512], lhsT=kr[:, kt * 128:kt * 128 + 128],
                                                   rhs=qr[:, q0:q0 + 512], start=False, stop=True),
                          reads=["kr", "kr0", "qr", "qr0"], writes=[pbk_])
                    sbank[kt] = (pb_, pbk_)

                emit_S(0)
                for kt in range(33):
                    if kt + 1 < 33:
                        emit_S(kt + 1)
                    pb, pbk = sbank.pop(kt)
                    pt = PT[kt % 2]
                    cx.op("act", lambda e: e.activation(out=pt[:], in_=pb[:, 0:512], func=AF.Exp, scale=scale,
                                                        bias=kbias[:, kt:kt + 1]),
                          reads=[pbk, "kbias"], writes=[("PT", kt % 2)])
                    ob_, sb_ = 4 + 2 * (qi_box[0] % 2), 5 + 2 * (qi_box[0] % 2)
                    cx.op("pe", lambda e: e.matmul(out=PS[ob_][:, 0:512], lhsT=Vh[:, kt, 0:128], rhs=pt[:, 0:512],
                                                   start=(kt == 0), stop=(kt == 32)),
                          reads=[("PT", kt % 2), "Vh"], writes=[("ps", ob_)], last=False)
                    cx.op("pe", lambda e: e.matmul(out=PS[sb_][:, 0:512], lhsT=onesb[:, :], rhs=pt[:, 0:512],
                                                   start=(kt == 0), stop=(kt == 32)),
                          reads=[("PT", kt % 2), "onesb"], writes=[("ps", sb_)])
                cx.op("dve", lambda e: e.reciprocal(out=bcs[:], in_=PS[sb_][:, 0:512]), reads=[("ps", sb_)], writes=["qa"])
                ym = ymt[qi_box[0] % 2]
                cx.op("dve", lambda e: e.tensor_tensor(out=ym[:], in0=PS[ob_][:, 0:512], in1=bcs[:], op=ALU.mult),
                      reads=[("ps", ob_), "qa"], writes=[("ymt", qi_box[0] % 2)])
                cx.dma("sp", T["YMT"][h * 128:h * 128 + 128, q0:q0 + 512], ym[:], reads=[("ymt", qi_box[0] % 2)])
                qi_box[0] += 1
    cx.barrier()


def stage_merge(cx, nc, T, PQ, PS):
    with ExitStack() as es:
        def sb(name, shape, dt=F32):
            return es.enter_context(nc.sbuf_tensor(name, shape, dt))
        stg = [sb("gstg%d" % i, [128, 2048]) for i in range(2)]
        prw = load_w_bf16(cx, nc, es, "prw", T["p_rwkv"], 1024, 2048, stg, "gstg")
        pml = load_w_bf16(cx, nc, es, "pml", T["p_mla"], 2048, 2048, stg, "gstg")
        bg = sb("bgate", [128, 32])
        cx.dma("sp", bg[:], T["bgate_fm"], writes=["bg"])
        yr = sb("yrblk", [128, 8, 512], BF16)
        ym = sb("ymblk", [128, 16, 512], BF16)
        gr = sb("grblk", [128, 512])
        gm = sb("gmblk", [128, 512])
        mg = [sb("mgblk%d" % i, [128, 512], BF16) for i in range(2)]
        cx.barrier()
        kk = 0
        for tb in range(4):
            t0 = tb * 512
            cx.dma("sp", yr[:], T["YRT"][:, t0:t0 + 512].rearrange("(c p) t -> p c t", p=128), writes=["yr"])
            cx.dma("sp", ym[:], T["YMT"][:, t0:t0 + 512].rearrange("(c p) t -> p c t", p=128), writes=["ym"])
            for dc in range(16):
                cx.dma("sp", gr[:], T["PRT"][R_GR + dc * 128:R_GR + dc * 128 + 128, 1 + OWN0 + t0:1 + OWN0 + t0 + 512],
                       writes=["gr"])
                cx.dma("sp", gm[:], T["PRT"][R_GM + dc * 128:R_GM + dc * 128 + 128, 1 + OWN0 + t0:1 + OWN0 + t0 + 512],
                       writes=["gm"])
                cx.op("act", lambda e: e.activation(out=gr[:], in_=gr[:], func=AF.Sigmoid, bias=bg[:, dc:dc + 1]),
                      reads=["gr", "bg"], writes=["gr"])
                cx.op("act", lambda e: e.activation(out=gm[:], in_=gm[:], func=AF.Sigmoid, bias=bg[:, 16 + dc:17 + dc]),
                      reads=["gm", "bg"], writes=["gm"])
                pa, pak = PS[kk % 4], ("ps", kk % 4)
                pb, pbk = PS[4 + kk % 4], ("ps", 4 + kk % 4)
                kk += 1
                for c in range(8):
                    cx.op("pe", lambda e: e.matmul(out=pa[:, 0:512], lhsT=prw[:, c, dc * 128:dc * 128 + 128], rhs=yr[:, c, :],
                                                   start=(c == 0), stop=(c == 7)), reads=["prw", "yr"], writes=[pak], last=(c == 7))
                for c in range(16):
                    cx.op("pe", lambda e: e.matmul(out=pb[:, 0:512], lhsT=pml[:, c, dc * 128:dc * 128 + 128], rhs=ym[:, c, :],
                                                   start=(c == 0), stop=(c == 15)), reads=["pml", "ym"], writes=[pbk], last=(c == 15))
                cx.op("dve", lambda e: e.tensor_tensor(out=gr[:], in0=gr[:], in1=pa[:, 0:512], op=ALU.mult),
                      reads=["gr", pak], writes=["gr"])
                cx.op("dve", lambda e: e.tensor_tensor(out=gm[:], in0=gm[:], in1=pb[:, 0:512], op=ALU.mult),
                      reads=["gm", pbk], writes=["gm"])
                m_ = mg[dc % 2]
                cx.op("dve", lambda e: e.tensor_tensor(out=m_[:], in0=gr[:], in1=gm[:], op=ALU.add),
                      reads=["gr", "gm"], writes=[("mg", dc % 2)])
                cx.dma("sp", T["MGT"][dc * 128:dc * 128 + 128, t0:t0 + 512], m_[:], reads=[("mg", dc % 2)])
    cx.barrier()
    with ExitStack() as es:
        def sb(name, shape, dt=F32):
            return es.enter_context(nc.sbuf_tensor(name, shape, dt))
        stg = [sb("hstg%d" % i, [128, 2048]) for i in range(2)]
        wo = load_w_bf16(cx, nc, es, "wo", T["w_o"], 2048, 2048, stg, "hstg")
        mt = sb("mgt", [128, 16, 128], BF16)
        xt = sb("xres", [128, 2048])
        cx.barrier()
        for ti in range(16):
            t0 = ti * 128
            cx.dma("sp", mt[:], T["MGT"][:, t0:t0 + 128].rearrange("(c p) t -> p c t", p=128), writes=["mt"])
            cx.dma("sp", xt[:], T["xp"][OWN0 + t0:OWN0 + t0 + 128, :], writes=["xt"])
            for nb in range(4):
                pb, pbk = PS[(ti * 4 + nb) % 8], ("ps", (ti * 4 + nb) % 8)
                for c in range(16):
                    cx.op("pe", lambda e: e.matmul(out=pb[:, 0:512], lhsT=mt[:, c, :], rhs=wo[:, c, nb * 512:nb * 512 + 512],
                                                   start=(c == 0), stop=(c == 15)), reads=["mt", "wo"], writes=[pbk], last=(c == 15))
                cx.op("dve", lambda e: e.tensor_tensor(out=xt[:, nb * 512:nb * 512 + 512], in0=xt[:, nb * 512:nb * 512 + 512],
                                                       in1=pb[:, 0:512], op=ALU.add), reads=["xt", pbk], writes=["xt"])
            cx.dma("sp", T["H2"][t0:t0 + 128, :], xt[:], reads=["xt"])
    cx.barrier()


def stage_peer(cx, nc, T, PQ, PS):
    with ExitStack() as es0:
        def sb0(name, shape, dt=F32):
            return es0.enter_context(nc.sbuf_tensor(name, shape, dt))
        eidi_all = sb0("eidi_all", [128, 16, 128], I32)
        gate_all = sb0("gate_all", [128, 16, 128])
        ident = sb0("identE", [128, 128])
        gf = sb0("gffn", [128, 2048])
        epsc = sb0("epscE", [128, 1])
        cx.dma("sp", ident[:], T["ident"], writes=["ident"])
        cx.dma("sp", gf[:], T["gffn_b"], writes=["gf"])
        cx.op("dve", lambda e: e.memset(epsc[:], EPS), writes=["epsc"])
        with ExitStack() as es:
            def sb(name, shape, dt=F32):
                return es.enter_context(nc.sbuf_tensor(name, shape, dt))
            stg = [sb("estg%d" % i, [128, 2048]) for i in range(2)]
            wq = load_w_bf16(cx, nc, es, "wqp", T["peer_wq"], 2048, 2048, stg, "estg")
            cx.barrier()
            keysT = sb("keysT", [128, 16, 128])
            iota = sb("iotaE", [128, 256])
            cx.dma("sp", iota[:], T["iota256"], writes=["iota"])
            for g in range(16):
                cx.dma("sp", stg[0][:, g * 128:g * 128 + 128], T["peer_keys"][g], writes=["estg0"])
            for g in range(16):
                pb, pbk = PS[g % 2], ("ps", g % 2)
                cx.op("pe", lambda e: e.transpose(out=pb[:, 0:128], in_=stg[0][:, g * 128:g * 128 + 128], identity=ident[:, :]),
                      reads=["estg0", "ident"], writes=[pbk])
                cx.op("act", lambda e: e.copy(out=keysT[:, g, :], in_=pb[:, 0:128]), reads=[pbk], writes=["keysT"])
            cx.barrier()
            junk = stg[1][:]
            JK = "junkA"
            h2 = sb("h2", [128, 2048])
            hn = sb("hn", [128, 2048])
            hnT = sb("hnT", [128, 16, 128], BF16)
            qT = sb("qTp", [128, 16, 128])
            sc = sb("scp", [128, 16, 128])
            sc2 = sb("scp2", [128, 128])
            tops = sb("tops", [128, 16, 16])
            topi = sb("topi", [128, 16, 16])
            tiu_all = sb("tiu_all", [128, 16, 16], U32)
            piu = sb("piu", [128, 8, 16], U32)
            sc2_all = sb("sc2_all", [128, 16, 128])
            cand = sb("cand", [128, 8, 16, 16])
            cidx = sb("cidx", [128, 8, 16, 16])
            best = sb("best", [128, 8, 16])
            pos = sb("pos", [128, 8, 16])
            eid = sb("eid", [128, 8, 16])
            gate = sb("gate", [128, 8, 16])
            gsum = sb("gsum", [128, 8])
            ssq = sb("ssqE", [128, 2])
            NEG = -1e30
            cand2 = sc[:].rearrange("p (h s) n -> p h (s n)", s=2)
            eqb = junk.rearrange("p (h n) -> p h n", h=8)
            for ti in range(16):
                t0 = ti * 128
                cx.dma("sp", h2[:], T["H2"][t0:t0 + 128, :], writes=["h2"])
                cx.op("act", lambda e: e.activation(out=junk, in_=h2[:], func=AF.Square, accum_out=ssq[:, 0:1]),
                      reads=["h2"], writes=[JK, "ssq0"] + [("jk", i_) for i_ in range(8)])
                cx.op("act", lambda e: e.activation(out=ssq[:, 0:1], in_=ssq[:, 0:1], func=AF.Sqrt, scale=1.0 / D,
                                                    bias=epsc[:, 0:1]), reads=["ssq0", "epsc"], writes=["ssq0"])
                cx.op("dve", lambda e: e.reciprocal(out=ssq[:, 0:1], in_=ssq[:, 0:1]), reads=["ssq0"], writes=["ssq0"])
                cx.op("dve", lambda e: e.scalar_tensor_tensor(out=hn[:], in0=h2[:], scalar=ssq[:, 0:1], in1=gf[:],
                                                              op0=ALU.mult, op1=ALU.mult), reads=["h2", "ssq0", "gf"], writes=["hn"])
                for cg in range(4):
                    pb, pbk = PS[cg % 2], ("ps", cg % 2)
                    for ci in range(4):
                        c = cg * 4 + ci
                        cx.op("pe", lambda e: e.transpose(out=pb[:, ci * 128:ci * 128 + 128], in_=hn[:, c * 128:(c + 1) * 128],
                                                          identity=ident[:, :]), reads=["hn", "ident"], writes=[pbk], last=(ci == 3))
                    cx.op("act", lambda e: e.copy(out=hnT[:, cg * 4:cg * 4 + 4, :],
                                                  in_=pb[:, 0:512].rearrange("p (c t) -> p c t", c=4)), reads=[pbk], writes=["hnT"])
                for g in range(16):
                    pb, pbk = PS[2 + g % 2], ("ps", 2 + g % 2)
                    for c in range(16):
                        cx.op("pe", lambda e: e.matmul(out=pb[:, 0:128], lhsT=wq[:, c, g * 128:g * 128 + 128], rhs=hnT[:, c, :],
                                                       start=(c == 0), stop=(c == 15)), reads=["wqp", "hnT"], writes=[pbk], last=(c == 15))
                    cx.op("act", lambda e: e.copy(out=qT[:, g, :], in_=pb[:, 0:128]), reads=[pbk], writes=[("qT", g)])
                for g in range(16):
                    pb, pbk = PS[g % 2], ("ps", g % 2)
                    cx.op("pe", lambda e: e.matmul(out=pb[:, 0:128], lhsT=qT[:, g, :], rhs=keysT[:, g, :], start=True, stop=True),
                          reads=[("qT", g), "keysT"], writes=[pbk])
                    cx.op("act", lambda e: e.copy(out=sc[:, g, :], in_=pb[:, 0:128]), reads=[pbk], writes=[("sc", g)])
                for g in range(16):
                    cx.op("dve", lambda e: e.max(out=tops[:, g, 0:8], in_=sc[:, g, :]), reads=[("sc", g)], writes=[("tops", g)])
                for g in range(16):
                    cx.op("dve", lambda e: e.max_index(out=tiu_all[:, g, 0:8], in_max=tops[:, g, 0:8], in_values=sc[:, g, :]),
                          reads=[("sc", g), ("tops", g)], writes=[("tiu", g)])
                for g in range(16):
                    cx.op("dve", lambda e: e.match_replace(out=sc2_all[:, g, :], in_to_replace=tops[:, g, 0:8], in_values=sc[:, g, :],
                                                           imm_value=NEG), reads=[("sc", g), ("tops", g)], writes=[("sc2", g)])
                for g in range(16):
                    cx.op("dve", lambda e: e.max(out=tops[:, g, 8:16], in_=sc2_all[:, g, :]), reads=[("sc2", g)], writes=[("tops", g)])
                for g in range(16):
                    cx.op("dve", lambda e: e.max_index(out=tiu_all[:, g, 8:16], in_max=tops[:, g, 8:16], in_values=sc2_all[:, g, :]),
                          reads=[("sc2", g), ("tops", g)], writes=[("tiu", g)])
                cx.op("dve", lambda e: e.tensor_copy(out=topi[:], in_=tiu_all[:]), reads=[("tiu", g) for g in range(16)],
                      writes=[("topi", g) for g in range(16)])
                tkeys = [("tops", g) for g in range(16)]
                ikeys = [("topi", g) for g in range(16)]
                ts4 = tops[:].rearrange("p (h s) k -> p h s k", s=2)
                ti4 = topi[:].rearrange("p (h s) k -> p h s k", s=2)
                cx.op("dve", lambda e: e.tensor_tensor(out=cand[:], in0=ts4[:, :, 0, :].unsqueeze(3).to_broadcast([128, 8, 16, 16]),
                                                       in1=ts4[:, :, 1, :].unsqueeze(2).to_broadcast([128, 8, 16, 16]), op=ALU.add),
                      reads=tkeys, writes=["cand"])
                cx.op("dve", lambda e: e.tensor_scalar(out=eid[:], in0=ti4[:, :, 0, :], scalar1=128.0, scalar2=None, op0=ALU.mult),
                      reads=ikeys, writes=["eid"] + [("eid", hh, k) for hh in range(8) for k in range(16)])
                cx.op("dve", lambda e: e.tensor_tensor(out=cidx[:], in0=eid[:].unsqueeze(3).to_broadcast([128, 8, 16, 16]),
                                                       in1=ti4[:, :, 1, :].unsqueeze(2).to_broadcast([128, 8, 16, 16]),
                                                       op=ALU.add), reads=ikeys + ["eid"], writes=["cidx"])
                c3 = cand[:].rearrange("p h a b -> p h (a b)")
                i3 = cidx[:].rearrange("p h a b -> p h (a b)")
                for hh in range(8):
                    cx.op("dve", lambda e: e.max(out=best[:, hh, 0:8], in_=c3[:, hh, :]), reads=["cand"], writes=[("best", hh)])
                for hh in range(8):
                    cx.op("dve", lambda e: e.max_index(out=piu[:, hh, 0:8], in_max=best[:, hh, 0:8], in_values=c3[:, hh, :]),
                          reads=["cand", ("best", hh)], writes=[("piu", hh)])
                for hh in range(8):
                    cx.op("dve", lambda e: e.match_replace(out=cand2[:, hh, :], in_to_replace=best[:, hh, 0:8], in_values=c3[:, hh, :],
                                                           imm_value=NEG), reads=["cand", ("best", hh)],
                          writes=[("sc", 2 * hh), ("sc", 2 * hh + 1)])
                for hh in range(8):
                    cx.op("dve", lambda e: e.max(out=best[:, hh, 8:16], in_=cand2[:, hh, :]),
                          reads=[("sc", 2 * hh), ("sc", 2 * hh + 1)], writes=[("best", hh)])
                for hh in range(8):
                    cx.op("dve", lambda e: e.max_index(out=piu[:, hh, 8:16], in_max=best[:, hh, 8:16], in_values=cand2[:, hh, :]),
                          reads=[("sc", 2 * hh), ("sc", 2 * hh + 1), ("best", hh)], writes=[("piu", hh)])
                cx.op("dve", lambda e: e.tensor_copy(out=pos[:], in_=piu[:]), reads=[("piu", hh) for hh in range(8)],
                      writes=[("pos", hh) for hh in range(8)])
                bkeys = [("best", hh) for hh in range(8)]
                pkeys = [("pos", hh) for hh in range(8)]
                for hh in range(8):
                    for k in range(16):
                        js_ = (hh * 16 + k) % 8
                        cx.op("dve", lambda e: e.scalar_tensor_tensor(out=junk[:, js_ * 256:js_ * 256 + 256], in0=iota[:, :],
                                                                      scalar=pos[:, hh, k:k + 1],
                                                                      in1=i3[:, hh, :], op0=ALU.is_equal, op1=ALU.mult,
                                                                      accum_out=eid[:, hh, k:k + 1]),
                              reads=["iota", "cidx", ("pos", hh)], writes=[("jk", js_), ("eid", hh, k)])
                cx.op("dve", lambda e: e.tensor_copy(out=eidi_all[:, ti, :], in_=eid[:].rearrange("p h k -> p (h k)")),
                      reads=[("eid", hh, k) for hh in range(8) for k in range(16)], writes=[("eidi", ti)])
                cx.op("dve", lambda e: e.tensor_tensor(out=gate[:], in0=best[:], in1=best[:, :, 0:1].to_broadcast([128, 8, 16]),
                                                       op=ALU.subtract), reads=bkeys, writes=["gate"])
                cx.op("act", lambda e: e.activation(out=gate[:], in_=gate[:], func=AF.Exp), reads=["gate"], writes=["gate"])
                cx.op("dve", lambda e: e.tensor_reduce(out=gsum[:], in_=gate[:], axis=AX.X, op=ALU.add), reads=["gate"], writes=["gsum"])
                cx.op("dve", lambda e: e.reciprocal(out=gsum[:], in_=gsum[:]), reads=["gsum"], writes=["gsum"])
                cx.op("dve", lambda e: e.tensor_tensor(out=gate_all[:, ti, :].rearrange("p (h k) -> p h k", h=8), in0=gate[:],
                                                       in1=gsum[:].unsqueeze(2).to_broadcast([128, 8, 16]),
                                                       op=ALU.mult), reads=["gate", "gsum"], writes=[("gate_all", ti)])

        cx.barrier()
        with ExitStack() as es:
            def sb(name, shape, dt=F32):
                return es.enter_context(nc.sbuf_tensor(name, shape, dt))
            NR = 14
            rows = [sb("rows%d" % i, [128, 4096], BF16)[:] for i in range(NR)]
            gfin = sb("gfin", [128, 2048])
            identb = sb("identEb", [128, 128], BF16)
            h2 = sb("h2b", [128, 2048])
            hnb = sb("hnb", [128, 2048], BF16)
            junkb = sb("junkb", [128, 2048], BF16)
            score = sb("score", [128, 128])
            coef = sb("coef", [128, 128])
            ssq = sb("ssqB", [128, 2])
            dg = [sb("dg%d" % i, [128, 128], BF16) for i in range(8)]
            cx.dma("sp", gfin[:], T["gfin_b"], writes=["gfin"])
            cx.op("dve", lambda e: e.tensor_copy(out=identb[:], in_=ident[:]), reads=["ident"], writes=["identb"])
            junk = rows[NR - 1].bitcast(F32)
            JK = ("rows", NR - 1)
            NG = NR - 1
            for ti in range(16):
                t0 = ti * 128
                cx.dma("sp", h2[:], T["H2"][t0:t0 + 128, :], writes=["h2"])
                cx.op("act", lambda e: e.activation(out=junk, in_=h2[:], func=AF.Square, accum_out=ssq[:, 0:1]),
                      reads=["h2"], writes=[JK, "ssq0"])
                cx.op("act", lambda e: e.activation(out=ssq[:, 0:1], in_=ssq[:, 0:1], func=AF.Sqrt, scale=1.0 / D,
                                                    bias=epsc[:, 0:1]), reads=["ssq0", "epsc"], writes=["ssq0"])
                cx.op("dve", lambda e: e.reciprocal(out=ssq[:, 0:1], in_=ssq[:, 0:1]), reads=["ssq0"], writes=["ssq0"])
                cx.op("dve", lambda e: e.scalar_tensor_tensor(out=hnb[:], in0=h2[:], scalar=ssq[:, 0:1], in1=gf[:],
                                                              op0=ALU.mult, op1=ALU.mult), reads=["h2", "ssq0", "gf"], writes=["hnb"])
                for g in range(32):
                    js = list(range(g * 4, g * 4 + 4))
                    for j in js:
                        rb = rows[j % NG]
                        cx.dma("pool", rb, T["UVB"], indirect=bass.IndirectOffsetOnAxis(ap=eidi_all[:, ti, j:j + 1], axis=0),
                               writes=[("rows", j % NG)])
                        cx.op("dve", lambda e: e.scalar_tensor_tensor(out=junkb[:], in0=rb[:, 0:2048], scalar=1.0, in1=hnb[:],
                                                                      op0=ALU.mult, op1=ALU.mult, accum_out=score[:, j:j + 1]),
                              reads=[("rows", j % NG), "hnb"], writes=["junkb", ("score", g)])
                    cx.op("act", lambda e: e.activation(out=coef[:, g * 4:g * 4 + 4], in_=score[:, g * 4:g * 4 + 4], func=AF.Gelu),
                          reads=[("score", g)], writes=[("coef", g)])
                    cx.op("dve", lambda e: e.tensor_tensor(out=coef[:, g * 4:g * 4 + 4], in0=coef[:, g * 4:g * 4 + 4],
                                                           in1=gate_all[:, ti, g * 4:g * 4 + 4], op=ALU.mult),
                          reads=[("coef", g)], writes=[("coef", g)])
                    for j in js:
                        rb = rows[j % NG]
                        d_ = dg[j % 8]
                        cx.op("act", lambda e: e.activation(out=d_[:], in_=identb[:], func=AF.Copy, scale=coef[:, j:j + 1]),
                              reads=[("coef", g), "identb"], writes=[("dg", j % 8)])
                        for nb in range(4):
                            cx.op("pe", lambda e: e.matmul(out=PS[4 + nb][:, 0:512], lhsT=d_[:],
                                                           rhs=rb[:, 2048 + nb * 512:2048 + nb * 512 + 512],
                                                           start=(j == 0), stop=(j == 127)),
                                  reads=[("dg", j % 8), ("rows", j % NG)], writes=[("ps", 4 + nb)], last=(nb == 3))
                for nb in range(4):
                    cx.op("dve", lambda e: e.tensor_tensor(out=h2[:, nb * 512:nb * 512 + 512], in0=h2[:, nb * 512:nb * 512 + 512],
                                                           in1=PS[4 + nb][:, 0:512], op=ALU.add), reads=["h2", ("ps", 4 + nb)], writes=["h2"])
                cx.op("act", lambda e: e.activation(out=junk, in_=h2[:], func=AF.Square, accum_out=ssq[:, 1:2]),
                      reads=["h2"], writes=[JK, "ssq1"])
                cx.op("act", lambda e: e.activation(out=ssq[:, 1:2], in_=ssq[:, 1:2], func=AF.Sqrt, scale=1.0 / D,
                                                    bias=epsc[:, 0:1]), reads=["ssq1", "epsc"], writes=["ssq1"])
                cx.op("dve", lambda e: e.reciprocal(out=ssq[:, 1:2], in_=ssq[:, 1:2]), reads=["ssq1"], writes=["ssq1"])
                cx.op("dve", lambda e: e.scalar_tensor_tensor(out=junk, in0=h2[:], scalar=ssq[:, 1:2], in1=gfin[:],
                                                              op0=ALU.mult, op1=ALU.mult), reads=["h2", "ssq1", "gfin"], writes=[JK])
                cx.dma("sp", T["out"][t0:t0 + 128, :], junk, reads=[JK])
    cx.barrier()


_NC = None


def kernel(**inputs):
    global _NC
    inputs = {k: np.asarray(v) for k, v in inputs.items()}
    if _NC is None:
        _NC = build()
    in_maps = []
    for b in range(4):
        for s_ in range(2):
            m = prep_core(inputs, b, s_)
            in_maps.append({k: np.ascontiguousarray(v, dtype=np.float32) for k, v in m.items()})
    res = run_bass_kernel_spmd(_NC, in_maps, core_ids=list(range(8)))
    out = np.zeros((4, 4096, D), np.float32)
    for b in range(4):
        for s_ in range(2):
            o = np.asarray(res.results[b * 2 + s_]["out"])
            if s_ == 0:
                out[b, 0:2048] = o
            else:
                out[b, 2048:4096] = o[::-1]
    return out
```

```python
import numpy as np
from contextlib import ExitStack
import concourse.bass as bass
import concourse.mybir as mybir
from concourse.bass_utils import run_bass_kernel_spmd

F32 = mybir.dt.float32
BF16 = mybir.dt.bfloat16
I32 = mybir.dt.int32
U32 = mybir.dt.uint32
AF = mybir.ActivationFunctionType
ALU = mybir.AluOpType
AX = mybir.AxisListType

D = 2048
NTOK = 4224
OWN0, OWN1 = 64, 2112
NOWN = 2048
HALF = 2112
NCH = 66
C = 64
RW = 3488
EPS = 1e-6
C0 = float(np.exp(-0.5))

R_R, R_K, R_V, R_GD, R_WDF, R_WDB, R_ADF, R_ADB = 0, 1024, 2048, 3072, 3232, 3296, 3360, 3424
R_QD, R_KVD, R_KR, R_KRR, R_GR, R_GM = 3488, 4000, 4512, 4576, 4640, 6688
NROWS = 8736


class Ctx:
    def __init__(self, nc, es):
        self.nc = nc
        self.E = {"pe": nc.tensor, "dve": nc.vector, "act": nc.scalar, "pool": nc.gpsimd, "sp": nc.sync}
        self.psem = {k: es.enter_context(nc.semaphore("prog_" + k)) for k in ["pe", "dve", "act", "pool"]}
        self.pcnt = {k: 0 for k in self.psem}
        self.seen = {}
        self.buf = {}
        self.dq = {}
        for q in ["sp", "pool", "act"]:
            sems = [es.enter_context(nc.semaphore("dq_%s_%d" % (q, i))) for i in range(20)]
            self.dq[q] = {"sems": sems, "cnt": [0] * len(sems), "i": 0}
        self.ninst = 0
        self.pend = {}

    def wait(self, eng, tok):
        key, sem, val = tok
        k = (eng, key)
        if self.seen.get(k, 0) >= val:
            return
        self.E[eng].wait_ge(sem, val)
        self.seen[k] = val

    def _deps(self, reads, writes):
        deps = []
        for k in reads:
            st = self.buf.get(k)
            if st and st["w"]:
                deps.append(st["w"])
        for k in writes:
            st = self.buf.get(k)
            if st:
                if st["w"]:
                    deps.append(st["w"])
                deps.extend(st["r"].values())
        return deps

    def _commit(self, tok, reads, writes):
        for k in reads:
            st = self.buf.setdefault(k, {"w": None, "r": {}})
            old = st["r"].get(tok[0])
            if old is None or old[2] < tok[2]:
                st["r"][tok[0]] = tok
        for k in writes:
            self.buf[k] = {"w": tok, "r": {}}

    def op(self, eng, fn, reads=(), writes=(), last=True):
        for d in self._deps(reads, writes):
            self.wait(eng, d)
        ins = fn(self.E[eng])
        self.ninst += 1
        pend = self.pend.setdefault(eng, [])
        pend.append((list(reads), list(writes)))
        if not last:
            return None
        self.pcnt[eng] += 1
        ins.then_inc(self.psem[eng], 1)
        tok = (eng, self.psem[eng], self.pcnt[eng])
        for r, w in pend:
            self._commit(tok, r, w)
        del pend[:]
        return tok

    def dma(self, q, out, in_, reads=(), writes=(), indirect=None, slow=False):
        dq = self.dq[q]
        i = dq["i"]
        dq["i"] = (i + 1) % len(dq["sems"])
        sem = dq["sems"][i]
        key = "dq_%s_%d" % (q, i)
        if dq["cnt"][i] > 0:
            self.wait(q, (key, sem, dq["cnt"][i]))
        for d in self._deps(reads, writes):
            self.wait(q, d)
        if indirect is None:
            ins = self.E[q].dma_start(out=out, in_=in_, allow_slow_non_contiguous=slow)
        else:
            ins = self.E[q].indirect_dma_start(out=out, out_offset=None, in_=in_, in_offset=indirect)
        dq["cnt"][i] += 16
        ins.then_inc(sem, 16)
        tok = (key, sem, dq["cnt"][i])
        self._commit(tok, reads, writes)
        self.ninst += 1
        return tok

    def barrier(self):
        toks = [(k, self.psem[k], self.pcnt[k]) for k in self.psem if self.pcnt[k] > 0]
        for q, dq in self.dq.items():
            for i, sem in enumerate(dq["sems"]):
                if dq["cnt"][i] > 0:
                    toks.append(("dq_%s_%d" % (q, i), sem, dq["cnt"][i]))
        for eng in ["pe", "dve", "act", "pool", "sp"]:
            for t in toks:
                self.wait(eng, t)
        self.buf = {}


def stage_inproj(cx, nc, T, PS):
    with ExitStack() as es:
        def sb(name, shape, dt):
            return es.enter_context(nc.sbuf_tensor(name, shape, dt))
        nT = sb("nT", [128, 16, HALF], BF16)
        xt = [sb("xt%d" % i, [128, D], F32) for i in range(2)]
        xs = [sb("xs%d" % i, [128, D], F32) for i in range(2)]
        ssq = sb("ssq", [128, 40], F32)
        rstd = sb("rstd", [128, 40], F32)
        gS = sb("gS", [128, 16], F32)
        ident = sb("identA", [128, 128], F32)
        wb = [sb("wb%d" % i, [128, 16, 256], BF16) for i in range(2)]
        wrot = sb("wrot", [128, 16, 64], BF16)
        stg = [sb("stg%d" % i, [128, 512], F32) for i in range(4)]
        zero = sb("zeroA", [128, 2], F32)

        cx.dma("sp", ident[:], T["ident"], writes=["ident"])
        cx.dma("sp", gS[:], T["norm_mix_g"], writes=["gS"])
        epsc = sb("epsc", [128, 1], F32)
        cx.op("dve", lambda e: e.memset(epsc[:], EPS), writes=["epsc"])
        cx.op("dve", lambda e: e.memset(zero[:], 0.0), writes=["zero"])
        for r0 in range(0, NROWS, 128):
            m = min(128, NROWS - r0)
            cx.dma("sp", T["PRT"][r0:r0 + m, 0:1], zero[0:m, 0:1], reads=["zero"], slow=True)
            cx.dma("sp", T["PRT"][r0:r0 + m, NTOK + 1:NTOK + 2], zero[0:m, 1:2], reads=["zero"], slow=True)

        units = []
        for u0 in range(0, 3072, 256):
            units.append((T["w_in"][:, u0:u0 + 256], 256, [(0, 128, u0, False), (128, 128, u0 + 128, False)], False))
        units.append((T["w_lora"][:, 0:160], 160, [(0, 128, R_GD, False), (128, 32, R_GD + 128, False)], False))
        units.append((T["w_lora"][:, 160:416], 256, [(0, 64, R_WDF, False), (64, 64, R_WDB, False),
                                                      (128, 64, R_ADF, False), (192, 64, R_ADB, False)], False))
        for u0 in range(0, 512, 256):
            units.append((T["w_in"][:, 4000 + u0:4000 + u0 + 256], 256,
                          [(0, 128, R_KVD + u0, False), (128, 128, R_KVD + u0 + 128, False)], False))
        units.append((T["w_in"][:, 4512:4576], 64, [(0, 64, R_KR, False), (0, 64, R_KRR, True)], False))
        for u0 in range(0, 512, 256):
            units.append((T["w_in"][:, 3488 + u0:3488 + u0 + 256], 256,
                          [(0, 128, R_QD + u0, False), (128, 128, R_QD + u0 + 128, False)], True))
        for u0 in range(0, 4096, 256):
            units.append((T["w_in"][:, 4576 + u0:4576 + u0 + 256], 256,
                          [(0, 128, R_GR + u0, False), (128, 128, R_GR + u0 + 128, False)], True))

        tcb = [sb("tcb%d" % i, [128, 2048], BF16) for i in range(4)]

        def tabconv_gen():
            k = 0
            for ti in range(128):
                for tab, src in enumerate(["peer_u", "peer_v"]):
                    b_ = k % 4
                    cx.dma("pool", tcb[b_][:], T[src][ti * 128:(ti + 1) * 128, :], writes=[("tcb", b_)])
                    cx.dma("act", T["UVB"][ti * 128:(ti + 1) * 128, tab * 2048:(tab + 1) * 2048], tcb[b_][:],
                           reads=[("tcb", b_)])
                    k += 1
                    yield

        tcg = tabconv_gen()
        ev = [0]
        for half in range(2):
            tbase = half * HALF
            for ti in range(HALF // 128 + 1):
                t0 = tbase + ti * 128
                n = min(128, tbase + HALF - t0)
                if n <= 0:
                    continue
                b = ti % 2
                tt = half * 17 + ti
                cx.dma("sp", xt[b][0:n, :], T["xp"][t0:t0 + n, :], writes=[("xt", b)])
                cx.op("act", lambda e: e.activation(out=xs[b][0:n, :], in_=xt[b][0:n, :], func=AF.Square,
                                                    accum_out=ssq[0:n, tt:tt + 1]),
                      reads=[("xt", b)], writes=[("xs", b), ("ssq", tt)])
                cx.op("act", lambda e: e.activation(out=ssq[0:n, tt:tt + 1], in_=ssq[0:n, tt:tt + 1], func=AF.Sqrt,
                                                    scale=1.0 / D, bias=epsc[0:n, 0:1]),
                      reads=[("ssq", tt), "epsc"], writes=[("ssq", tt)])
                cx.op("dve", lambda e: e.reciprocal(out=rstd[0:n, tt:tt + 1], in_=ssq[0:n, tt:tt + 1]),
                      reads=[("ssq", tt)], writes=[("rstd", tt)])
                cx.op("act", lambda e: e.activation(out=xs[b][0:n, :], in_=xt[b][0:n, :], func=AF.Copy,
                                                    scale=rstd[0:n, tt:tt + 1]),
                      reads=[("xt", b), ("rstd", tt)], writes=[("xs", b)])
                for cg in range(4):
                    pb = PS[(tt * 4 + cg) % 2]
                    for ci in range(4):
                        c = cg * 4 + ci
                        cx.op("pe", lambda e: e.transpose(out=pb[:, ci * 128:ci * 128 + n],
                                                          in_=xs[b][0:n, c * 128:(c + 1) * 128], identity=ident[0:n, 0:n]),
                              reads=[("xs", b), "ident"], writes=[("ps", (tt * 4 + cg) % 2, ci)], last=(ci == 3))
                    tl = t0 - tbase
                    cx.op("dve", lambda e: e.tensor_tensor(
                        out=nT[:, cg * 4:cg * 4 + 4, tl:tl + n],
                        in0=pb[:, 0:512].rearrange("p (c t) -> p c t", c=4)[:, :, 0:n],
                        in1=gS[:, cg * 4:cg * 4 + 4].unsqueeze(2).to_broadcast([128, 4, n]), op=ALU.mult),
                        reads=[("ps", (tt * 4 + cg) % 2, i) for i in range(4)] + ["gS"],
                        writes=[("nT", ti)])
            if half == 0:
                blocks = [(0, 64, False)] + [(64 + 512 * i, 512, True) for i in range(4)]
            else:
                blocks = [(HALF + 512 * i, 512, False) for i in range(4)] + [(4160, 64, False)]
            nTreads = [("nT", i) for i in range(17)]
            active = [u for u in units if not (u[3] and half == 1)]

            def issue_load(idx):
                src_, U_, segs_, _ = active[idx]
                wb_ = wb[idx % 2]
                cx.dma("pool", wb_[:, :, 0:U_], src_.rearrange("(c p) u -> p c u", p=128), writes=[("wb", idx % 2)])
                if any(s_[3] for s_ in segs_):
                    cx.op("pool", lambda e: e.tensor_scalar(out=wrot[:, :, 0:32], in0=wb_[:, :, 32:64], scalar1=-1.0,
                                                            scalar2=None, op0=ALU.mult),
                          reads=[("wb", idx % 2)], writes=["wrot"])
                    cx.op("pool", lambda e: e.tensor_copy(out=wrot[:, :, 32:64], in_=wb_[:, :, 0:32]),
                          reads=[("wb", idx % 2)], writes=["wrot"])

            issue_load(0)
            for ui, (src, U, segs, own_only) in enumerate(active):
                if ui + 1 < len(active):
                    issue_load(ui + 1)
                wbuf = ui % 2
                for _ in range(5):
                    next(tcg, None)
                for (off, M, drow, rot) in segs:
                    for (t0, n, isown) in blocks:
                        if own_only and not isown:
                            continue
                        k = ev[0]
                        ev[0] += 1
                        pbk = 2 + (k % 4)
                        pb = PS[pbk]
                        tl = t0 - tbase
                        for c in range(16):
                            lhsT = wrot[:, c, 0:64] if rot else wb[wbuf][:, c, off:off + M]
                            cx.op("pe", lambda e: e.matmul(out=pb[0:M, 0:n], lhsT=lhsT, rhs=nT[:, c, tl:tl + n],
                                                           start=(c == 0), stop=(c == 15)),
                                  reads=[("wb", wbuf), "wrot"] + (nTreads if c == 0 else []),
                                  writes=[("psb", pbk)], last=(c == 15))
                        sg = stg[k % 4]
                        if k % 2 == 0:
                            cx.op("act", lambda e: e.copy(out=sg[0:M, 0:n], in_=pb[0:M, 0:n]),
                                  reads=[("psb", pbk)], writes=[("stg", k % 4)])
                        else:
                            cx.op("dve", lambda e: e.tensor_copy(out=sg[0:M, 0:n], in_=pb[0:M, 0:n]),
                                  reads=[("psb", pbk)], writes=[("stg", k % 4)])
                        cx.dma("sp", T["PRT"][drow:drow + M, 1 + t0:1 + t0 + n], sg[0:M, 0:n],
                               reads=[("stg", k % 4)])
        for _ in tcg:
            pass
    cx.barrier()


IN_SPECS = [
    ("xp", [NTOK, D]), ("ident", [128, 128]),
    ("norm_mix_g", [128, 16]), ("w_in", [D, 8672]), ("w_lora", [D, 416]), ("shiftc_fm", [128, 28, 3]), ("ones64", [64, 64]), ("rmask", [64, 16, 64]),
    ("maskG0", [128, 128]), ("maskG1", [128, 128]), ("maskN0", [64, 64]), ("maskN1", [64, 64]),
    ("wup0", [65, 1024]), ("wup1", [65, 1024]), ("aup0", [65, 1024]), ("aup1", [65, 1024]),
    ("kk_fm", [64, 16]), ("ka_fm", [64, 16]), ("rk_fm", [64, 16]),
    ("lng_b", [128, 1024]), ("lnb_b", [128, 1024]), ("g_up", [160, 1024]), ("invf", [64, 1]),
    ("gq_fm", [128, 4]), ("gkv_fm", [128, 4]), ("valid_tm", [128, 33]), ("pos64", [64, NTOK]),
    ("w_uq", [512, 3072]), ("w_ukv", [512, 4096]), ("p_rwkv", [1024, 2048]), ("p_mla", [2048, 2048]),
    ("w_o", [2048, 2048]), ("bgate_fm", [128, 32]), ("peer_wq", [2048, 2048]), ("peer_keys", [16, 128, 128]),
    ("peer_u", [16384, 2048]), ("peer_v", [16384, 2048]), ("gffn_b", [128, 2048]), ("gfin_b", [128, 2048]), ("iota256", [128, 256]),
]


def build(upto="all", debug_out=()):
    nc = bass.Bass("TRN2", target_bir_lowering=False)
    T = {}
    for name, shape in IN_SPECS:
        T[name] = nc.dram_tensor(name, shape, F32, kind="ExternalInput").ap()
    def scratch(name, shape, dt=F32):
        kind = "ExternalOutput" if name in debug_out else "Internal"
        T[name] = nc.dram_tensor(name, shape, dt, kind=kind).ap()
    scratch("PRT", [NROWS, NTOK + 2])
    scratch("XSQ", [3, NCH, 64, 16, 64])
    scratch("XSL", [416, NTOK])
    scratch("YF", [NOWN, 1040])
    scratch("YB", [NOWN, 1040])
    scratch("VTM", [NOWN, 1024])
    scratch("YRT", [1024, NOWN], BF16)
    scratch("YMT", [2048, NOWN], BF16)
    scratch("MGT", [2048, NOWN], BF16)
    scratch("H2", [NOWN, D])
    scratch("UVB", [16384, 4096], BF16)
    T["out"] = nc.dram_tensor("out", [NOWN, D], F32, kind="ExternalOutput").ap()
    with ExitStack() as es:
        PQ = [es.enter_context(nc.psum_tensor("pq%d" % i, [128, 1024], F32)) for i in range(4)]
        PS = [PQ[i // 2][:, (i % 2) * 512:(i % 2) * 512 + 512] for i in range(8)]
        cx = Ctx(nc, es)
        stage_inproj(cx, nc, T, PS)
        if upto == "inproj":
            print("instructions:", cx.ninst)
            return nc
        stage_shift(cx, nc, T)
        stage_rwkv(cx, nc, T, PQ, PS)
        if upto == "rwkv":
            print("instructions:", cx.ninst)
            return nc
        stage_post(cx, nc, T, PQ)
        stage_mla(cx, nc, T, PQ, PS)
        if upto == "mla":
            print("instructions:", cx.ninst)
            return nc
        stage_merge(cx, nc, T, PQ, PS)
        stage_peer(cx, nc, T, PQ, PS)
        print("instructions:", cx.ninst)
    return nc


def prep_core(inputs, b, s):
    x = inputs["x"]
    meta = inputs["meta_tokens"]
    xp = np.zeros((NTOK, D), np.float32)
    posv = np.zeros((1, NTOK), np.float32)
    valid = np.zeros((1, NTOK), np.float32)
    if s == 0:
        xp[48:64] = meta
        xp[64:64 + 4096] = x[b]
        posv[0, 48:64] = np.arange(16)
        posv[0, 64:64 + 4096] = 16 + np.arange(4096)
        valid[0, 48:64 + 4096] = 1
    else:
        xp[64:64 + 4096] = x[b, ::-1]
        xp[4160:4176] = meta[::-1]
        posv[0, 64:64 + 4096] = 16 + np.arange(4095, -1, -1)
        posv[0, 4160:4176] = np.arange(15, -1, -1)
        valid[0, 64:4176] = 1
    w_in = inputs["w_in"][0]
    sc = inputs["shift_c"][0]
    lo = R_GD
    if s == 0:
        order = [(R_GD, 160), (R_WDF, 64), (R_WDB, 64), (R_ADF, 64), (R_ADB, 64)]
    else:
        order = [(R_GD, 160), (R_WDB, 64), (R_WDF, 64), (R_ADB, 64), (R_ADF, 64)]
    cols = np.concatenate([np.arange(a, a + n) for a, n in order])
    w_lora = np.ascontiguousarray(w_in[:, cols])
    shiftc = sc.copy()
    shiftc[:, lo:RW] = sc[:, cols]
    if s == 1:
        shiftc = shiftc[::-1]
    m = {
        "xp": xp, "posv": posv, "valid": valid, "ident": np.eye(128, dtype=np.float32),
        "norm_mix_g": np.ascontiguousarray(inputs["norm_mix_g"][0].reshape(16, 128).T), "w_in": w_in, "w_lora": w_lora,
    }
    scp = np.zeros((3, 28 * 128), np.float32)
    scp[:, :RW] = shiftc
    m["shiftc_fm"] = np.ascontiguousarray(scp.reshape(3, 28, 128).transpose(2, 1, 0))
    m["ones64"] = np.ones((64, 64), np.float32)
    rm = np.ones((64, 16, 64), np.float32)
    rm[:, :, 0] = 0
    m["rmask"] = rm
    tt = np.arange(64)
    for di in range(2):
        if di == 0:
            strict = (tt[:, None] < tt[None, :]); incl = (tt[:, None] <= tt[None, :])
        else:
            strict = (tt[:, None] > tt[None, :]); incl = (tt[:, None] >= tt[None, :])
        m["maskG%d" % di] = np.block([[strict, incl], [strict, incl]]).astype(np.float32)
        m["maskN%d" % di] = np.ascontiguousarray(strict.T).astype(np.float32)
        d = di if s == 0 else 1 - di
        m["wup%d" % di] = np.concatenate([inputs["w_up"][0, d], inputs["w0"][0, d][None]], 0)
        m["aup%d" % di] = np.concatenate([inputs["a_up"][0, d], inputs["a0"][0, d][None]], 0)
    m["kk_fm"] = np.ascontiguousarray(inputs["k_k"][0].reshape(16, 64).T)
    m["ka_fm"] = np.ascontiguousarray(inputs["k_a"][0].reshape(16, 64).T)
    m["rk_fm"] = np.ascontiguousarray(inputs["r_k"][0].T)
    rep = lambda v, n: np.ascontiguousarray(np.broadcast_to(v[None, :], (n, v.shape[0])))
    m["lng_b"] = rep(inputs["ln_x_g"][0], 128)
    m["lnb_b"] = rep(inputs["ln_x_b"][0], 128)
    m["g_up"] = inputs["g_up"][0]
    invf = (10000.0 ** (-np.arange(0, 64, 2, dtype=np.float32) / 64)).astype(np.float32)
    m["invf"] = np.concatenate([invf, invf])[:, None]
    m["gq_fm"] = np.ascontiguousarray(inputs["q_norm_g"][0].reshape(4, 128).T)
    m["gkv_fm"] = np.ascontiguousarray(inputs["kv_norm_g"][0].reshape(4, 128).T)
    m["valid_tm"] = np.ascontiguousarray(valid[0].reshape(33, 128).T)
    m["pos64"] = rep(posv[0], 64)
    for k in ["w_uq", "w_ukv", "p_rwkv", "p_mla", "w_o", "peer_wq", "peer_u", "peer_v"]:
        m[k] = inputs[k][0]
    m["bgate_fm"] = np.ascontiguousarray(inputs["b_gate"][0].reshape(32, 128).T)
    m["peer_keys"] = inputs["peer_keys"][0].reshape(16, 128, 128)
    m["gffn_b"] = rep(inputs["norm_ffn_g"][0], 128)
    m["gfin_b"] = rep(inputs["final_norm_g"], 128)
    m["iota256"] = np.ascontiguousarray(np.broadcast_to(np.arange(256, dtype=np.float32)[None, :], (128, 256)))
    del m["posv"], m["valid"]
    return m


def stage_shift(cx, nc, T):
    with ExitStack() as es:
        def sb(name, shape, dt):
            return es.enter_context(nc.sbuf_tensor(name, shape, dt))
        sc = sb("shc", [128, 28, 3], F32)
        raw = [sb("shraw%d" % i, [128, 514], F32) for i in range(4)]
        xo = [sb("shxo%d" % i, [128, 512], F32) for i in range(4)]
        cx.dma("sp", sc[:], T["shiftc_fm"], writes=["sc"])
        k = 0
        for rt in range(28):
            r0 = rt * 128
            m = min(128, RW - r0)
            for t0 in range(0, NTOK, 512):
                n = min(512, NTOK - t0)
                b = k % 4
                k += 1
                cx.dma("sp", raw[b][0:m, 0:n + 2], T["PRT"][r0:r0 + m, t0:t0 + n + 2], writes=[("raw", b)])
                cx.op("act", lambda e: e.activation(out=xo[b][0:m, 0:n], in_=raw[b][0:m, 1:n + 1], func=AF.Copy,
                                                    scale=sc[0:m, rt, 1:2]),
                      reads=[("raw", b), "sc"], writes=[("xo", b)])
                cx.op("dve", lambda e: e.scalar_tensor_tensor(out=xo[b][0:m, 0:n], in0=raw[b][0:m, 0:n],
                                                              scalar=sc[0:m, rt, 0:1], in1=xo[b][0:m, 0:n],
                                                              op0=ALU.mult, op1=ALU.add),
                      reads=[("raw", b), ("xo", b), "sc"], writes=[("xo", b)])
                cx.op("dve", lambda e: e.scalar_tensor_tensor(out=xo[b][0:m, 0:n], in0=raw[b][0:m, 2:n + 2],
                                                              scalar=sc[0:m, rt, 2:3], in1=xo[b][0:m, 0:n],
                                                              op0=ALU.mult, op1=ALU.add),
                      reads=[("raw", b), ("xo", b), "sc"], writes=[("xo", b)])
                if rt < 24:
                    q, hp = rt // 8, rt % 8
                    c0 = t0 // 64
                    ncs = n // 64
                    for hh in range(2):
                        h = 2 * hp + hh
                        cx.dma("pool", T["XSQ"][q, c0:c0 + ncs, :, h, :].rearrange("c j t -> j c t"),
                               xo[b][hh * 64:hh * 64 + 64, 0:n].rearrange("j (c t) -> j c t", t=64),
                               reads=[("xo", b)])
                else:
                    cx.dma("pool", T["XSL"][r0 - 3072:r0 - 3072 + m, t0:t0 + n], xo[b][0:m, 0:n], reads=[("xo", b)])
    cx.barrier()


def stage_rwkv(cx, nc, T, PQ, PS):
    F32R = mybir.dt.float32r
    HG = 4
    with ExitStack() as es:
        def sb(name, shape, dt=F32):
            return es.enter_context(nc.sbuf_tensor(name, shape, dt))
        identf = sb("identRf", [128, 128])
        ident = sb("identR", [128, 128], F32R)
        ones64 = sb("ones64s", [64, 64], F32R)
        rmask = sb("rmasks", [64, HG, 64])
        maskG = [sb("maskGs%d" % i, [128, 128]) for i in range(2)]
        maskN = [sb("maskNs%d" % i, [64, 64]) for i in range(2)]
        wup = [sb("wups%d" % i, [65, 1024], F32R) for i in range(2)]
        aup = [sb("aups%d" % i, [65, 1024], F32R) for i in range(2)]
        kkp = sb("kkp", [64, 16])
        kap = sb("kap", [64, 16])
        omka = sb("omka", [64, 16])
        rkp = sb("rkp", [64, 16])
        wstg = sb("wstg", [65, 1024])
        cx.dma("sp", identf[:], T["ident"], writes=["identf"])
        cx.op("dve", lambda e: e.tensor_copy(out=ident[:], in_=identf[:]), reads=["identf"], writes=["ident"])
        onesf = sb("onesf", [65, 64])
        cx.op("dve", lambda e: e.memset(onesf[:], 1.0), writes=["onesf"])
        cx.op("dve", lambda e: e.tensor_copy(out=ones64[:], in_=onesf[0:64, :]), reads=["onesf"], writes=["ones64"])
        cx.dma("sp", rmask[:], T["rmask"][:, 0:HG, :], writes=["rmask"])
        for i in range(2):
            cx.dma("sp", maskG[i][:], T["maskG%d" % i], writes=["maskG%d" % i])
            cx.dma("sp", maskN[i][:], T["maskN%d" % i], writes=["maskN%d" % i])
            for (dst, src, nm) in [(wup[i], "wup%d" % i, "wup%d" % i), (aup[i], "aup%d" % i, "aup%d" % i)]:
                cx.dma("sp", wstg[:], T[src], writes=["wstg"])
                cx.op("dve", lambda e: e.tensor_copy(out=dst[:], in_=wstg[:]), reads=["wstg"], writes=[nm])
        cx.dma("sp", kkp[:], T["kk_fm"], writes=["kkp"])
        cx.dma("sp", kap[:], T["ka_fm"], writes=["kap"])
        cx.dma("sp", rkp[:], T["rk_fm"], writes=["rkp"])
        cx.op("dve", lambda e: e.tensor_scalar(out=omka[:], in0=kap[:], scalar1=-1.0, scalar2=1.0, op0=ALU.mult,
                                               op1=ALU.add), reads=["kap"], writes=["omka"])

        bank = [0]

        def newbank():
            i = bank[0] % 8
            bank[0] += 1
            return PS[i], ("ps", i)

        def make_chain(cid):
            h0 = cid * HG
            A = {}
            for n in ["rT", "kT", "sg", "ic", "PF", "Pex", "Pin", "ea", "er", "ei", "em", "kx", "t1", "kk", "kt", "kki",
                      "Q0", "YO", "ST"]:
                A[n] = sb("c%d_%s" % (cid, n), [64, HG, 64])
            for n in ["t2", "Nk0", "Nk1", "Mk0", "Mk1", "TT0", "TT1", "X", "STr"]:
                A[n] = sb("c%d_%s" % (cid, n), [64, HG, 64], F32R)
            A["vpad"] = sb("c%d_vpad" % cid, [64, HG, 128])
            A["LBp"] = sb("c%d_LBp" % cid, [64, HG, 128])
            A["LB"] = sb("c%d_LB" % cid, [64, HG, 128], F32R)
            A["RA"] = sb("c%d_RA" % cid, [64, HG, 128], F32R)
            A["GM"] = sb("c%d_GM" % cid, [128, HG, 128], F32R)
            A["UV"] = sb("c%d_UV" % cid, [128, HG, 64], F32R)
            A["BK"] = sb("c%d_BK" % cid, [128, HG, 64], F32R)
            A["wdraw"] = sb("c%d_wdraw" % cid, [64, 64])
            A["adraw"] = sb("c%d_adraw" % cid, [64, 64])
            A["wd"] = sb("c%d_wd" % cid, [65, 64], F32R)
            A["ad"] = sb("c%d_ad" % cid, [65, 64], F32R)
            A["tot"] = sb("c%d_tot" % cid, [64, HG])
            A["etot"] = sb("c%d_etot" % cid, [64, HG])
            A["rkr"] = sb("c%d_rkr" % cid, [64, HG])
            return A

        chains = [make_chain(cid) for cid in range(16 // HG)]

        def run_chain(cid, di, chunks, ychunks, ydst):
            A = chains[cid]
            h0 = cid * HG

            def K(*names):
                return [(cid, n) for n in names]

            def bc(p):
                return p[:, h0:h0 + HG].unsqueeze(2).to_broadcast([64, HG, 64])

            def bcl(p):
                return p[:].unsqueeze(2).to_broadcast([64, HG, 64])

            def tt(eng, out, a, b, op, reads, writes):
                return cx.op(eng, lambda e: e.tensor_tensor(out=out, in0=a, in1=b, op=op), reads=reads, writes=writes)

            def act(out, in_, func, reads, writes, **kw):
                return cx.op("act", lambda e: e.activation(out=out, in_=in_, func=func, **kw), reads=reads, writes=writes)

            def hmm(pb, pk, lhsf, rhsf, reads, w=64):
                for h in range(HG):
                    cx.op("pe", lambda e: e.matmul(out=pb[0:64, h * w:h * w + w], lhsT=lhsf(h), rhs=rhsf(h), start=True,
                                                   stop=True), reads=reads, writes=[pk], last=(h == HG - 1))

            def p3(pb, rows=64, w=64):
                return pb[0:rows, 0:HG * w].rearrange("p (h t) -> p h t", h=HG)

            def loads(c):
                t0 = c * 64
                cx.dma("sp", A["rT"][:], T["XSQ"][0, c, :, h0:h0 + HG, :], writes=K("rT"))
                cx.dma("sp", A["kT"][:], T["XSQ"][1, c, :, h0:h0 + HG, :], writes=K("kT"))
                cx.dma("sp", A["vpad"][:, :, 64:128], T["XSQ"][2, c, :, h0:h0 + HG, :], writes=K("vpad"))
                lw0 = (R_WDF if di == 0 else R_WDB) - 3072
                la0 = (R_ADF if di == 0 else R_ADB) - 3072
                cx.dma("sp", A["wdraw"][:], T["XSL"][lw0:lw0 + 64, t0:t0 + 64], writes=K("wdraw"))
                cx.dma("sp", A["adraw"][:], T["XSL"][la0:la0 + 64, t0:t0 + 64], writes=K("adraw"))

            cx.op("dve", lambda e: e.memset(A["ST"][:], 0.0), writes=K("ST"))
            cx.op("dve", lambda e: e.tensor_copy(out=A["STr"][:], in_=A["ST"][:]), reads=K("ST"), writes=K("STr"))
            cx.op("dve", lambda e: e.tensor_copy(out=A["wd"][64:65, :], in_=onesf[64:65, :]), reads=["onesf"], writes=K("wd1"))
            cx.op("dve", lambda e: e.tensor_copy(out=A["ad"][64:65, :], in_=onesf[64:65, :]), reads=["onesf"], writes=K("ad1"))
            cx.op("dve", lambda e: e.memset(A["vpad"][:], 0.0), writes=K("vpad"))
            loads(chunks[0])
            yield
            for ci, c in enumerate(chunks):
                t0 = c * 64
                want_y = c in ychunks
                act(A["wd"][0:64, :], A["wdraw"][:], AF.Tanh, K("wdraw"), K("wd"))
                act(A["ad"][0:64, :], A["adraw"][:], AF.Copy, K("adraw"), K("ad"))
                pb, pk = newbank()
                hmm(pb, pk, lambda h: wup[di][0:65, (h0 + h) * 64:(h0 + h) * 64 + 64], lambda h: A["wd"][0:65, :],
                    K("wd", "wd1") + ["wup%d" % di])
                act(A["sg"][:], p3(pb), AF.Sigmoid, [pk], K("sg"))
                pb, pk = newbank()
                hmm(pb, pk, lambda h: aup[di][0:65, (h0 + h) * 64:(h0 + h) * 64 + 64], lambda h: A["ad"][0:65, :],
                    K("ad", "ad1") + ["aup%d" % di])
                act(A["ic"][:], p3(pb), AF.Sigmoid, [pk], K("ic"))
                yield
                cx.op("dve", lambda e: e.tensor_tensor_scan(out=A["PF"][:].rearrange("p h t -> p (h t)"),
                                                            data0=rmask[:].rearrange("p h t -> p (h t)"),
                                                            data1=A["sg"][:].rearrange("p h t -> p (h t)"),
                                                            initial=0.0, op0=ALU.mult, op1=ALU.add),
                      reads=["rmask"] + K("sg"), writes=K("PF"))
                cx.op("dve", lambda e: e.tensor_copy(out=A["tot"][:], in_=A["PF"][:, :, 63]), reads=K("PF"), writes=K("tot"))
                if di == 0:
                    tt("dve", A["Pex"][:], A["PF"][:], A["sg"][:], ALU.subtract, K("PF", "sg"), K("Pex"))
                    pin = "PF"
                else:
                    tt("dve", A["Pex"][:], bcl(A["tot"]), A["PF"][:], ALU.subtract, K("PF", "tot"), K("Pex"))
                    tt("dve", A["Pin"][:], A["Pex"][:], A["sg"][:], ALU.add, K("Pex", "sg"), K("Pin"))
                    pin = "Pin"
                tt("pool", A["kx"][:], A["kT"][:], bc(kkp), ALU.mult, K("kT") + ["kkp"], K("kx"))
                tt("pool", A["t2"][:], A["kx"][:], A["kx"][:], ALU.mult, K("kx"), K("t2"))
                yield
                act(A["ea"][:], A["Pex"][:], AF.Exp, K("Pex"), K("ea"), scale=-C0)
                act(A["er"][:], A[pin][:], AF.Exp, K(pin), K("er"), scale=-C0)
                act(A["ei"][:], A[pin][:], AF.Exp, K(pin), K("ei"), scale=C0)
                act(A["etot"][:], A["tot"][:], AF.Exp, K("tot"), K("etot"), scale=-C0)
                pb, pk = newbank()
                cx.op("pe", lambda e: e.matmul(out=pb[0:64, 0:HG * 64], lhsT=ones64[:, :], rhs=A["t2"][:, :, :], start=True,
                                               stop=True), reads=K("t2") + ["ones64"], writes=[pk])
                act(A["kk"][:], p3(pb), AF.Sqrt, [pk], K("kk"))
                tt("pool", A["t1"][:], A["ic"][:], bc(kap), ALU.mult, K("ic") + ["kap"], K("t1"))
                tt("pool", A["t1"][:], A["t1"][:], bc(omka), ALU.add, K("t1") + ["omka"], K("t1"))
                tt("pool", A["kt"][:], A["kT"][:], A["t1"][:], ALU.mult, K("kT", "t1"), K("kt"))
                yield
                cx.op("dve", lambda e: e.tensor_scalar(out=A["kk"][:], in0=A["kk"][:], scalar1=1e-12, scalar2=None,
                                                       op0=ALU.max), reads=K("kk"), writes=K("kk"))
                cx.op("dve", lambda e: e.reciprocal(out=A["kk"][:], in_=A["kk"][:]), reads=K("kk"), writes=K("kk"))
                tt("dve", A["kk"][:], A["kk"][:], A["kx"][:], ALU.mult, K("kk", "kx"), K("kk"))
                tt("pool", A["kki"][:], A["kk"][:], A["ic"][:], ALU.mult, K("kk", "ic"), K("kki"))
                LB4 = A["LB"][:].rearrange("p h (s t) -> p h s t", s=2)
                RA4 = A["RA"][:].rearrange("p h (s t) -> p h s t", s=2)
                LP4 = A["LBp"][:].rearrange("p h (s t) -> p h s t", s=2)
                tt("pool", LB4[:, :, 0, :], A["kki"][:], A["ei"][:], ALU.mult, K("kki", "ei"), K("LB"))
                tt("pool", LB4[:, :, 1, :], A["kt"][:], A["ei"][:], ALU.mult, K("kt", "ei", "LB"), K("LB"))
                cx.op("dve", lambda e: e.scalar_tensor_tensor(out=RA4[:, :, 0, :], in0=A["kk"][:], scalar=-1.0,
                                                              in1=A["ea"][:], op0=ALU.mult, op1=ALU.mult),
                      reads=K("kk", "ea"), writes=K("RA"))
                tt("dve", RA4[:, :, 1, :], A["rT"][:], A["er"][:], ALU.mult, K("rT", "er", "RA"), K("RA"))
                tt("pool", A["LBp"][:], A["LB"][:], A["etot"][:].unsqueeze(2).to_broadcast([64, HG, 128]), ALU.mult,
                   K("LB", "etot"), K("LBp"))
                if want_y:
                    tt("pool", A["t1"][:], A["rT"][:], A["kt"][:], ALU.mult, K("rT", "kt"), K("t1"))
                    tt("pool", A["t2"][:], A["t1"][:], bc(rkp), ALU.mult, K("t1") + ["rkp"], K("t2"))
                yield
                if want_y:
                    pb, pk = newbank()
                    hmm(pb, pk, lambda h: A["t2"][:, h, :], lambda h: ones64[:, 0:2], K("t2") + ["ones64"], w=2)
                    cx.op("act", lambda e: e.copy(out=A["rkr"][:], in_=pb[0:64, 0:2 * HG].rearrange("p (h t) -> p h t", t=2)[:, :, 0]),
                          reads=[pk], writes=K("rkr"))
                pb, pk = newbank()
                for h in range(HG):
                    cx.op("pe", lambda e: e.matmul(out=pb[:, h * 128:h * 128 + 128], lhsT=A["LB"][:, h, :], rhs=A["RA"][:, h, :],
                                                   start=True, stop=True), reads=K("LB", "RA"), writes=[pk], last=(h == HG - 1))
                tt("dve", A["GM"][:], pb[:, 0:HG * 128].rearrange("p (h t) -> p h t", h=HG),
                   maskG[di][:].unsqueeze(1).to_broadcast([128, HG, 128]), ALU.mult, [pk, "maskG%d" % di], K("GM"))
                pb, pk = newbank()
                hmm(pb, pk, lambda h: A["RA"][:, h, 0:64], lambda h: A["LB"][:, h, 0:64], K("RA", "LB"))
                tt("dve", A["Nk0"][:], p3(pb), maskN[di][:].unsqueeze(1).to_broadcast([64, HG, 64]), ALU.mult,
                   [pk, "maskN%d" % di], K("Nk0"))
                yield
                cx.op("act", lambda e: e.copy(out=A["Mk0"][:], in_=A["GM"][0:64, :, 0:64]), reads=K("GM"), writes=K("Mk0"))
                tt("pool", A["TT0"][:], A["GM"][0:64, :, 0:64], identf[0:64, 0:64].unsqueeze(1).to_broadcast([64, HG, 64]),
                   ALU.add, K("GM") + ["identf"], K("TT0"))
                pb, pk = newbank()
                for h in range(HG):
                    cx.op("pe", lambda e: e.transpose(out=pb[:, h * 64:h * 64 + 64], in_=A["vpad"][:, h, :],
                                                      identity=identf[0:64, 0:64]), reads=K("vpad") + ["identf"], writes=[pk], last=(h == HG - 1))
                cx.op("act", lambda e: e.copy(out=A["UV"][64:128, :, :], in_=pb[64:128, 0:HG * 64].rearrange("p (h t) -> p h t", h=HG)),
                      reads=[pk], writes=K("UVv"))
                pb, pk = newbank()
                for h in range(HG):
                    cx.op("pe", lambda e: e.transpose(out=pb[:, h * 64:h * 64 + 64], in_=A["LBp"][:, h, :],
                                                      identity=identf[0:64, 0:64]), reads=K("LBp") + ["identf"], writes=[pk], last=(h == HG - 1))
                cx.op("act", lambda e: e.copy(out=A["BK"][:], in_=pb[:, 0:HG * 64].rearrange("p (h t) -> p h t", h=HG)),
                      reads=[pk], writes=K("BK"))
                if ci + 1 < len(chunks):
                    loads(chunks[ci + 1])
                yield
                cur = 0
                for lvl in range(5):
                    nxt = 1 - cur
                    Nc, Mc, Nn, Mn = "Nk%d" % cur, "Mk%d" % cur, "Nk%d" % nxt, "Mk%d" % nxt
                    pb, pk = newbank()
                    hmm(pb, pk, lambda h: A[Mc][:, h, :], lambda h: A[Nc][:, h, :], K(Mc, Nc))
                    cx.op("act", lambda e: e.copy(out=A[Nn][:], in_=p3(pb)), reads=[pk], writes=K(Nn))
                    if lvl < 4:
                        pb, pk = newbank()
                        hmm(pb, pk, lambda h: A[Nc][:, h, :], lambda h: A[Mc][:, h, :], K(Mc, Nc))
                        cx.op("act", lambda e: e.copy(out=A[Mn][:], in_=p3(pb)), reads=[pk], writes=K(Mn))
                    yield
                    Tc, Tn = "TT%d" % cur, "TT%d" % nxt
                    pb, pk = newbank()
                    hmm(pb, pk, lambda h: A[Nn][:, h, :], lambda h: A[Tc][:, h, :], K(Nn, Tc))
                    tt("dve", A[Tn][:], p3(pb), A[Tc][:], ALU.add, [pk] + K(Tc), K(Tn))
                    cur = nxt
                    yield
                TTf = "TT%d" % cur
                pb, pk = newbank()
                hmm(pb, pk, lambda h: A["GM"][64:128, h, 0:64], lambda h: A["UV"][64:128, h, :], K("GM", "UVv"))
                cx.op("act", lambda e: e.copy(out=A["Q0"][:], in_=p3(pb)), reads=[pk], writes=K("Q0"))
                yield
                pb, pk = newbank()
                hmm(pb, pk, lambda h: A["RA"][:, h, 0:64], lambda h: A["STr"][:, h, :], K("RA", "STr"))
                tt("dve", A["X"][:], p3(pb), A["Q0"][:], ALU.add, [pk] + K("Q0"), K("X"))
                yield
                pb, pk = newbank()
                hmm(pb, pk, lambda h: A[TTf][:, h, :], lambda h: A["X"][:, h, :], K(TTf, "X"))
                cx.op("act", lambda e: e.copy(out=A["UV"][0:64, :, :], in_=p3(pb)), reads=[pk], writes=K("UVu"))
                yield
                if want_y:
                    pb, pk = newbank()
                    for h in range(HG):
                        cx.op("pe", lambda e: e.matmul(out=pb[0:64, h * 64:h * 64 + 64], lhsT=A["RA"][:, h, 64:128],
                                                       rhs=A["STr"][:, h, :], start=True, stop=False),
                              reads=K("RA", "STr"), writes=[pk], last=False)
                        cx.op("pe", lambda e: e.matmul(out=pb[0:64, h * 64:h * 64 + 64], lhsT=A["GM"][:, h, 64:128],
                                                       rhs=A["UV"][:, h, :], start=False, stop=True),
                              reads=K("GM", "UVu", "UVv"), writes=[pk], last=(h == HG - 1))
                    cx.op("act", lambda e: e.copy(out=A["YO"][:], in_=p3(pb)), reads=[pk], writes=K("YO"))
                    cx.dma("act", T[ydst][t0 - 64:t0, h0 * 64:(h0 + HG) * 64], A["YO"][:].rearrange("p h t -> p (h t)"),
                           reads=K("YO"))
                    cx.dma("act", T[ydst][t0 - 64:t0, 1024 + h0:1024 + h0 + HG], A["rkr"][:], reads=K("rkr"))
                    if di == 0:
                        cx.dma("act", T["VTM"][t0 - 64:t0, h0 * 64:(h0 + HG) * 64],
                               A["UV"][64:128, :, :].bitcast(F32).rearrange("p h t -> p (h t)"), reads=K("UVv"))
                pb, pk = newbank()
                hmm(pb, pk, lambda h: A["BK"][:, h, :], lambda h: A["UV"][:, h, :], K("BK", "UVu", "UVv"))
                tt("dve", A["ST"][:], A["ST"][:], bcl(A["etot"]), ALU.mult, K("ST", "etot"), K("ST"))
                tt("dve", A["ST"][:], A["ST"][:], p3(pb), ALU.add, K("ST") + [pk], K("ST"))
                cx.op("act", lambda e: e.copy(out=A["STr"][:], in_=A["ST"][:]), reads=K("ST"), writes=K("STr"))
                yield

        own = set(range(1, 33))
        for (di, chunks, ydst) in [(0, list(range(0, 33)), "YF"), (1, list(range(65, 0, -1)), "YB")]:
            gens = [run_chain(cid, di, chunks, own, ydst) for cid in range(16 // HG)]
            alive = list(gens)
            while alive:
                for g in list(alive):
                    try:
                        next(g)
                    except StopIteration:
                        alive.remove(g)
    cx.barrier()


def load_w_bf16(cx, nc, es, name, src, K, N, stg, stgkey):
    w = es.enter_context(nc.sbuf_tensor(name, [128, K // 128, N], BF16))
    for c in range(K // 128):
        for n0 in range(0, N, 2048):
            n = min(2048, N - n0)
            cx.dma("pool", w[:, c, n0:n0 + n], src[c * 128:(c + 1) * 128, n0:n0 + n], writes=[name])
    return w


def stage_post(cx, nc, T, PQ):
    with ExitStack() as es:
        def sb(name, shape, dt=F32):
            return es.enter_context(nc.sbuf_tensor(name, shape, dt))
        H = 16
        ident = sb("identP", [128, 128])
        lng = sb("lng", [128, 1024])
        lnb = sb("lnb", [128, 1024])
        gup0 = sb("gup0", [128, 1024])
        gup1 = sb("gup1", [32, 1024])
        gne = sb("gne", [128, 1])
        yf = sb("yf", [128, 1040])
        yb = sb("yb", [128, 1040])
        vt = sb("vt", [128, H, 64])
        y = sb("ypost", [128, H, 64])
        sq = sb("sqpost", [128, H, 64])
        mu = sb("mu", [128, H])
        var = sb("var", [128, H])
        bon = sb("bon", [128, H])
        sg0 = sb("sg0", [128, 128])
        sg1 = sb("sg1", [32, 128])
        yrt = sb("yrt", [128, 8, 128], BF16)
        cx.dma("sp", ident[:], T["ident"], writes=["ident"])
        cx.dma("sp", lng[:], T["lng_b"], writes=["lng"])
        cx.dma("sp", lnb[:], T["lnb_b"], writes=["lnb"])
        cx.dma("sp", gup0[:], T["g_up"][0:128, :], writes=["gup0"])
        cx.dma("sp", gup1[:], T["g_up"][128:160, :], writes=["gup1"])
        cx.op("dve", lambda e: e.memset(gne[:], 64e-5), writes=["gne"])
        for ti in range(16):
            t0 = ti * 128
            cx.dma("sp", yf[:], T["YF"][t0:t0 + 128, :], writes=["yf"])
            cx.dma("sp", yb[:], T["YB"][t0:t0 + 128, :], writes=["yb"])
            cx.dma("sp", vt[:].rearrange("p h t -> p (h t)"), T["VTM"][t0:t0 + 128, :], writes=["vt"])
            cx.dma("sp", sg0[:], T["XSL"][0:128, 64 + t0:64 + t0 + 128], writes=["sg0"])
            cx.dma("sp", sg1[:], T["XSL"][128:160, 64 + t0:64 + t0 + 128], writes=["sg1"])
            cx.op("act", lambda e: e.activation(out=sg0[:], in_=sg0[:], func=AF.Sigmoid), reads=["sg0"], writes=["sg0"])
            cx.op("act", lambda e: e.activation(out=sg1[:], in_=sg1[:], func=AF.Sigmoid), reads=["sg1"], writes=["sg1"])
            pq = PQ[ti % 2]
            pk = ("pq", ti % 2)
            for nb in range(2):
                cx.op("pe", lambda e: e.matmul(out=pq[:, nb * 512:nb * 512 + 512], lhsT=sg0[:, :],
                                               rhs=gup0[:, nb * 512:nb * 512 + 512], start=True, stop=False),
                      reads=["sg0", "gup0"], writes=[pk], last=False)
                cx.op("pe", lambda e: e.matmul(out=pq[:, nb * 512:nb * 512 + 512], lhsT=sg1[:, :],
                                               rhs=gup1[:, nb * 512:nb * 512 + 512], start=False, stop=True),
                      reads=["sg1", "gup1"], writes=[pk], last=(nb == 1))
            y3f = yf[:, 0:1024].rearrange("p (h t) -> p h t", h=H)
            y3b = yb[:, 0:1024].rearrange("p (h t) -> p h t", h=H)
            cx.op("dve", lambda e: e.tensor_tensor(out=y[:], in0=y3f, in1=y3b, op=ALU.add), reads=["yf", "yb"], writes=["y"])
            cx.op("dve", lambda e: e.tensor_reduce(out=mu[:], in_=y[:], axis=AX.X, op=ALU.add), reads=["y"], writes=["mu"])
            cx.op("dve", lambda e: e.tensor_scalar(out=mu[:], in0=mu[:], scalar1=1.0 / 64, scalar2=None, op0=ALU.mult),
                  reads=["mu"], writes=["mu"])
            cx.op("dve", lambda e: e.tensor_tensor(out=y[:], in0=y[:], in1=mu[:].unsqueeze(2).to_broadcast([128, H, 64]),
                                                   op=ALU.subtract), reads=["y", "mu"], writes=["y"])
            cx.op("dve", lambda e: e.tensor_tensor(out=sq[:], in0=y[:], in1=y[:], op=ALU.mult), reads=["y"], writes=["sq"])
            cx.op("dve", lambda e: e.tensor_reduce(out=var[:], in_=sq[:], axis=AX.X, op=ALU.add), reads=["sq"], writes=["var"])
            cx.op("act", lambda e: e.activation(out=var[:], in_=var[:], func=AF.Sqrt, scale=1.0 / 64, bias=gne[:, 0:1]),
                  reads=["var", "gne"], writes=["var"])
            cx.op("dve", lambda e: e.reciprocal(out=var[:], in_=var[:]), reads=["var"], writes=["var"])
            cx.op("dve", lambda e: e.tensor_tensor(out=y[:], in0=y[:], in1=var[:].unsqueeze(2).to_broadcast([128, H, 64]),
                                                   op=ALU.mult), reads=["y", "var"], writes=["y"])
            yfl = y[:].rearrange("p h t -> p (h t)")
            cx.op("dve", lambda e: e.tensor_tensor(out=yfl, in0=yfl, in1=lng[:], op=ALU.mult), reads=["y", "lng"], writes=["y"])
            cx.op("dve", lambda e: e.tensor_tensor(out=yfl, in0=yfl, in1=lnb[:], op=ALU.add), reads=["y", "lnb"], writes=["y"])
            cx.op("dve", lambda e: e.tensor_tensor(out=bon[:], in0=yf[:, 1024:1040], in1=yb[:, 1024:1040], op=ALU.add),
                  reads=["yf", "yb"], writes=["bon"])
            cx.op("dve", lambda e: e.scalar_tensor_tensor(out=sq[:], in0=vt[:], scalar=0.5,
                                                          in1=bon[:].unsqueeze(2).to_broadcast([128, H, 64]),
                                                          op0=ALU.mult, op1=ALU.mult), reads=["vt", "bon"], writes=["sq"])
            cx.op("dve", lambda e: e.tensor_tensor(out=y[:], in0=y[:], in1=sq[:], op=ALU.add), reads=["y", "sq"], writes=["y"])
            cx.op("dve", lambda e: e.tensor_tensor(out=yfl, in0=yfl, in1=pq[:, :], op=ALU.mult), reads=["y", pk], writes=["y"])
            pq2 = PQ[2 + ti % 2]
            pk2 = ("pq", 2 + ti % 2)
            for c in range(8):
                cx.op("pe", lambda e: e.transpose(out=pq2[:, c * 128:(c + 1) * 128], in_=yfl[:, c * 128:(c + 1) * 128],
                                                  identity=ident[:, :]), reads=["y", "ident"], writes=[pk2], last=(c == 7))
            cx.op("act", lambda e: e.copy(out=yrt[:], in_=pq2[:, :].rearrange("p (c t) -> p c t", c=8)),
                  reads=[pk2], writes=["yrt"])
            cx.dma("sp", T["YRT"][:, t0:t0 + 128].rearrange("(c p) t -> p c t", p=128), yrt[:], reads=["yrt"])
    cx.barrier()


def stage_mla(cx, nc, T, PQ, PS):
    with ExitStack() as es:
        def sb(name, shape, dt=F32):
            return es.enter_context(nc.sbuf_tensor(name, shape, dt))
        ident = sb("identM", [128, 128])
        ones = sb("onesM", [128, 128])
        stg = [sb("mstg%d" % i, [128, 2048]) for i in range(2)]
        epsc = sb("epscM", [128, 1])
        negpi = sb("negpi", [64, 1])
        invf = sb("invfs", [64, 1])
        gq = sb("gq", [128, 4])
        gkv = sb("gkv", [128, 4])
        kbias = sb("kbias", [128, 33])
        kvn = sb("kvnT", [128, 4, NTOK], BF16)
        qn = sb("qnT", [128, 4, NOWN], BF16)
        cos2 = sb("cos2", [64, NTOK])
        sin2 = sb("sin2", [64, NTOK])
        kr = sb("krT", [128, NTOK], BF16)
        cx.dma("sp", ident[:], T["ident"], writes=["ident"])
        cx.dma("sp", invf[:], T["invf"], writes=["invf"])
        cx.dma("sp", gq[:], T["gq_fm"], writes=["gq"])
        cx.dma("sp", gkv[:], T["gkv_fm"], writes=["gkv"])
        cx.dma("sp", kbias[:], T["valid_tm"], writes=["kbias"])
        cx.op("dve", lambda e: e.tensor_scalar(out=kbias[:], in0=kbias[:], scalar1=-1.0, scalar2=30000.0, op0=ALU.add,
                                               op1=ALU.mult), reads=["kbias"], writes=["kbias"])
        cx.op("dve", lambda e: e.memset(ones[:], 1.0), writes=["ones"])
        cx.op("dve", lambda e: e.memset(epsc[:], EPS), writes=["epsc"])
        cx.op("dve", lambda e: e.memset(negpi[:], -float(np.pi)), writes=["negpi"])
        wuq = load_w_bf16(cx, nc, es, "wuq", T["w_uq"], 512, 3072, stg, "mstg")
        wukv = load_w_bf16(cx, nc, es, "wukv", T["w_ukv"], 512, 4096, stg, "mstg")
        wqr = sb("wqrot", [128, 4, 16, 64], BF16)
        wuq4 = wuq[:].rearrange("p c (h e) -> p c h e", h=16)
        cx.op("pool", lambda e: e.tensor_scalar(out=wqr[:, :, :, 0:32], in0=wuq4[:, :, :, 160:192], scalar1=-1.0,
                                                scalar2=None, op0=ALU.mult), reads=["wuq"], writes=["wqr"])
        cx.op("pool", lambda e: e.tensor_copy(out=wqr[:, :, :, 32:64], in_=wuq4[:, :, :, 128:160]), reads=["wuq"],
              writes=["wqr"])
        cx.barrier()
        kf = stg[0][0:64, 0:1056]
        ki = stg[1][0:64, 0:1056].bitcast(I32)
        TWO_PI = float(2 * np.pi)
        for (tab, off, nm) in [(sin2, 0.0, "sin2"), (cos2, float(0.5 * np.pi), "cos2")]:
            for hb in range(4):
                cs = slice(hb * 1056, hb * 1056 + 1056)
                cx.dma("sp", tab[:, cs], T["pos64"][:, cs], writes=[nm])
                cx.op("dve", lambda e: e.tensor_scalar(out=tab[:, cs], in0=tab[:, cs], scalar1=invf[:, 0:1], scalar2=off,
                                                       op0=ALU.mult, op1=ALU.add), reads=[nm, "invf"], writes=[nm])
                cx.op("dve", lambda e: e.tensor_scalar(out=kf, in0=tab[:, cs], scalar1=1.0 / TWO_PI, scalar2=None,
                                                       op0=ALU.mult), reads=[nm], writes=["kf"])
                cx.op("dve", lambda e: e.tensor_copy(out=ki, in_=kf), reads=["kf"], writes=["ki"])
                cx.op("dve", lambda e: e.tensor_copy(out=kf, in_=ki), reads=["ki"], writes=["kf"])
                cx.op("dve", lambda e: e.scalar_tensor_tensor(out=tab[:, cs], in0=kf, scalar=-TWO_PI, in1=tab[:, cs],
                                                              op0=ALU.mult, op1=ALU.add), reads=["kf", nm], writes=[nm])
                cx.op("dve", lambda e: e.tensor_scalar(out=kf, in0=tab[:, cs], scalar1=float(np.pi), scalar2=None,
                                                       op0=ALU.is_gt), reads=[nm], writes=["kf"])
                cx.op("dve", lambda e: e.scalar_tensor_tensor(out=tab[:, cs], in0=kf, scalar=-TWO_PI, in1=tab[:, cs],
                                                              op0=ALU.mult, op1=ALU.add), reads=["kf", nm], writes=[nm])
                cx.op("dve", lambda e: e.tensor_scalar(out=kf, in0=tab[:, cs], scalar1=-float(np.pi), scalar2=None,
                                                       op0=ALU.is_lt), reads=[nm], writes=["kf"])
                cx.op("dve", lambda e: e.scalar_tensor_tensor(out=tab[:, cs], in0=kf, scalar=TWO_PI, in1=tab[:, cs],
                                                              op0=ALU.mult, op1=ALU.add), reads=["kf", nm], writes=[nm])
                cx.op("act", lambda e: e.activation(out=tab[:, cs], in_=tab[:, cs], func=AF.Sin),
                      reads=[nm], writes=[nm])
        cx.barrier()
        def latent_norm(row0, tok0, ntok, dst, g, nm):
            for b0 in range(0, ntok, 512):
                n = min(512, ntok - b0)
                xs_ = []
                pq, pk = PQ[0], ("pq", 0)
                for c in range(4):
                    st_ = stg[c % 2]
                    half = (c // 2) * 1024
                    cx.dma("sp", st_[:, half:half + n], T["PRT"][row0 + c * 128:row0 + c * 128 + 128,
                                                                  1 + tok0 + b0:1 + tok0 + b0 + n],
                           writes=[("mstg", c % 2, c // 2)])
                    cx.op("act", lambda e: e.activation(out=st_[:, half + 512:half + 512 + n], in_=st_[:, half:half + n],
                                                        func=AF.Square),
                          reads=[("mstg", c % 2, c // 2)], writes=[("msq", c % 2, c // 2)])
                    cx.op("pe", lambda e: e.matmul(out=pq[:, 0:n], lhsT=ones[:, :], rhs=st_[:, half + 512:half + 512 + n],
                                                   start=(c == 0), stop=(c == 3)),
                          reads=[("msq", c % 2, c // 2), "ones"], writes=[pk], last=(c == 3))
                cx.op("act", lambda e: e.activation(out=pq[:, 512:512 + n], in_=pq[:, 0:n], func=AF.Sqrt, scale=1.0 / 512,
                                                    bias=epsc[:, 0:1]), reads=[pk, "epsc"], writes=[("pqb", 0)])
                cx.op("dve", lambda e: e.reciprocal(out=pq[:, 512:512 + n], in_=pq[:, 512:512 + n]),
                      reads=[("pqb", 0)], writes=[("pqb", 0)])
                for c in range(4):
                    st_ = stg[c % 2]
                    half = (c // 2) * 1024
                    cx.op("dve", lambda e: e.scalar_tensor_tensor(out=dst[:, c, b0:b0 + n], in0=st_[:, half:half + n],
                                                                  scalar=g[:, c:c + 1], in1=pq[:, 512:512 + n],
                                                                  op0=ALU.mult, op1=ALU.mult),
                          reads=[("mstg", c % 2, c // 2), ("pqb", 0), nm], writes=[("dst", nm)])
                    cx.buf[("msq", c % 2, c // 2)] = cx.buf.get(("msq", c % 2, c // 2), {"w": None, "r": {}})
        latent_norm(R_KVD, 0, NTOK, kvn, gkv, "gkv")
        latent_norm(R_QD, OWN0, NOWN, qn, gq, "gq")
        for b0 in range(0, NTOK, 1024):
            n = min(1024, NTOK - b0)
            cx.dma("sp", stg[0][0:64, 0:n], T["PRT"][R_KR:R_KR + 64, 1 + b0:1 + b0 + n], writes=[("mstg", 0, 0), ("mstg", 0, 1)])
            cx.dma("sp", stg[1][0:64, 0:n], T["PRT"][R_KRR:R_KRR + 64, 1 + b0:1 + b0 + n], writes=[("mstg", 1, 0), ("mstg", 1, 1)])
            cx.op("dve", lambda e: e.tensor_tensor(out=stg[0][0:64, 0:n], in0=stg[0][0:64, 0:n], in1=cos2[:, b0:b0 + n],
                                                   op=ALU.mult), reads=[("mstg", 0, 0), ("mstg", 0, 1), "cos2"],
                  writes=[("mstg", 0, 0), ("mstg", 0, 1)])
            cx.op("dve", lambda e: e.tensor_tensor(out=stg[1][0:64, 0:n], in0=stg[1][0:64, 0:n], in1=sin2[:, b0:b0 + n],
                                                   op=ALU.mult), reads=[("mstg", 1, 0), ("mstg", 1, 1), "sin2"],
                  writes=[("mstg", 1, 0), ("mstg", 1, 1)])
            cx.op("dve", lambda e: e.tensor_tensor(out=kr[0:64, b0:b0 + n], in0=stg[0][0:64, 0:n], in1=stg[1][0:64, 0:n],
                                                   op=ALU.add), reads=[("mstg", 0, 0), ("mstg", 0, 1), ("mstg", 1, 0), ("mstg", 1, 1)],
                  writes=["kr"])
        cx.barrier()
        kT = sb("kTh", [128, NTOK], BF16)
        Vh = sb("Vh", [128, 33, 132], BF16)
        qT = sb("qTh", [128, NOWN], BF16)
        qr = sb("qrh", [128, NOWN], BF16)
        qa = sb("qra", [128, 512])
        qb_ = sb("qrb", [64, 512])
        PT = [sb("PT%d" % i, [128, 512], BF16) for i in range(2)]
        ymt = [sb("ymt%d" % i, [128, 512], BF16) for i in range(2)]
        rs = sb("rsum", [1, 512])
        bcs = qa
        ones1 = sb("ones1", [1, 128])
        onesb = sb("onesb", [128, 128], BF16)
        cx.op("dve", lambda e: e.memset(ones1[:], 1.0), writes=["ones1"])
        cx.op("dve", lambda e: e.memset(onesb[:], 1.0), writes=["onesb"])
        qi_box = [0]
        cx.op("dve", lambda e: e.memset(Vh[:, :, 128:129], 1.0), writes=["Vh1"])
        cx.op("pool", lambda e: e.memset(kr[64:128, :], 0.0), writes=["kr0"])
        cx.op("pool", lambda e: e.memset(qr[64:128, :], 0.0), writes=["qr0"])
        scale = float(192 ** -0.5)
        kk = 0
        kk_box = [0]
        for h in range(16):
            for b0 in range(0, NTOK, 512):
                n = min(512, NTOK - b0)
                pb, pbk = PS[kk % 2], ("ps", kk % 2)
                kk += 1
                for c in range(4):
                    cx.op("pe", lambda e: e.matmul(out=pb[:, 0:n], lhsT=wukv[:, c, h * 256:h * 256 + 128],
                                                   rhs=kvn[:, c, b0:b0 + n], start=(c == 0), stop=(c == 3)),
                          reads=["wukv", ("dst", "gkv")], writes=[pbk], last=(c == 3))
                cx.op("act", lambda e: e.copy(out=kT[:, b0:b0 + n], in_=pb[:, 0:n]), reads=[pbk], writes=["kT"])
            for kt in range(33):
                pb, pbk = PS[kk % 2], ("ps", kk % 2)
                kk += 1
                for c in range(4):
                    cx.op("pe", lambda e: e.matmul(out=pb[:, 0:128], lhsT=kvn[:, c, kt * 128:kt * 128 + 128],
                                                   rhs=wukv[:, c, h * 256 + 128:h * 256 + 256], start=(c == 0), stop=(c == 3)),
                          reads=["wukv", ("dst", "gkv")], writes=[pbk], last=(c == 3))
                cx.op("dve", lambda e: e.tensor_copy(out=Vh[:, kt, 0:128], in_=pb[:, 0:128]), reads=[pbk], writes=["Vh"])
            for b0 in range(0, NOWN, 512):
                pb, pbk = PS[kk % 2], ("ps", kk % 2)
                kk += 1
                for c in range(4):
                    cx.op("pe", lambda e: e.matmul(out=pb[:, 0:512], lhsT=wuq[:, c, h * 192:h * 192 + 128],
                                                   rhs=qn[:, c, b0:b0 + 512], start=(c == 0), stop=(c == 3)),
                          reads=["wuq", ("dst", "gq")], writes=[pbk], last=(c == 3))
                cx.op("act", lambda e: e.copy(out=qT[:, b0:b0 + 512], in_=pb[:, 0:512]), reads=[pbk], writes=["qT"])
                pb, pbk = PS[kk % 2], ("ps", kk % 2)
                kk += 1
                for c in range(4):
                    cx.op("pe", lambda e: e.matmul(out=pb[0:64, 0:512], lhsT=wuq[:, c, h * 192 + 128:h * 192 + 192],
                                                   rhs=qn[:, c, b0:b0 + 512], start=(c == 0), stop=(c == 3)),
                          reads=["wuq", ("dst", "gq")], writes=[pbk], last=(c == 3))
                cx.op("dve", lambda e: e.tensor_tensor(out=qa[0:64, :], in0=pb[0:64, 0:512], in1=cos2[:, OWN0 + b0:OWN0 + b0 + 512],
                                                       op=ALU.mult), reads=[pbk, "cos2"], writes=["qa"])
                pb, pbk = PS[kk % 2], ("ps", kk % 2)
                kk += 1
                for c in range(4):
                    cx.op("pe", lambda e: e.matmul(out=pb[0:64, 0:512], lhsT=wqr[:, c, h, :],
                                                   rhs=qn[:, c, b0:b0 + 512], start=(c == 0), stop=(c == 3)),
                          reads=["wqr", ("dst", "gq")], writes=[pbk], last=(c == 3))
                cx.op("dve", lambda e: e.tensor_tensor(out=qb_[:], in0=pb[0:64, 0:512], in1=sin2[:, OWN0 + b0:OWN0 + b0 + 512],
                                                       op=ALU.mult), reads=[pbk, "sin2"], writes=["qb"])
                cx.op("dve", lambda e: e.tensor_tensor(out=qr[0:64, b0:b0 + 512], in0=qa[0:64, :], in1=qb_[:], op=ALU.add),
                      reads=["qa", "qb"], writes=["qr"])
            for qblk in range(4):
                q0 = qblk * 512
                sbank = {}

                def emit_S(kt):
                    i = kk_box[0] % 2
                    kk_box[0] += 1
                    pb_, pbk_ = PS[i], ("ps", i)
                    cx.op("pe", lambda e: e.matmul(out=pb_[:, 0:512], lhsT=kT[:, kt * 128:kt * 128 + 128],
                                                   rhs=qT[:, q0:q0 + 512], start=True, stop=False),
                          reads=["kT", "qT"], writes=[pbk_], last=False)
                    cx.op("pe", lambda e: e.matmul(out=pb_[:, 0:512], lhsT=kr[:, kt * 128:kt * 128 + 128],
                                                   rhs=qr[:, q0:q0 + 512], start=False, stop=True),
                          reads=["kr", "kr0", "qr", "qr0"], writes=[pbk_])
                    sbank[kt] = (pb_, pbk_)

                emit_S(0)
                for kt in range(33):
                    if kt + 1 < 33:
                        emit_S(kt + 1)
                    pb, pbk = sbank.pop(kt)
                    pt = PT[kt % 2]
                    cx.op("act", lambda e: e.activation(out=pt[:], in_=pb[:, 0:512], func=AF.Exp, scale=scale,
                                                        bias=kbias[:, kt:kt + 1]),
                          reads=[pbk, "kbias"], writes=[("PT", kt % 2)])
                    ob_, sb_ = 4 + 2 * (qi_box[0] % 2), 5 + 2 * (qi_box[0] % 2)
                    cx.op("pe", lambda e: e.matmul(out=PS[ob_][:, 0:512], lhsT=Vh[:, kt, 0:128], rhs=pt[:, 0:512],
                                                   start=(kt == 0), stop=(kt == 32)),
                          reads=[("PT", kt % 2), "Vh"], writes=[("ps", ob_)], last=False)
                    cx.op("pe", lambda e: e.matmul(out=PS[sb_][:, 0:512], lhsT=onesb[:, :], rhs=pt[:, 0:512],
                                                   start=(kt == 0), stop=(kt == 32)),
                          reads=[("PT", kt % 2), "onesb"], writes=[("ps", sb_)])
                cx.op("dve", lambda e: e.reciprocal(out=bcs[:], in_=PS[sb_][:, 0:512]), reads=[("ps", sb_)], writes=["qa"])
                ym = ymt[qi_box[0] % 2]
                cx.op("dve", lambda e: e.tensor_tensor(out=ym[:], in0=PS[ob_][:, 0:512], in1=bcs[:], op=ALU.mult),
                      reads=[("ps", ob_), "qa"], writes=[("ymt", qi_box[0] % 2)])
                cx.dma("sp", T["YMT"][h * 128:h * 128 + 128, q0:q0 + 512], ym[:], reads=[("ymt", qi_box[0] % 2)])
                qi_box[0] += 1
    cx.barrier()


def stage_merge(cx, nc, T, PQ, PS):
    with ExitStack() as es:
        def sb(name, shape, dt=F32):
            return es.enter_context(nc.sbuf_tensor(name, shape, dt))
        stg = [sb("gstg%d" % i, [128, 2048]) for i in range(2)]
        prw = load_w_bf16(cx, nc, es, "prw", T["p_rwkv"], 1024, 2048, stg, "gstg")
        pml = load_w_bf16(cx, nc, es, "pml", T["p_mla"], 2048, 2048, stg, "gstg")
        bg = sb("bgate", [128, 32])
        cx.dma("sp", bg[:], T["bgate_fm"], writes=["bg"])
        yr = sb("yrblk", [128, 8, 512], BF16)
        ym = sb("ymblk", [128, 16, 512], BF16)
        gr = sb("grblk", [128, 512])
        gm = sb("gmblk", [128, 512])
        mg = [sb("mgblk%d" % i, [128, 512], BF16) for i in range(2)]
        cx.barrier()
        kk = 0
        for tb in range(4):
            t0 = tb * 512
            cx.dma("sp", yr[:], T["YRT"][:, t0:t0 + 512].rearrange("(c p) t -> p c t", p=128), writes=["yr"])
            cx.dma("sp", ym[:], T["YMT"][:, t0:t0 + 512].rearrange("(c p) t -> p c t", p=128), writes=["ym"])
            for dc in range(16):
                cx.dma("sp", gr[:], T["PRT"][R_GR + dc * 128:R_GR + dc * 128 + 128, 1 + OWN0 + t0:1 + OWN0 + t0 + 512],
                       writes=["gr"])
                cx.dma("sp", gm[:], T["PRT"][R_GM + dc * 128:R_GM + dc * 128 + 128, 1 + OWN0 + t0:1 + OWN0 + t0 + 512],
                       writes=["gm"])
                cx.op("act", lambda e: e.activation(out=gr[:], in_=gr[:], func=AF.Sigmoid, bias=bg[:, dc:dc + 1]),
                      reads=["gr", "bg"], writes=["gr"])
                cx.op("act", lambda e: e.activation(out=gm[:], in_=gm[:], func=AF.Sigmoid, bias=bg[:, 16 + dc:17 + dc]),
                      reads=["gm", "bg"], writes=["gm"])
                pa, pak = PS[kk % 4], ("ps", kk % 4)
                pb, pbk = PS[4 + kk % 4], ("ps", 4 + kk % 4)
                kk += 1
                for c in range(8):
                    cx.op("pe", lambda e: e.matmul(out=pa[:, 0:512], lhsT=prw[:, c, dc * 128:dc * 128 + 128], rhs=yr[:, c, :],
                                                   start=(c == 0), stop=(c == 7)), reads=["prw", "yr"], writes=[pak], last=(c == 7))
                for c in range(16):
                    cx.op("pe", lambda e: e.matmul(out=pb[:, 0:512], lhsT=pml[:, c, dc * 128:dc * 128 + 128], rhs=ym[:, c, :],
                                                   start=(c == 0), stop=(c == 15)), reads=["pml", "ym"], writes=[pbk], last=(c == 15))
                cx.op("dve", lambda e: e.tensor_tensor(out=gr[:], in0=gr[:], in1=pa[:, 0:512], op=ALU.mult),
                      reads=["gr", pak], writes=["gr"])
                cx.op("dve", lambda e: e.tensor_tensor(out=gm[:], in0=gm[:], in1=pb[:, 0:512], op=ALU.mult),
                      reads=["gm", pbk], writes=["gm"])
                m_ = mg[dc % 2]
                cx.op("dve", lambda e: e.tensor_tensor(out=m_[:], in0=gr[:], in1=gm[:], op=ALU.add),
                      reads=["gr", "gm"], writes=[("mg", dc % 2)])
                cx.dma("sp", T["MGT"][dc * 128:dc * 128 + 128, t0:t0 + 512], m_[:], reads=[("mg", dc % 2)])
    cx.barrier()
    with ExitStack() as es:
        def sb(name, shape, dt=F32):
            return es.enter_context(nc.sbuf_tensor(name, shape, dt))
        stg = [sb("hstg%d" % i, [128, 2048]) for i in range(2)]
        wo = load_w_bf16(cx, nc, es, "wo", T["w_o"], 2048, 2048, stg, "hstg")
        mt = sb("mgt", [128, 16, 128], BF16)
        xt = sb("xres", [128, 2048])
        cx.barrier()
        for ti in range(16):
            t0 = ti * 128
            cx.dma("sp", mt[:], T["MGT"][:, t0:t0 + 128].rearrange("(c p) t -> p c t", p=128), writes=["mt"])
            cx.dma("sp", xt[:], T["xp"][OWN0 + t0:OWN0 + t0 + 128, :], writes=["xt"])
            for nb in range(4):
                pb, pbk = PS[(ti * 4 + nb) % 8], ("ps", (ti * 4 + nb) % 8)
                for c in range(16):
                    cx.op("pe", lambda e: e.matmul(out=pb[:, 0:512], lhsT=mt[:, c, :], rhs=wo[:, c, nb * 512:nb * 512 + 512],
                                                   start=(c == 0), stop=(c == 15)), reads=["mt", "wo"], writes=[pbk], last=(c == 15))
                cx.op("dve", lambda e: e.tensor_tensor(out=xt[:, nb * 512:nb * 512 + 512], in0=xt[:, nb * 512:nb * 512 + 512],
                                                       in1=pb[:, 0:512], op=ALU.add), reads=["xt", pbk], writes=["xt"])
            cx.dma("sp", T["H2"][t0:t0 + 128, :], xt[:], reads=["xt"])
    cx.barrier()


def stage_peer(cx, nc, T, PQ, PS):
    with ExitStack() as es0:
        def sb0(name, shape, dt=F32):
            return es0.enter_context(nc.sbuf_tensor(name, shape, dt))
        eidi_all = sb0("eidi_all", [128, 16, 128], I32)
        gate_all = sb0("gate_all", [128, 16, 128])
        ident = sb0("identE", [128, 128])
        gf = sb0("gffn", [128, 2048])
        epsc = sb0("epscE", [128, 1])
        cx.dma("sp", ident[:], T["ident"], writes=["ident"])
        cx.dma("sp", gf[:], T["gffn_b"], writes=["gf"])
        cx.op("dve", lambda e: e.memset(epsc[:], EPS), writes=["epsc"])
        with ExitStack() as es:
            def sb(name, shape, dt=F32):
                return es.enter_context(nc.sbuf_tensor(name, shape, dt))
            stg = [sb("estg%d" % i, [128, 2048]) for i in range(2)]
            wq = load_w_bf16(cx, nc, es, "wqp", T["peer_wq"], 2048, 2048, stg, "estg")
            cx.barrier()
            keysT = sb("keysT", [128, 16, 128])
            iota = sb("iotaE", [128, 256])
            cx.dma("sp", iota[:], T["iota256"], writes=["iota"])
            for g in range(16):
                cx.dma("sp", stg[0][:, g * 128:g * 128 + 128], T["peer_keys"][g], writes=["estg0"])
            for g in range(16):
                pb, pbk = PS[g % 2], ("ps", g % 2)
                cx.op("pe", lambda e: e.transpose(out=pb[:, 0:128], in_=stg[0][:, g * 128:g * 128 + 128], identity=ident[:, :]),
                      reads=["estg0", "ident"], writes=[pbk])
                cx.op("act", lambda e: e.copy(out=keysT[:, g, :], in_=pb[:, 0:128]), reads=[pbk], writes=["keysT"])
            cx.barrier()
            junk = stg[1][:]
            JK = "junkA"
            h2 = sb("h2", [128, 2048])
            hn = sb("hn", [128, 2048])
            hnT = sb("hnT", [128, 16, 128], BF16)
            qT = sb("qTp", [128, 16, 128])
            sc = sb("scp", [128, 16, 128])
            sc2 = sb("scp2", [128, 128])
            tops = sb("tops", [128, 16, 16])
            topi = sb("topi", [128, 16, 16])
            tiu_all = sb("tiu_all", [128, 16, 16], U32)
            piu = sb("piu", [128, 8, 16], U32)
            sc2_all = sb("sc2_all", [128, 16, 128])
            cand = sb("cand", [128, 8, 16, 16])
            cidx = sb("cidx", [128, 8, 16, 16])
            best = sb("best", [128, 8, 16])
            pos = sb("pos", [128, 8, 16])
            eid = sb("eid", [128, 8, 16])
            gate = sb("gate", [128, 8, 16])
            gsum = sb("gsum", [128, 8])
            ssq = sb("ssqE", [128, 2])
            NEG = -1e30
            cand2 = sc[:].rearrange("p (h s) n -> p h (s n)", s=2)
            eqb = junk.rearrange("p (h n) -> p h n", h=8)
            for ti in range(16):
                t0 = ti * 128
                cx.dma("sp", h2[:], T["H2"][t0:t0 + 128, :], writes=["h2"])
                cx.op("act", lambda e: e.activation(out=junk, in_=h2[:], func=AF.Square, accum_out=ssq[:, 0:1]),
                      reads=["h2"], writes=[JK, "ssq0"] + [("jk", i_) for i_ in range(8)])
                cx.op("act", lambda e: e.activation(out=ssq[:, 0:1], in_=ssq[:, 0:1], func=AF.Sqrt, scale=1.0 / D,
                                                    bias=epsc[:, 0:1]), reads=["ssq0", "epsc"], writes=["ssq0"])
                cx.op("dve", lambda e: e.reciprocal(out=ssq[:, 0:1], in_=ssq[:, 0:1]), reads=["ssq0"], writes=["ssq0"])
                cx.op("dve", lambda e: e.scalar_tensor_tensor(out=hn[:], in0=h2[:], scalar=ssq[:, 0:1], in1=gf[:],
                                                              op0=ALU.mult, op1=ALU.mult), reads=["h2", "ssq0", "gf"], writes=["hn"])
                for cg in range(4):
                    pb, pbk = PS[cg % 2], ("ps", cg % 2)
                    for ci in range(4):
                        c = cg * 4 + ci
                        cx.op("pe", lambda e: e.transpose(out=pb[:, ci * 128:ci * 128 + 128], in_=hn[:, c * 128:(c + 1) * 128],
                                                          identity=ident[:, :]), reads=["hn", "ident"], writes=[pbk], last=(ci == 3))
                    cx.op("act", lambda e: e.copy(out=hnT[:, cg * 4:cg * 4 + 4, :],
                                                  in_=pb[:, 0:512].rearrange("p (c t) -> p c t", c=4)), reads=[pbk], writes=["hnT"])
                for g in range(16):
                    pb, pbk = PS[2 + g % 2], ("ps", 2 + g % 2)
                    for c in range(16):
                        cx.op("pe", lambda e: e.matmul(out=pb[:, 0:128], lhsT=wq[:, c, g * 128:g * 128 + 128], rhs=hnT[:, c, :],
                                                       start=(c == 0), stop=(c == 15)), reads=["wqp", "hnT"], writes=[pbk], last=(c == 15))
                    cx.op("act", lambda e: e.copy(out=qT[:, g, :], in_=pb[:, 0:128]), reads=[pbk], writes=[("qT", g)])
                for g in range(16):
                    pb, pbk = PS[g % 2], ("ps", g % 2)
                    cx.op("pe", lambda e: e.matmul(out=pb[:, 0:128], lhsT=qT[:, g, :], rhs=keysT[:, g, :], start=True, stop=True),
                          reads=[("qT", g), "keysT"], writes=[pbk])
                    cx.op("act", lambda e: e.copy(out=sc[:, g, :], in_=pb[:, 0:128]), reads=[pbk], writes=[("sc", g)])
                for g in range(16):
                    cx.op("dve", lambda e: e.max(out=tops[:, g, 0:8], in_=sc[:, g, :]), reads=[("sc", g)], writes=[("tops", g)])
                for g in range(16):
                    cx.op("dve", lambda e: e.max_index(out=tiu_all[:, g, 0:8], in_max=tops[:, g, 0:8], in_values=sc[:, g, :]),
                          reads=[("sc", g), ("tops", g)], writes=[("tiu", g)])
                for g in range(16):
                    cx.op("dve", lambda e: e.match_replace(out=sc2_all[:, g, :], in_to_replace=tops[:, g, 0:8], in_values=sc[:, g, :],
                                                           imm_value=NEG), reads=[("sc", g), ("tops", g)], writes=[("sc2", g)])
                for g in range(16):
                    cx.op("dve", lambda e: e.max(out=tops[:, g, 8:16], in_=sc2_all[:, g, :]), reads=[("sc2", g)], writes=[("tops", g)])
                for g in range(16):
                    cx.op("dve", lambda e: e.max_index(out=tiu_all[:, g, 8:16], in_max=tops[:, g, 8:16], in_values=sc2_all[:, g, :]),
                          reads=[("sc2", g), ("tops", g)], writes=[("tiu", g)])
                cx.op("dve", lambda e: e.tensor_copy(out=topi[:], in_=tiu_all[:]), reads=[("tiu", g) for g in range(16)],
                      writes=[("topi", g) for g in range(16)])
                tkeys = [("tops", g) for g in range(16)]
                ikeys = [("topi", g) for g in range(16)]
                ts4 = tops[:].rearrange("p (h s) k -> p h s k", s=2)
                ti4 = topi[:].rearrange("p (h s) k -> p h s k", s=2)
                cx.op("dve", lambda e: e.tensor_tensor(out=cand[:], in0=ts4[:, :, 0, :].unsqueeze(3).to_broadcast([128, 8, 16, 16]),
                                                       in1=ts4[:, :, 1, :].unsqueeze(2).to_broadcast([128, 8, 16, 16]), op=ALU.add),
                      reads=tkeys, writes=["cand"])
                cx.op("dve", lambda e: e.tensor_scalar(out=eid[:], in0=ti4[:, :, 0, :], scalar1=128.0, scalar2=None, op0=ALU.mult),
                      reads=ikeys, writes=["eid"] + [("eid", hh, k) for hh in range(8) for k in range(16)])
                cx.op("dve", lambda e: e.tensor_tensor(out=cidx[:], in0=eid[:].unsqueeze(3).to_broadcast([128, 8, 16, 16]),
                                                       in1=ti4[:, :, 1, :].unsqueeze(2).to_broadcast([128, 8, 16, 16]),
                                                       op=ALU.add), reads=ikeys + ["eid"], writes=["cidx"])
                c3 = cand[:].rearrange("p h a b -> p h (a b)")
                i3 = cidx[:].rearrange("p h a b -> p h (a b)")
                for hh in range(8):
                    cx.op("dve", lambda e: e.max(out=best[:, hh, 0:8], in_=c3[:, hh, :]), reads=["cand"], writes=[("best", hh)])
                for hh in range(8):
                    cx.op("dve", lambda e: e.max_index(out=piu[:, hh, 0:8], in_max=best[:, hh, 0:8], in_values=c3[:, hh, :]),
                          reads=["cand", ("best", hh)], writes=[("piu", hh)])
                for hh in range(8):
                    cx.op("dve", lambda e: e.match_replace(out=cand2[:, hh, :], in_to_replace=best[:, hh, 0:8], in_values=c3[:, hh, :],
                                                           imm_value=NEG), reads=["cand", ("best", hh)],
                          writes=[("sc", 2 * hh), ("sc", 2 * hh + 1)])
                for hh in range(8):
                    cx.op("dve", lambda e: e.max(out=best[:, hh, 8:16], in_=cand2[:, hh, :]),
                          reads=[("sc", 2 * hh), ("sc", 2 * hh + 1)], writes=[("best", hh)])
                for hh in range(8):
                    cx.op("dve", lambda e: e.max_index(out=piu[:, hh, 8:16], in_max=best[:, hh, 8:16], in_values=cand2[:, hh, :]),
                          reads=[("sc", 2 * hh), ("sc", 2 * hh + 1), ("best", hh)], writes=[("piu", hh)])
                cx.op("dve", lambda e: e.tensor_copy(out=pos[:], in_=piu[:]), reads=[("piu", hh) for hh in range(8)],
                      writes=[("pos", hh) for hh in range(8)])
                bkeys = [("best", hh) for hh in range(8)]
                pkeys = [("pos", hh) for hh in range(8)]
                for hh in range(8):
                    for k in range(16):
                        js_ = (hh * 16 + k) % 8
                        cx.op("dve", lambda e: e.scalar_tensor_tensor(out=junk[:, js_ * 256:js_ * 256 + 256], in0=iota[:, :],
                                                                      scalar=pos[:, hh, k:k + 1],
                                                                      in1=i3[:, hh, :], op0=ALU.is_equal, op1=ALU.mult,
                                                                      accum_out=eid[:, hh, k:k + 1]),
                              reads=["iota", "cidx", ("pos", hh)], writes=[("jk", js_), ("eid", hh, k)])
                cx.op("dve", lambda e: e.tensor_copy(out=eidi_all[:, ti, :], in_=eid[:].rearrange("p h k -> p (h k)")),
                      reads=[("eid", hh, k) for hh in range(8) for k in range(16)], writes=[("eidi", ti)])
                cx.op("dve", lambda e: e.tensor_tensor(out=gate[:], in0=best[:], in1=best[:, :, 0:1].to_broadcast([128, 8, 16]),
                                                       op=ALU.subtract), reads=bkeys, writes=["gate"])
                cx.op("act", lambda e: e.activation(out=gate[:], in_=gate[:], func=AF.Exp), reads=["gate"], writes=["gate"])
                cx.op("dve", lambda e: e.tensor_reduce(out=gsum[:], in_=gate[:], axis=AX.X, op=ALU.add), reads=["gate"], writes=["gsum"])
                cx.op("dve", lambda e: e.reciprocal(out=gsum[:], in_=gsum[:]), reads=["gsum"], writes=["gsum"])
                cx.op("dve", lambda e: e.tensor_tensor(out=gate_all[:, ti, :].rearrange("p (h k) -> p h k", h=8), in0=gate[:],
                                                       in1=gsum[:].unsqueeze(2).to_broadcast([128, 8, 16]),
                                                       op=ALU.mult), reads=["gate", "gsum"], writes=[("gate_all", ti)])

        cx.barrier()
        with ExitStack() as es:
            def sb(name, shape, dt=F32):
                return es.enter_context(nc.sbuf_tensor(name, shape, dt))
            NR = 14
            rows = [sb("rows%d" % i, [128, 4096], BF16)[:] for i in range(NR)]
            gfin = sb("gfin", [128, 2048])
            identb = sb("identEb", [128, 128], BF16)
            h2 = sb("h2b", [128, 2048])
            hnb = sb("hnb", [128, 2048], BF16)
            junkb = sb("junkb", [128, 2048], BF16)
            score = sb("score", [128, 128])
            coef = sb("coef", [128, 128])
            ssq = sb("ssqB", [128, 2])
            dg = [sb("dg%d" % i, [128, 128], BF16) for i in range(8)]
            cx.dma("sp", gfin[:], T["gfin_b"], writes=["gfin"])
            cx.op("dve", lambda e: e.tensor_copy(out=identb[:], in_=ident[:]), reads=["ident"], writes=["identb"])
            junk = rows[NR - 1].bitcast(F32)
            JK = ("rows", NR - 1)
            NG = NR - 1
            for ti in range(16):
                t0 = ti * 128
                cx.dma("sp", h2[:], T["H2"][t0:t0 + 128, :], writes=["h2"])
                cx.op("act", lambda e: e.activation(out=junk, in_=h2[:], func=AF.Square, accum_out=ssq[:, 0:1]),
                      reads=["h2"], writes=[JK, "ssq0"])
                cx.op("act", lambda e: e.activation(out=ssq[:, 0:1], in_=ssq[:, 0:1], func=AF.Sqrt, scale=1.0 / D,
                                                    bias=epsc[:, 0:1]), reads=["ssq0", "epsc"], writes=["ssq0"])
                cx.op("dve", lambda e: e.reciprocal(out=ssq[:, 0:1], in_=ssq[:, 0:1]), reads=["ssq0"], writes=["ssq0"])
                cx.op("dve", lambda e: e.scalar_tensor_tensor(out=hnb[:], in0=h2[:], scalar=ssq[:, 0:1], in1=gf[:],
                                                              op0=ALU.mult, op1=ALU.mult), reads=["h2", "ssq0", "gf"], writes=["hnb"])
                for g in range(32):
                    js = list(range(g * 4, g * 4 + 4))
                    for j in js:
                        rb = rows[j % NG]
                        cx.dma("pool", rb, T["UVB"], indirect=bass.IndirectOffsetOnAxis(ap=eidi_all[:, ti, j:j + 1], axis=0),
                               writes=[("rows", j % NG)])
                        cx.op("dve", lambda e: e.scalar_tensor_tensor(out=junkb[:], in0=rb[:, 0:2048], scalar=1.0, in1=hnb[:],
                                                                      op0=ALU.mult, op1=ALU.mult, accum_out=score[:, j:j + 1]),
                              reads=[("rows", j % NG), "hnb"], writes=["junkb", ("score", g)])
                    cx.op("act", lambda e: e.activation(out=coef[:, g * 4:g * 4 + 4], in_=score[:, g * 4:g * 4 + 4], func=AF.Gelu),
                          reads=[("score", g)], writes=[("coef", g)])
                    cx.op("dve", lambda e: e.tensor_tensor(out=coef[:, g * 4:g * 4 + 4], in0=coef[:, g * 4:g * 4 + 4],
                                                           in1=gate_all[:, ti, g * 4:g * 4 + 4], op=ALU.mult),
                          reads=[("coef", g)], writes=[("coef", g)])
                    for j in js:
                        rb = rows[j % NG]
                        d_ = dg[j % 8]
                        cx.op("act", lambda e: e.activation(out=d_[:], in_=identb[:], func=AF.Copy, scale=coef[:, j:j + 1]),
                              reads=[("coef", g), "identb"], writes=[("dg", j % 8)])
                        for nb in range(4):
                            cx.op("pe", lambda e: e.matmul(out=PS[4 + nb][:, 0:512], lhsT=d_[:],
                                                           rhs=rb[:, 2048 + nb * 512:2048 + nb * 512 + 512],
                                                           start=(j == 0), stop=(j == 127)),
                                  reads=[("dg", j % 8), ("rows", j % NG)], writes=[("ps", 4 + nb)], last=(nb == 3))
                for nb in range(4):
                    cx.op("dve", lambda e: e.tensor_tensor(out=h2[:, nb * 512:nb * 512 + 512], in0=h2[:, nb * 512:nb * 512 + 512],
                                                           in1=PS[4 + nb][:, 0:512], op=ALU.add), reads=["h2", ("ps", 4 + nb)], writes=["h2"])
                cx.op("act", lambda e: e.activation(out=junk, in_=h2[:], func=AF.Square, accum_out=ssq[:, 1:2]),
                      reads=["h2"], writes=[JK, "ssq1"])
                cx.op("act", lambda e: e.activation(out=ssq[:, 1:2], in_=ssq[:, 1:2], func=AF.Sqrt, scale=1.0 / D,
                                                    bias=epsc[:, 0:1]), reads=["ssq1", "epsc"], writes=["ssq1"])
                cx.op("dve", lambda e: e.reciprocal(out=ssq[:, 1:2], in_=ssq[:, 1:2]), reads=["ssq1"], writes=["ssq1"])
                cx.op("dve", lambda e: e.scalar_tensor_tensor(out=junk, in0=h2[:], scalar=ssq[:, 1:2], in1=gfin[:],
                                                              op0=ALU.mult, op1=ALU.mult), reads=["h2", "ssq1", "gfin"], writes=[JK])
                cx.dma("sp", T["out"][t0:t0 + 128, :], junk, reads=[JK])
    cx.barrier()


_NC = None


def kernel(**inputs):
    global _NC
    inputs = {k: np.asarray(v) for k, v in inputs.items()}
    if _NC is None:
        _NC = build()
    in_maps = []
    for b in range(4):
        for s_ in range(2):
            m = prep_core(inputs, b, s_)
            in_maps.append({k: np.ascontiguousarray(v, dtype=np.float32) for k, v in m.items()})
    res = run_bass_kernel_spmd(_NC, in_maps, core_ids=list(range(8)))
    out = np.zeros((4, 4096, D), np.float32)
    for b in range(4):
        for s_ in range(2):
            o = np.asarray(res.results[b * 2 + s_]["out"])
            if s_ == 0:
                out[b, 0:2048] = o
            else:
                out[b, 2048:4096] = o[::-1]
    return out
```

```python
import numpy as np
from contextlib import ExitStack
import concourse.bass as bass
import concourse.mybir as mybir
from concourse.bass_utils import run_bass_kernel_spmd

F32 = mybir.dt.float32
BF16 = mybir.dt.bfloat16
I32 = mybir.dt.int32
U32 = mybir.dt.uint32
AF = mybir.ActivationFunctionType
ALU = mybir.AluOpType
AX = mybir.AxisListType

D = 2048
NTOK = 4224
OWN0, OWN1 = 64, 2112
NOWN = 2048
HALF = 2112
NCH = 66
C = 64
RW = 3488
EPS = 1e-6
C0 = float(np.exp(-0.5))

R_R, R_K, R_V, R_GD, R_WDF, R_WDB, R_ADF, R_ADB = 0, 1024, 2048, 3072, 3232, 3296, 3360, 3424
R_QD, R_KVD, R_KR, R_KRR, R_GR, R_GM = 3488, 4000, 4512, 4576, 4640, 6688
NROWS = 8736


class Ctx:
    def __init__(self, nc, es):
        self.nc = nc
        self.E = {"pe": nc.tensor, "dve": nc.vector, "act": nc.scalar, "pool": nc.gpsimd, "sp": nc.sync}
        self.psem = {k: es.enter_context(nc.semaphore("prog_" + k)) for k in ["pe", "dve", "act", "pool"]}
        self.pcnt = {k: 0 for k in self.psem}
        self.seen = {}
        self.buf = {}
        self.dq = {}
        for q in ["sp", "pool", "act"]:
            sems = [es.enter_context(nc.semaphore("dq_%s_%d" % (q, i))) for i in range(20)]
            self.dq[q] = {"sems": sems, "cnt": [0] * len(sems), "i": 0}
        self.ninst = 0
        self.pend = {}

    def wait(self, eng, tok):
        key, sem, val = tok
        k = (eng, key)
        if self.seen.get(k, 0) >= val:
            return
        self.E[eng].wait_ge(sem, val)
        self.seen[k] = val

    def _deps(self, reads, writes):
        deps = []
        for k in reads:
            st = self.buf.get(k)
            if st and st["w"]:
                deps.append(st["w"])
        for k in writes:
            st = self.buf.get(k)
            if st:
                if st["w"]:
                    deps.append(st["w"])
                deps.extend(st["r"].values())
        return deps

    def _commit(self, tok, reads, writes):
        for k in reads:
            st = self.buf.setdefault(k, {"w": None, "r": {}})
            old = st["r"].get(tok[0])
            if old is None or old[2] < tok[2]:
                st["r"][tok[0]] = tok
        for k in writes:
            self.buf[k] = {"w": tok, "r": {}}

    def op(self, eng, fn, reads=(), writes=(), last=True):
        for d in self._deps(reads, writes):
            self.wait(eng, d)
        ins = fn(self.E[eng])
        self.ninst += 1
        pend = self.pend.setdefault(eng, [])
        pend.append((list(reads), list(writes)))
        if not last:
            return None
        self.pcnt[eng] += 1
        ins.then_inc(self.psem[eng], 1)
        tok = (eng, self.psem[eng], self.pcnt[eng])
        for r, w in pend:
            self._commit(tok, r, w)
        del pend[:]
        return tok

    def dma(self, q, out, in_, reads=(), writes=(), indirect=None, slow=False):
        dq = self.dq[q]
        i = dq["i"]
        dq["i"] = (i + 1) % len(dq["sems"])
        sem = dq["sems"][i]
        key = "dq_%s_%d" % (q, i)
        if dq["cnt"][i] > 0:
            self.wait(q, (key, sem, dq["cnt"][i]))
        for d in self._deps(reads, writes):
            self.wait(q, d)
        if indirect is None:
            ins = self.E[q].dma_start(out=out, in_=in_, allow_slow_non_contiguous=slow)
        else:
            ins = self.E[q].indirect_dma_start(out=out, out_offset=None, in_=in_, in_offset=indirect)
        dq["cnt"][i] += 16
        ins.then_inc(sem, 16)
        tok = (key, sem, dq["cnt"][i])
        self._commit(tok, reads, writes)
        self.ninst += 1
        return tok

    def barrier(self):
        toks = [(k, self.psem[k], self.pcnt[k]) for k in self.psem if self.pcnt[k] > 0]
        for q, dq in self.dq.items():
            for i, sem in enumerate(dq["sems"]):
                if dq["cnt"][i] > 0:
                    toks.append(("dq_%s_%d" % (q, i), sem, dq["cnt"][i]))
        for eng in ["pe", "dve", "act", "pool", "sp"]:
            for t in toks:
                self.wait(eng, t)
        self.buf = {}


def stage_inproj(cx, nc, T, PS):
    with ExitStack() as es:
        def sb(name, shape, dt):
            return es.enter_context(nc.sbuf_tensor(name, shape, dt))
        nT = sb("nT", [128, 16, HALF], BF16)
        xt = [sb("xt%d" % i, [128, D], F32) for i in range(2)]
        xs = [sb("xs%d" % i, [128, D], F32) for i in range(2)]
        ssq = sb("ssq", [128, 40], F32)
        rstd = sb("rstd", [128, 40], F32)
        gS = sb("gS", [128, 16], F32)
        ident = sb("identA", [128, 128], F32)
        wb = [sb("wb%d" % i, [128, 16, 256], BF16) for i in range(2)]
        wrot = sb("wrot", [128, 16, 64], BF16)
        stg = [sb("stg%d" % i, [128, 512], F32) for i in range(4)]
        zero = sb("zeroA", [128, 2], F32)

        cx.dma("sp", ident[:], T["ident"], writes=["ident"])
        cx.dma("sp", gS[:], T["norm_mix_g"], writes=["gS"])
        epsc = sb("epsc", [128, 1], F32)
        cx.op("dve", lambda e: e.memset(epsc[:], EPS), writes=["epsc"])
        cx.op("dve", lambda e: e.memset(zero[:], 0.0), writes=["zero"])
        for r0 in range(0, NROWS, 128):
            m = min(128, NROWS - r0)
            cx.dma("sp", T["PRT"][r0:r0 + m, 0:1], zero[0:m, 0:1], reads=["zero"], slow=True)
            cx.dma("sp", T["PRT"][r0:r0 + m, NTOK + 1:NTOK + 2], zero[0:m, 1:2], reads=["zero"], slow=True)

        units = []
        for u0 in range(0, 3072, 256):
            units.append((T["w_in"][:, u0:u0 + 256], 256, [(0, 128, u0, False), (128, 128, u0 + 128, False)], False))
        units.append((T["w_lora"][:, 0:160], 160, [(0, 128, R_GD, False), (128, 32, R_GD + 128, False)], False))
        units.append((T["w_lora"][:, 160:416], 256, [(0, 64, R_WDF, False), (64, 64, R_WDB, False),
                                                      (128, 64, R_ADF, False), (192, 64, R_ADB, False)], False))
        for u0 in range(0, 512, 256):
            units.append((T["w_in"][:, 4000 + u0:4000 + u0 + 256], 256,
                          [(0, 128, R_KVD + u0, False), (128, 128, R_KVD + u0 + 128, False)], False))
        units.append((T["w_in"][:, 4512:4576], 64, [(0, 64, R_KR, False), (0, 64, R_KRR, True)], False))
        for u0 in range(0, 512, 256):
            units.append((T["w_in"][:, 3488 + u0:3488 + u0 + 256], 256,
                          [(0, 128, R_QD + u0, False), (128, 128, R_QD + u0 + 128, False)], True))
        for u0 in range(0, 4096, 256):
            units.append((T["w_in"][:, 4576 + u0:4576 + u0 + 256], 256,
                          [(0, 128, R_GR + u0, False), (128, 128, R_GR + u0 + 128, False)], True))

        tcb = [sb("tcb%d" % i, [128, 2048], BF16) for i in range(4)]

        def tabconv_gen():
            k = 0
            for ti in range(128):
                for tab, src in enumerate(["peer_u", "peer_v"]):
                    b_ = k % 4
                    cx.dma("pool", tcb[b_][:], T[src][ti * 128:(ti + 1) * 128, :], writes=[("tcb", b_)])
                    cx.dma("act", T["UVB"][ti * 128:(ti + 1) * 128, tab * 2048:(tab + 1) * 2048], tcb[b_][:],
                           reads=[("tcb", b_)])
                    k += 1
                    yield

        tcg = tabconv_gen()
        ev = [0]
        for half in range(2):
            tbase = half * HALF
            for ti in range(HALF // 128 + 1):
                t0 = tbase + ti * 128
                n = min(128, tbase + HALF - t0)
                if n <= 0:
                    continue
                b = ti % 2
                tt = half * 17 + ti
                cx.dma("sp", xt[b][0:n, :], T["xp"][t0:t0 + n, :], writes=[("xt", b)])
                cx.op("act", lambda e: e.activation(out=xs[b][0:n, :], in_=xt[b][0:n, :], func=AF.Square,
                                                    accum_out=ssq[0:n, tt:tt + 1]),
                      reads=[("xt", b)], writes=[("xs", b), ("ssq", tt)])
                cx.op("act", lambda e: e.activation(out=ssq[0:n, tt:tt + 1], in_=ssq[0:n, tt:tt + 1], func=AF.Sqrt,
                                                    scale=1.0 / D, bias=epsc[0:n, 0:1]),
                      reads=[("ssq", tt), "epsc"], writes=[("ssq", tt)])
                cx.op("dve", lambda e: e.reciprocal(out=rstd[0:n, tt:tt + 1], in_=ssq[0:n, tt:tt + 1]),
                      reads=[("ssq", tt)], writes=[("rstd", tt)])
                cx.op("act", lambda e: e.activation(out=xs[b][0:n, :], in_=xt[b][0:n, :], func=AF.Copy,
                                                    scale=rstd[0:n, tt:tt + 1]),
                      reads=[("xt", b), ("rstd", tt)], writes=[("xs", b)])
                for cg in range(4):
                    pb = PS[(tt * 4 + cg) % 2]
                    for ci in range(4):
                        c = cg * 4 + ci
                        cx.op("pe", lambda e: e.transpose(out=pb[:, ci * 128:ci * 128 + n],
                                                          in_=xs[b][0:n, c * 128:(c + 1) * 128], identity=ident[0:n, 0:n]),
                              reads=[("xs", b), "ident"], writes=[("ps", (tt * 4 + cg) % 2, ci)], last=(ci == 3))
                    tl = t0 - tbase
                    cx.op("dve", lambda e: e.tensor_tensor(
                        out=nT[:, cg * 4:cg * 4 + 4, tl:tl + n],
                        in0=pb[:, 0:512].rearrange("p (c t) -> p c t", c=4)[:, :, 0:n],
                        in1=gS[:, cg * 4:cg * 4 + 4].unsqueeze(2).to_broadcast([128, 4, n]), op=ALU.mult),
                        reads=[("ps", (tt * 4 + cg) % 2, i) for i in range(4)] + ["gS"],
                        writes=[("nT", ti)])
            if half == 0:
                blocks = [(0, 64, False)] + [(64 + 512 * i, 512, True) for i in range(4)]
            else:
                blocks = [(HALF + 512 * i, 512, False) for i in range(4)] + [(4160, 64, False)]
            nTreads = [("nT", i) for i in range(17)]
            active = [u for u in units if not (u[3] and half == 1)]

            def issue_load(idx):
                src_, U_, segs_, _ = active[idx]
                wb_ = wb[idx % 2]
                cx.dma("pool", wb_[:, :, 0:U_], src_.rearrange("(c p) u -> p c u", p=128), writes=[("wb", idx % 2)])
                if any(s_[3] for s_ in segs_):
                    cx.op("pool", lambda e: e.tensor_scalar(out=wrot[:, :, 0:32], in0=wb_[:, :, 32:64], scalar1=-1.0,
                                                            scalar2=None, op0=ALU.mult),
                          reads=[("wb", idx % 2)], writes=["wrot"])
                    cx.op("pool", lambda e: e.tensor_copy(out=wrot[:, :, 32:64], in_=wb_[:, :, 0:32]),
                          reads=[("wb", idx % 2)], writes=["wrot"])

            issue_load(0)
            for ui, (src, U, segs, own_only) in enumerate(active):
                if ui + 1 < len(active):
                    issue_load(ui + 1)
                wbuf = ui % 2
                for _ in range(5):
                    next(tcg, None)
                for (off, M, drow, rot) in segs:
                    for (t0, n, isown) in blocks:
                        if own_only and not isown:
                            continue
                        k = ev[0]
                        ev[0] += 1
                        pbk = 2 + (k % 4)
                        pb = PS[pbk]
                        tl = t0 - tbase
                        for c in range(16):
                            lhsT = wrot[:, c, 0:64] if rot else wb[wbuf][:, c, off:off + M]
                            cx.op("pe", lambda e: e.matmul(out=pb[0:M, 0:n], lhsT=lhsT, rhs=nT[:, c, tl:tl + n],
                                                           start=(c == 0), stop=(c == 15)),
                                  reads=[("wb", wbuf), "wrot"] + (nTreads if c == 0 else []),
                                  writes=[("psb", pbk)], last=(c == 15))
                        sg = stg[k % 4]
                        if k % 2 == 0:
                            cx.op("act", lambda e: e.copy(out=sg[0:M, 0:n], in_=pb[0:M, 0:n]),
                                  reads=[("psb", pbk)], writes=[("stg", k % 4)])
                        else:
                            cx.op("dve", lambda e: e.tensor_copy(out=sg[0:M, 0:n], in_=pb[0:M, 0:n]),
                                  reads=[("psb", pbk)], writes=[("stg", k % 4)])
                        cx.dma("sp", T["PRT"][drow:drow + M, 1 + t0:1 + t0 + n], sg[0:M, 0:n],
                               reads=[("stg", k % 4)])
        for _ in tcg:
            pass
    cx.barrier()


IN_SPECS = [
    ("xp", [NTOK, D]), ("ident", [128, 128]),
    ("norm_mix_g", [128, 16]), ("w_in", [D, 8672]), ("w_lora", [D, 416]), ("shiftc_fm", [128, 28, 3]), ("ones64", [64, 64]), ("rmask", [64, 16, 64]),
    ("maskG0", [128, 128]), ("maskG1", [128, 128]), ("maskN0", [64, 64]), ("maskN1", [64, 64]),
    ("wup0", [65, 1024]), ("wup1", [65, 1024]), ("aup0", [65, 1024]), ("aup1", [65, 1024]),
    ("kk_fm", [64, 16]), ("ka_fm", [64, 16]), ("rk_fm", [64, 16]),
    ("lng_b", [128, 1024]), ("lnb_b", [128, 1024]), ("g_up", [160, 1024]), ("invf", [64, 1]),
    ("gq_fm", [128, 4]), ("gkv_fm", [128, 4]), ("valid_tm", [128, 33]), ("pos64", [64, NTOK]),
    ("w_uq", [512, 3072]), ("w_ukv", [512, 4096]), ("p_rwkv", [1024, 2048]), ("p_mla", [2048, 2048]),
    ("w_o", [2048, 2048]), ("bgate_fm", [128, 32]), ("peer_wq", [2048, 2048]), ("peer_keys", [16, 128, 128]),
    ("peer_u", [16384, 2048]), ("peer_v", [16384, 2048]), ("gffn_b", [128, 2048]), ("gfin_b", [128, 2048]), ("iota256", [128, 256]),
]


def build(upto="all", debug_out=()):
    nc = bass.Bass("TRN2", target_bir_lowering=False)
    T = {}
    for name, shape in IN_SPECS:
        T[name] = nc.dram_tensor(name, shape, F32, kind="ExternalInput").ap()
    def scratch(name, shape, dt=F32):
        kind = "ExternalOutput" if name in debug_out else "Internal"
        T[name] = nc.dram_tensor(name, shape, dt, kind=kind).ap()
    scratch("PRT", [NROWS, NTOK + 2])
    scratch("XSQ", [3, NCH, 64, 16, 64])
    scratch("XSL", [416, NTOK])
    scratch("YF", [NOWN, 1040])
    scratch("YB", [NOWN, 1040])
    scratch("VTM", [NOWN, 1024])
    scratch("YRT", [1024, NOWN], BF16)
    scratch("YMT", [2048, NOWN], BF16)
    scratch("MGT", [2048, NOWN], BF16)
    scratch("H2", [NOWN, D])
    scratch("UVB", [16384, 4096], BF16)
    T["out"] = nc.dram_tensor("out", [NOWN, D], F32, kind="ExternalOutput").ap()
    with ExitStack() as es:
        PQ = [es.enter_context(nc.psum_tensor("pq%d" % i, [128, 1024], F32)) for i in range(4)]
        PS = [PQ[i // 2][:, (i % 2) * 512:(i % 2) * 512 + 512] for i in range(8)]
        cx = Ctx(nc, es)
        stage_inproj(cx, nc, T, PS)
        if upto == "inproj":
            print("instructions:", cx.ninst)
            return nc
        stage_shift(cx, nc, T)
        stage_rwkv(cx, nc, T, PQ, PS)
        if upto == "rwkv":
            print("instructions:", cx.ninst)
            return nc
        stage_post(cx, nc, T, PQ)
        stage_mla(cx, nc, T, PQ, PS)
        if upto == "mla":
            print("instructions:", cx.ninst)
            return nc
        stage_merge(cx, nc, T, PQ, PS)
        stage_peer(cx, nc, T, PQ, PS)
        print("instructions:", cx.ninst)
    return nc


def prep_core(inputs, b, s):
    x = inputs["x"]
    meta = inputs["meta_tokens"]
    xp = np.zeros((NTOK, D), np.float32)
    posv = np.zeros((1, NTOK), np.float32)
    valid = np.zeros((1, NTOK), np.float32)
    if s == 0:
        xp[48:64] = meta
        xp[64:64 + 4096] = x[b]
        posv[0, 48:64] = np.arange(16)
        posv[0, 64:64 + 4096] = 16 + np.arange(4096)
        valid[0, 48:64 + 4096] = 1
    else:
        xp[64:64 + 4096] = x[b, ::-1]
        xp[4160:4176] = meta[::-1]
        posv[0, 64:64 + 4096] = 16 + np.arange(4095, -1, -1)
        posv[0, 4160:4176] = np.arange(15, -1, -1)
        valid[0, 64:4176] = 1
    w_in = inputs["w_in"][0]
    sc = inputs["shift_c"][0]
    lo = R_GD
    if s == 0:
        order = [(R_GD, 160), (R_WDF, 64), (R_WDB, 64), (R_ADF, 64), (R_ADB, 64)]
    else:
        order = [(R_GD, 160), (R_WDB, 64), (R_WDF, 64), (R_ADB, 64), (R_ADF, 64)]
    cols = np.concatenate([np.arange(a, a + n) for a, n in order])
    w_lora = np.ascontiguousarray(w_in[:, cols])
    shiftc = sc.copy()
    shiftc[:, lo:RW] = sc[:, cols]
    if s == 1:
        shiftc = shiftc[::-1]
    m = {
        "xp": xp, "posv": posv, "valid": valid, "ident": np.eye(128, dtype=np.float32),
        "norm_mix_g": np.ascontiguousarray(inputs["norm_mix_g"][0].reshape(16, 128).T), "w_in": w_in, "w_lora": w_lora,
    }
    scp = np.zeros((3, 28 * 128), np.float32)
    scp[:, :RW] = shiftc
    m["shiftc_fm"] = np.ascontiguousarray(scp.reshape(3, 28, 128).transpose(2, 1, 0))
    m["ones64"] = np.ones((64, 64), np.float32)
    rm = np.ones((64, 16, 64), np.float32)
    rm[:, :, 0] = 0
    m["rmask"] = rm
    tt = np.arange(64)
    for di in range(2):
        if di == 0:
            strict = (tt[:, None] < tt[None, :]); incl = (tt[:, None] <= tt[None, :])
        else:
            strict = (tt[:, None] > tt[None, :]); incl = (tt[:, None] >= tt[None, :])
        m["maskG%d" % di] = np.block([[strict, incl], [strict, incl]]).astype(np.float32)
        m["maskN%d" % di] = np.ascontiguousarray(strict.T).astype(np.float32)
        d = di if s == 0 else 1 - di
        m["wup%d" % di] = np.concatenate([inputs["w_up"][0, d], inputs["w0"][0, d][None]], 0)
        m["aup%d" % di] = np.concatenate([inputs["a_up"][0, d], inputs["a0"][0, d][None]], 0)
    m["kk_fm"] = np.ascontiguousarray(inputs["k_k"][0].reshape(16, 64).T)
    m["ka_fm"] = np.ascontiguousarray(inputs["k_a"][0].reshape(16, 64).T)
    m["rk_fm"] = np.ascontiguousarray(inputs["r_k"][0].T)
    rep = lambda v, n: np.ascontiguousarray(np.broadcast_to(v[None, :], (n, v.shape[0])))
    m["lng_b"] = rep(inputs["ln_x_g"][0], 128)
    m["lnb_b"] = rep(inputs["ln_x_b"][0], 128)
    m["g_up"] = inputs["g_up"][0]
    invf = (10000.0 ** (-np.arange(0, 64, 2, dtype=np.float32) / 64)).astype(np.float32)
    m["invf"] = np.concatenate([invf, invf])[:, None]
    m["gq_fm"] = np.ascontiguousarray(inputs["q_norm_g"][0].reshape(4, 128).T)
    m["gkv_fm"] = np.ascontiguousarray(inputs["kv_norm_g"][0].reshape(4, 128).T)
    m["valid_tm"] = np.ascontiguousarray(valid[0].reshape(33, 128).T)
    m["pos64"] = rep(posv[0], 64)
    for k in ["w_uq", "w_ukv", "p_rwkv", "p_mla", "w_o", "peer_wq", "peer_u", "peer_v"]:
        m[k] = inputs[k][0]
    m["bgate_fm"] = np.ascontiguousarray(inputs["b_gate"][0].reshape(32, 128).T)
    m["peer_keys"] = inputs["peer_keys"][0].reshape(16, 128, 128)
    m["gffn_b"] = rep(inputs["norm_ffn_g"][0], 128)
    m["gfin_b"] = rep(inputs["final_norm_g"], 128)
    m["iota256"] = np.ascontiguousarray(np.broadcast_to(np.arange(256, dtype=np.float32)[None, :], (128, 256)))
    del m["posv"], m["valid"]
    return m


def stage_shift(cx, nc, T):
    with ExitStack() as es:
        def sb(name, shape, dt):
            return es.enter_context(nc.sbuf_tensor(name, shape, dt))
        sc = sb("shc", [128, 28, 3], F32)
        raw = [sb("shraw%d" % i, [128, 514], F32) for i in range(4)]
        xo = [sb("shxo%d" % i, [128, 512], F32) for i in range(4)]
        cx.dma("sp", sc[:], T["shiftc_fm"], writes=["sc"])
        k = 0
        for rt in range(28):
            r0 = rt * 128
            m = min(128, RW - r0)
            for t0 in range(0, NTOK, 512):
                n = min(512, NTOK - t0)
                b = k % 4
                k += 1
                cx.dma("sp", raw[b][0:m, 0:n + 2], T["PRT"][r0:r0 + m, t0:t0 + n + 2], writes=[("raw", b)])
                cx.op("act", lambda e: e.activation(out=xo[b][0:m, 0:n], in_=raw[b][0:m, 1:n + 1], func=AF.Copy,
                                                    scale=sc[0:m, rt, 1:2]),
                      reads=[("raw", b), "sc"], writes=[("xo", b)])
                cx.op("dve", lambda e: e.scalar_tensor_tensor(out=xo[b][0:m, 0:n], in0=raw[b][0:m, 0:n],
                                                              scalar=sc[0:m, rt, 0:1], in1=xo[b][0:m, 0:n],
                                                              op0=ALU.mult, op1=ALU.add),
                      reads=[("raw", b), ("xo", b), "sc"], writes=[("xo", b)])
                cx.op("dve", lambda e: e.scalar_tensor_tensor(out=xo[b][0:m, 0:n], in0=raw[b][0:m, 2:n + 2],
                                                              scalar=sc[0:m, rt, 2:3], in1=xo[b][0:m, 0:n],
                                                              op0=ALU.mult, op1=ALU.add),
                      reads=[("raw", b), ("xo", b), "sc"], writes=[("xo", b)])
                if rt < 24:
                    q, hp = rt // 8, rt % 8
                    c0 = t0 // 64
                    ncs = n // 64
                    for hh in range(2):
                        h = 2 * hp + hh
                        cx.dma("pool", T["XSQ"][q, c0:c0 + ncs, :, h, :].rearrange("c j t -> j c t"),
                               xo[b][hh * 64:hh * 64 + 64, 0:n].rearrange("j (c t) -> j c t", t=64),
                               reads=[("xo", b)])
                else:
                    cx.dma("pool", T["XSL"][r0 - 3072:r0 - 3072 + m, t0:t0 + n], xo[b][0:m, 0:n], reads=[("xo", b)])
    cx.barrier()


def stage_rwkv(cx, nc, T, PQ, PS):
    F32R = mybir.dt.float32r
    HG = 4
    with ExitStack() as es:
        def sb(name, shape, dt=F32):
            return es.enter_context(nc.sbuf_tensor(name, shape, dt))
        identf = sb("identRf", [128, 128])
        ident = sb("identR", [128, 128], F32R)
        ones64 = sb("ones64s", [64, 64], F32R)
        rmask = sb("rmasks", [64, HG, 64])
        maskG = [sb("maskGs%d" % i, [128, 128]) for i in range(2)]
        maskN = [sb("maskNs%d" % i, [64, 64]) for i in range(2)]
        wup = [sb("wups%d" % i, [65, 1024], F32R) for i in range(2)]
        aup = [sb("aups%d" % i, [65, 1024], F32R) for i in range(2)]
        kkp = sb("kkp", [64, 16])
        kap = sb("kap", [64, 16])
        omka = sb("omka", [64, 16])
        rkp = sb("rkp", [64, 16])
        wstg = sb("wstg", [65, 1024])
        tinyb = sb("tinyb", [64, 1])
        cx.op("dve", lambda e: e.memset(tinyb[:], 1e-24), writes=["tinyb"])
        cx.dma("sp", identf[:], T["ident"], writes=["identf"])
        cx.op("dve", lambda e: e.tensor_copy(out=ident[:], in_=identf[:]), reads=["identf"], writes=["ident"])
        onesf = sb("onesf", [65, 64])
        cx.op("dve", lambda e: e.memset(onesf[:], 1.0), writes=["onesf"])
        cx.op("dve", lambda e: e.tensor_copy(out=ones64[:], in_=onesf[0:64, :]), reads=["onesf"], writes=["ones64"])
        cx.dma("sp", rmask[:], T["rmask"][:, 0:HG, :], writes=["rmask"])
        for i in range(2):
            cx.dma("sp", maskG[i][:], T["maskG%d" % i], writes=["maskG%d" % i])
            cx.dma("sp", maskN[i][:], T["maskN%d" % i], writes=["maskN%d" % i])
            for (dst, src, nm) in [(wup[i], "wup%d" % i, "wup%d" % i), (aup[i], "aup%d" % i, "aup%d" % i)]:
                cx.dma("sp", wstg[:], T[src], writes=["wstg"])
                cx.op("dve", lambda e: e.tensor_copy(out=dst[:], in_=wstg[:]), reads=["wstg"], writes=[nm])
        cx.dma("sp", kkp[:], T["kk_fm"], writes=["kkp"])
        cx.dma("sp", kap[:], T["ka_fm"], writes=["kap"])
        cx.dma("sp", rkp[:], T["rk_fm"], writes=["rkp"])
        cx.op("dve", lambda e: e.tensor_scalar(out=omka[:], in0=kap[:], scalar1=-1.0, scalar2=1.0, op0=ALU.mult,
                                               op1=ALU.add), reads=["kap"], writes=["omka"])

        bank = [0]

        def newbank():
            i = bank[0] % 8
            bank[0] += 1
            return PS[i], ("ps", i)

        def make_chain(cid):
            h0 = cid * HG
            A = {}
            for n in ["rT", "kT", "sg", "ic", "PF", "Pex", "Pin", "ea", "er", "ei", "em", "kx", "t1", "kk", "kt", "kki",
                      "Q0", "YO", "ST"]:
                A[n] = sb("c%d_%s" % (cid, n), [64, HG, 64])
            for n in ["t2", "Nk0", "Nk1", "Mk0", "Mk1", "TT0", "TT1", "X", "STr"]:
                A[n] = sb("c%d_%s" % (cid, n), [64, HG, 64], F32R)
            A["vpad"] = sb("c%d_vpad" % cid, [64, HG, 128])
            A["LBp"] = sb("c%d_LBp" % cid, [64, HG, 128])
            A["LB"] = sb("c%d_LB" % cid, [64, HG, 128], F32R)
            A["RA"] = sb("c%d_RA" % cid, [64, HG, 128], F32R)
            A["GM"] = sb("c%d_GM" % cid, [128, HG, 128], F32R)
            A["UV"] = sb("c%d_UV" % cid, [128, HG, 64], F32R)
            A["BK"] = sb("c%d_BK" % cid, [128, HG, 64], F32R)
            A["wdraw"] = sb("c%d_wdraw" % cid, [64, 64])
            A["adraw"] = sb("c%d_adraw" % cid, [64, 64])
            A["wd"] = sb("c%d_wd" % cid, [65, 64], F32R)
            A["ad"] = sb("c%d_ad" % cid, [65, 64], F32R)
            A["tot"] = sb("c%d_tot" % cid, [64, HG])
            A["etot"] = sb("c%d_etot" % cid, [64, HG])
            A["rkr"] = sb("c%d_rkr" % cid, [64, HG])
            return A

        chains = [make_chain(cid) for cid in range(16 // HG)]

        def run_chain(cid, di, chunks, ychunks, ydst):
            A = chains[cid]
            h0 = cid * HG

            def K(*names):
                return [(cid, n) for n in names]

            def bc(p):
                return p[:, h0:h0 + HG].unsqueeze(2).to_broadcast([64, HG, 64])

            def bcl(p):
                return p[:].unsqueeze(2).to_broadcast([64, HG, 64])

            def tt(eng, out, a, b, op, reads, writes):
                return cx.op(eng, lambda e: e.tensor_tensor(out=out, in0=a, in1=b, op=op), reads=reads, writes=writes)

            def act(out, in_, func, reads, writes, **kw):
                return cx.op("act", lambda e: e.activation(out=out, in_=in_, func=func, **kw), reads=reads, writes=writes)

            def hmm(pb, pk, lhsf, rhsf, reads, w=64):
                for h in range(HG):
                    cx.op("pe", lambda e: e.matmul(out=pb[0:64, h * w:h * w + w], lhsT=lhsf(h), rhs=rhsf(h), start=True,
                                                   stop=True), reads=reads, writes=[pk], last=(h == HG - 1))

            def p3(pb, rows=64, w=64):
                return pb[0:rows, 0:HG * w].rearrange("p (h t) -> p h t", h=HG)

            def loads(c):
                t0 = c * 64
                cx.dma("sp", A["rT"][:], T["XSQ"][0, c, :, h0:h0 + HG, :], writes=K("rT"))
                cx.dma("sp", A["kT"][:], T["XSQ"][1, c, :, h0:h0 + HG, :], writes=K("kT"))
                cx.dma("sp", A["vpad"][:, :, 64:128], T["XSQ"][2, c, :, h0:h0 + HG, :], writes=K("vpad"))
                lw0 = (R_WDF if di == 0 else R_WDB) - 3072
                la0 = (R_ADF if di == 0 else R_ADB) - 3072
                cx.dma("sp", A["wdraw"][:], T["XSL"][lw0:lw0 + 64, t0:t0 + 64], writes=K("wdraw"))
                cx.dma("sp", A["adraw"][:], T["XSL"][la0:la0 + 64, t0:t0 + 64], writes=K("adraw"))

            cx.op("dve", lambda e: e.memset(A["ST"][:], 0.0), writes=K("ST"))
            cx.op("dve", lambda e: e.tensor_copy(out=A["STr"][:], in_=A["ST"][:]), reads=K("ST"), writes=K("STr"))
            cx.op("dve", lambda e: e.tensor_copy(out=A["wd"][64:65, :], in_=onesf[64:65, :]), reads=["onesf"], writes=K("wd1"))
            cx.op("dve", lambda e: e.tensor_copy(out=A["ad"][64:65, :], in_=onesf[64:65, :]), reads=["onesf"], writes=K("ad1"))
            cx.op("dve", lambda e: e.memset(A["vpad"][:], 0.0), writes=K("vpad"))
            loads(chunks[0])
            yield
            for ci, c in enumerate(chunks):
                t0 = c * 64
                want_y = c in ychunks
                act(A["wd"][0:64, :], A["wdraw"][:], AF.Tanh, K("wdraw"), K("wd"))
                act(A["ad"][0:64, :], A["adraw"][:], AF.Copy, K("adraw"), K("ad"))
                pb, pk = newbank()
                hmm(pb, pk, lambda h: wup[di][0:65, (h0 + h) * 64:(h0 + h) * 64 + 64], lambda h: A["wd"][0:65, :],
                    K("wd", "wd1") + ["wup%d" % di])
                act(A["sg"][:], p3(pb), AF.Sigmoid, [pk], K("sg"))
                pb, pk = newbank()
                hmm(pb, pk, lambda h: aup[di][0:65, (h0 + h) * 64:(h0 + h) * 64 + 64], lambda h: A["ad"][0:65, :],
                    K("ad", "ad1") + ["aup%d" % di])
                act(A["ic"][:], p3(pb), AF.Sigmoid, [pk], K("ic"))
                yield
                cx.op("dve", lambda e: e.tensor_tensor_scan(out=A["PF"][:].rearrange("p h t -> p (h t)"),
                                                            data0=rmask[:].rearrange("p h t -> p (h t)"),
                                                            data1=A["sg"][:].rearrange("p h t -> p (h t)"),
                                                            initial=0.0, op0=ALU.mult, op1=ALU.add),
                      reads=["rmask"] + K("sg"), writes=K("PF"))
                if di == 0:
                    tt("dve", A["Pex"][:], A["PF"][:], A["sg"][:], ALU.subtract, K("PF", "sg"), K("Pex"))
                    pin = "PF"
                else:
                    tt("dve", A["Pex"][:], A["PF"][:, :, 63:64].to_broadcast([64, HG, 64]), A["PF"][:], ALU.subtract, K("PF"), K("Pex"))
                    tt("dve", A["Pin"][:], A["Pex"][:], A["sg"][:], ALU.add, K("Pex", "sg"), K("Pin"))
                    pin = "Pin"
                tt("pool", A["kx"][:], A["kT"][:], bc(kkp), ALU.mult, K("kT") + ["kkp"], K("kx"))
                tt("pool", A["t2"][:], A["kx"][:], A["kx"][:], ALU.mult, K("kx"), K("t2"))
                yield
                act(A["ea"][:], A["Pex"][:], AF.Exp, K("Pex"), K("ea"), scale=-C0)
                act(A["er"][:], A[pin][:], AF.Exp, K(pin), K("er"), scale=-C0)
                act(A["ei"][:], A[pin][:], AF.Exp, K(pin), K("ei"), scale=C0)
                act(A["etot"][:], A["PF"][:, :, 63], AF.Exp, K("PF"), K("etot"), scale=-C0)
                pb, pk = newbank()
                cx.op("pe", lambda e: e.matmul(out=pb[0:64, 0:HG * 64], lhsT=ones64[:, :], rhs=A["t2"][:, :, :], start=True,
                                               stop=True), reads=K("t2") + ["ones64"], writes=[pk])
                act(A["kk"][:], p3(pb), AF.Sqrt, [pk, "tinyb"], K("kk"), bias=tinyb[:, 0:1])
                tt("pool", A["t1"][:], A["ic"][:], bc(kap), ALU.mult, K("ic") + ["kap"], K("t1"))
                tt("pool", A["t1"][:], A["t1"][:], bc(omka), ALU.add, K("t1") + ["omka"], K("t1"))
                tt("pool", A["kt"][:], A["kT"][:], A["t1"][:], ALU.mult, K("kT", "t1"), K("kt"))
                yield
                cx.op("dve", lambda e: e.reciprocal(out=A["kk"][:], in_=A["kk"][:]), reads=K("kk"), writes=K("kk"))
                tt("dve", A["kk"][:], A["kk"][:], A["kx"][:], ALU.mult, K("kk", "kx"), K("kk"))
                tt("pool", A["kki"][:], A["kk"][:], A["ic"][:], ALU.mult, K("kk", "ic"), K("kki"))
                LB4 = A["LB"][:].rearrange("p h (s t) -> p h s t", s=2)
                RA4 = A["RA"][:].rearrange("p h (s t) -> p h s t", s=2)
                LP4 = A["LBp"][:].rearrange("p h (s t) -> p h s t", s=2)
                tt("pool", LB4[:, :, 0, :], A["kki"][:], A["ei"][:], ALU.mult, K("kki", "ei"), K("LB"))
                tt("pool", LB4[:, :, 1, :], A["kt"][:], A["ei"][:], ALU.mult, K("kt", "ei", "LB"), K("LB"))
                cx.op("dve", lambda e: e.scalar_tensor_tensor(out=RA4[:, :, 0, :], in0=A["kk"][:], scalar=-1.0,
                                                              in1=A["ea"][:], op0=ALU.mult, op1=ALU.mult),
                      reads=K("kk", "ea"), writes=K("RA"))
                tt("dve", RA4[:, :, 1, :], A["rT"][:], A["er"][:], ALU.mult, K("rT", "er", "RA"), K("RA"))
                tt("pool", A["LBp"][:], A["LB"][:], A["etot"][:].unsqueeze(2).to_broadcast([64, HG, 128]), ALU.mult,
                   K("LB", "etot"), K("LBp"))
                if want_y:
                    tt("pool", A["t1"][:], A["rT"][:], A["kt"][:], ALU.mult, K("rT", "kt"), K("t1"))
                    tt("pool", A["t2"][:], A["t1"][:], bc(rkp), ALU.mult, K("t1") + ["rkp"], K("t2"))
                yield
                if want_y:
                    pb, pk = newbank()
                    hmm(pb, pk, lambda h: A["t2"][:, h, :], lambda h: ones64[:, 0:2], K("t2") + ["ones64"], w=2)
                    cx.op("act", lambda e: e.copy(out=A["rkr"][:], in_=pb[0:64, 0:2 * HG].rearrange("p (h t) -> p h t", t=2)[:, :, 0]),
                          reads=[pk], writes=K("rkr"))
                pb, pk = newbank()
                for h in range(HG):
                    cx.op("pe", lambda e: e.matmul(out=pb[:, h * 128:h * 128 + 128], lhsT=A["LB"][:, h, :], rhs=A["RA"][:, h, :],
                                                   start=True, stop=True), reads=K("LB", "RA"), writes=[pk], last=(h == HG - 1))
                tt("dve", A["GM"][:], pb[:, 0:HG * 128].rearrange("p (h t) -> p h t", h=HG),
                   maskG[di][:].unsqueeze(1).to_broadcast([128, HG, 128]), ALU.mult, [pk, "maskG%d" % di], K("GM"))
                pb, pk = newbank()
                hmm(pb, pk, lambda h: A["RA"][:, h, 0:64], lambda h: A["LB"][:, h, 0:64], K("RA", "LB"))
                tt("dve", A["Nk0"][:], p3(pb), maskN[di][:].unsqueeze(1).to_broadcast([64, HG, 64]), ALU.mult,
                   [pk, "maskN%d" % di], K("Nk0"))
                yield
                cx.op("act", lambda e: e.copy(out=A["Mk0"][:], in_=A["GM"][0:64, :, 0:64]), reads=K("GM"), writes=K("Mk0"))
                tt("pool", A["TT0"][:], A["GM"][0:64, :, 0:64], identf[0:64, 0:64].unsqueeze(1).to_broadcast([64, HG, 64]),
                   ALU.add, K("GM") + ["identf"], K("TT0"))
                pb, pk = newbank()
                for h in range(HG):
                    cx.op("pe", lambda e: e.transpose(out=pb[:, h * 64:h * 64 + 64], in_=A["vpad"][:, h, :],
                                                      identity=identf[0:64, 0:64]), reads=K("vpad") + ["identf"], writes=[pk], last=(h == HG - 1))
                cx.op("act", lambda e: e.copy(out=A["UV"][64:128, :, :], in_=pb[64:128, 0:HG * 64].rearrange("p (h t) -> p h t", h=HG)),
                      reads=[pk], writes=K("UVv"))
                pb, pk = newbank()
                for h in range(HG):
                    cx.op("pe", lambda e: e.transpose(out=pb[:, h * 64:h * 64 + 64], in_=A["LBp"][:, h, :],
                                                      identity=identf[0:64, 0:64]), reads=K("LBp") + ["identf"], writes=[pk], last=(h == HG - 1))
                cx.op("act", lambda e: e.copy(out=A["BK"][:], in_=pb[:, 0:HG * 64].rearrange("p (h t) -> p h t", h=HG)),
                      reads=[pk], writes=K("BK"))
                if ci + 1 < len(chunks):
                    loads(chunks[ci + 1])
                yield
                cur = 0
                for lvl in range(5):
                    nxt = 1 - cur
                    Nc, Mc, Nn, Mn = "Nk%d" % cur, "Mk%d" % cur, "Nk%d" % nxt, "Mk%d" % nxt
                    pb, pk = newbank()
                    hmm(pb, pk, lambda h: A[Mc][:, h, :], lambda h: A[Nc][:, h, :], K(Mc, Nc))
                    cx.op("act", lambda e: e.copy(out=A[Nn][:], in_=p3(pb)), reads=[pk], writes=K(Nn))
                    if lvl < 4:
                        pb, pk = newbank()
                        hmm(pb, pk, lambda h: A[Nc][:, h, :], lambda h: A[Mc][:, h, :], K(Mc, Nc))
                        cx.op("act", lambda e: e.copy(out=A[Mn][:], in_=p3(pb)), reads=[pk], writes=K(Mn))
                    yield
                    Tc, Tn = "TT%d" % cur, "TT%d" % nxt
                    pb, pk = newbank()
                    hmm(pb, pk, lambda h: A[Nn][:, h, :], lambda h: A[Tc][:, h, :], K(Nn, Tc))
                    tt("dve", A[Tn][:], p3(pb), A[Tc][:], ALU.add, [pk] + K(Tc), K(Tn))
                    cur = nxt
                    yield
                TTf = "TT%d" % cur
                pb, pk = newbank()
                hmm(pb, pk, lambda h: A["GM"][64:128, h, 0:64], lambda h: A["UV"][64:128, h, :], K("GM", "UVv"))
                cx.op("act", lambda e: e.copy(out=A["Q0"][:], in_=p3(pb)), reads=[pk], writes=K("Q0"))
                yield
                pb, pk = newbank()
                hmm(pb, pk, lambda h: A["RA"][:, h, 0:64], lambda h: A["STr"][:, h, :], K("RA", "STr"))
                tt("dve", A["X"][:], p3(pb), A["Q0"][:], ALU.add, [pk] + K("Q0"), K("X"))
                yield
                pb, pk = newbank()
                hmm(pb, pk, lambda h: A[TTf][:, h, :], lambda h: A["X"][:, h, :], K(TTf, "X"))
                cx.op("act", lambda e: e.copy(out=A["UV"][0:64, :, :], in_=p3(pb)), reads=[pk], writes=K("UVu"))
                yield
                if want_y:
                    pb, pk = newbank()
                    for h in range(HG):
                        cx.op("pe", lambda e: e.matmul(out=pb[0:64, h * 64:h * 64 + 64], lhsT=A["RA"][:, h, 64:128],
                                                       rhs=A["STr"][:, h, :], start=True, stop=False),
                              reads=K("RA", "STr"), writes=[pk], last=False)
                        cx.op("pe", lambda e: e.matmul(out=pb[0:64, h * 64:h * 64 + 64], lhsT=A["GM"][:, h, 64:128],
                                                       rhs=A["UV"][:, h, :], start=False, stop=True),
                              reads=K("GM", "UVu", "UVv"), writes=[pk], last=(h == HG - 1))
                    cx.op("act", lambda e: e.copy(out=A["YO"][:], in_=p3(pb)), reads=[pk], writes=K("YO"))
                    cx.dma("act", T[ydst][t0 - 64:t0, h0 * 64:(h0 + HG) * 64], A["YO"][:].rearrange("p h t -> p (h t)"),
                           reads=K("YO"))
                    cx.dma("act", T[ydst][t0 - 64:t0, 1024 + h0:1024 + h0 + HG], A["rkr"][:], reads=K("rkr"))
                    if di == 0:
                        cx.dma("act", T["VTM"][t0 - 64:t0, h0 * 64:(h0 + HG) * 64],
                               A["UV"][64:128, :, :].bitcast(F32).rearrange("p h t -> p (h t)"), reads=K("UVv"))
                pb, pk = newbank()
                hmm(pb, pk, lambda h: A["BK"][:, h, :], lambda h: A["UV"][:, h, :], K("BK", "UVu", "UVv"))
                tt("dve", A["ST"][:], A["ST"][:], bcl(A["etot"]), ALU.mult, K("ST", "etot"), K("ST"))
                tt("dve", A["ST"][:], A["ST"][:], p3(pb), ALU.add, K("ST") + [pk], K("ST"))
                cx.op("act", lambda e: e.copy(out=A["STr"][:], in_=A["ST"][:]), reads=K("ST"), writes=K("STr"))
                yield

        own = set(range(1, 33))
        for (di, chunks, ydst) in [(0, list(range(0, 33)), "YF"), (1, list(range(65, 0, -1)), "YB")]:
            gens = [run_chain(cid, di, chunks, own, ydst) for cid in range(16 // HG)]
            alive = list(gens)
            while alive:
                for g in list(alive):
                    try:
                        next(g)
                    except StopIteration:
                        alive.remove(g)
    cx.barrier()


def load_w_bf16(cx, nc, es, name, src, K, N, stg, stgkey):
    w = es.enter_context(nc.sbuf_tensor(name, [128, K // 128, N], BF16))
    for c in range(K // 128):
        for n0 in range(0, N, 2048):
            n = min(2048, N - n0)
            cx.dma("pool", w[:, c, n0:n0 + n], src[c * 128:(c + 1) * 128, n0:n0 + n], writes=[name])
    return w


def stage_post(cx, nc, T, PQ):
    with ExitStack() as es:
        def sb(name, shape, dt=F32):
            return es.enter_context(nc.sbuf_tensor(name, shape, dt))
        H = 16
        ident = sb("identP", [128, 128])
        lng = sb("lng", [128, 1024])
        lnb = sb("lnb", [128, 1024])
        gup0 = sb("gup0", [128, 1024])
        gup1 = sb("gup1", [32, 1024])
        gne = sb("gne", [128, 1])
        yf = sb("yf", [128, 1040])
        yb = sb("yb", [128, 1040])
        vt = sb("vt", [128, H, 64])
        y = sb("ypost", [128, H, 64])
        sq = sb("sqpost", [128, H, 64])
        mu = sb("mu", [128, H])
        var = sb("var", [128, H])
        bon = sb("bon", [128, H])
        sg0 = sb("sg0", [128, 128])
        sg1 = sb("sg1", [32, 128])
        yrt = sb("yrt", [128, 8, 128], BF16)
        cx.dma("sp", ident[:], T["ident"], writes=["ident"])
        cx.dma("sp", lng[:], T["lng_b"], writes=["lng"])
        cx.dma("sp", lnb[:], T["lnb_b"], writes=["lnb"])
        cx.dma("sp", gup0[:], T["g_up"][0:128, :], writes=["gup0"])
        cx.dma("sp", gup1[:], T["g_up"][128:160, :], writes=["gup1"])
        cx.op("dve", lambda e: e.memset(gne[:], 64e-5), writes=["gne"])
        for ti in range(16):
            t0 = ti * 128
            cx.dma("sp", yf[:], T["YF"][t0:t0 + 128, :], writes=["yf"])
            cx.dma("sp", yb[:], T["YB"][t0:t0 + 128, :], writes=["yb"])
            cx.dma("sp", vt[:].rearrange("p h t -> p (h t)"), T["VTM"][t0:t0 + 128, :], writes=["vt"])
            cx.dma("sp", sg0[:], T["XSL"][0:128, 64 + t0:64 + t0 + 128], writes=["sg0"])
            cx.dma("sp", sg1[:], T["XSL"][128:160, 64 + t0:64 + t0 + 128], writes=["sg1"])
            cx.op("act", lambda e: e.activation(out=sg0[:], in_=sg0[:], func=AF.Sigmoid), reads=["sg0"], writes=["sg0"])
            cx.op("act", lambda e: e.activation(out=sg1[:], in_=sg1[:], func=AF.Sigmoid), reads=["sg1"], writes=["sg1"])
            pq = PQ[ti % 2]
            pk = ("pq", ti % 2)
            for nb in range(2):
                cx.op("pe", lambda e: e.matmul(out=pq[:, nb * 512:nb * 512 + 512], lhsT=sg0[:, :],
                                               rhs=gup0[:, nb * 512:nb * 512 + 512], start=True, stop=False),
                      reads=["sg0", "gup0"], writes=[pk], last=False)
                cx.op("pe", lambda e: e.matmul(out=pq[:, nb * 512:nb * 512 + 512], lhsT=sg1[:, :],
                                               rhs=gup1[:, nb * 512:nb * 512 + 512], start=False, stop=True),
                      reads=["sg1", "gup1"], writes=[pk], last=(nb == 1))
            y3f = yf[:, 0:1024].rearrange("p (h t) -> p h t", h=H)
            y3b = yb[:, 0:1024].rearrange("p (h t) -> p h t", h=H)
            cx.op("dve", lambda e: e.tensor_tensor(out=y[:], in0=y3f, in1=y3b, op=ALU.add), reads=["yf", "yb"], writes=["y"])
            cx.op("dve", lambda e: e.tensor_reduce(out=mu[:], in_=y[:], axis=AX.X, op=ALU.add), reads=["y"], writes=["mu"])
            cx.op("dve", lambda e: e.tensor_scalar(out=mu[:], in0=mu[:], scalar1=1.0 / 64, scalar2=None, op0=ALU.mult),
                  reads=["mu"], writes=["mu"])
            cx.op("dve", lambda e: e.tensor_tensor(out=y[:], in0=y[:], in1=mu[:].unsqueeze(2).to_broadcast([128, H, 64]),
                                                   op=ALU.subtract), reads=["y", "mu"], writes=["y"])
            cx.op("dve", lambda e: e.tensor_tensor(out=sq[:], in0=y[:], in1=y[:], op=ALU.mult), reads=["y"], writes=["sq"])
            cx.op("dve", lambda e: e.tensor_reduce(out=var[:], in_=sq[:], axis=AX.X, op=ALU.add), reads=["sq"], writes=["var"])
            cx.op("act", lambda e: e.activation(out=var[:], in_=var[:], func=AF.Sqrt, scale=1.0 / 64, bias=gne[:, 0:1]),
                  reads=["var", "gne"], writes=["var"])
            cx.op("dve", lambda e: e.reciprocal(out=var[:], in_=var[:]), reads=["var"], writes=["var"])
            cx.op("dve", lambda e: e.tensor_tensor(out=y[:], in0=y[:], in1=var[:].unsqueeze(2).to_broadcast([128, H, 64]),
                                                   op=ALU.mult), reads=["y", "var"], writes=["y"])
            yfl = y[:].rearrange("p h t -> p (h t)")
            cx.op("dve", lambda e: e.tensor_tensor(out=yfl, in0=yfl, in1=lng[:], op=ALU.mult), reads=["y", "lng"], writes=["y"])
            cx.op("dve", lambda e: e.tensor_tensor(out=yfl, in0=yfl, in1=lnb[:], op=ALU.add), reads=["y", "lnb"], writes=["y"])
            cx.op("dve", lambda e: e.tensor_tensor(out=bon[:], in0=yf[:, 1024:1040], in1=yb[:, 1024:1040], op=ALU.add),
                  reads=["yf", "yb"], writes=["bon"])
            cx.op("dve", lambda e: e.scalar_tensor_tensor(out=sq[:], in0=vt[:], scalar=0.5,
                                                          in1=bon[:].unsqueeze(2).to_broadcast([128, H, 64]),
                                                          op0=ALU.mult, op1=ALU.mult), reads=["vt", "bon"], writes=["sq"])
            cx.op("dve", lambda e: e.tensor_tensor(out=y[:], in0=y[:], in1=sq[:], op=ALU.add), reads=["y", "sq"], writes=["y"])
            cx.op("dve", lambda e: e.tensor_tensor(out=yfl, in0=yfl, in1=pq[:, :], op=ALU.mult), reads=["y", pk], writes=["y"])
            pq2 = PQ[2 + ti % 2]
            pk2 = ("pq", 2 + ti % 2)
            for c in range(8):
                cx.op("pe", lambda e: e.transpose(out=pq2[:, c * 128:(c + 1) * 128], in_=yfl[:, c * 128:(c + 1) * 128],
                                                  identity=ident[:, :]), reads=["y", "ident"], writes=[pk2], last=(c == 7))
            cx.op("act", lambda e: e.copy(out=yrt[:], in_=pq2[:, :].rearrange("p (c t) -> p c t", c=8)),
                  reads=[pk2], writes=["yrt"])
            cx.dma("sp", T["YRT"][:, t0:t0 + 128].rearrange("(c p) t -> p c t", p=128), yrt[:], reads=["yrt"])
    cx.barrier()


def stage_mla(cx, nc, T, PQ, PS):
    with ExitStack() as es:
        def sb(name, shape, dt=F32):
            return es.enter_context(nc.sbuf_tensor(name, shape, dt))
        ident = sb("identM", [128, 128])
        ones = sb("onesM", [128, 128])
        stg = [sb("mstg%d" % i, [128, 2048]) for i in range(2)]
        epsc = sb("epscM", [128, 1])
        negpi = sb("negpi", [64, 1])
        invf = sb("invfs", [64, 1])
        gq = sb("gq", [128, 4])
        gkv = sb("gkv", [128, 4])
        kbias = sb("kbias", [128, 33])
        kvn = sb("kvnT", [128, 4, NTOK], BF16)
        qn = sb("qnT", [128, 4, NOWN], BF16)
        cos2 = sb("cos2", [64, NTOK])
        sin2 = sb("sin2", [64, NTOK])
        kr = sb("krT", [128, NTOK], BF16)
        cx.dma("sp", ident[:], T["ident"], writes=["ident"])
        cx.dma("sp", invf[:], T["invf"], writes=["invf"])
        cx.dma("sp", gq[:], T["gq_fm"], writes=["gq"])
        cx.dma("sp", gkv[:], T["gkv_fm"], writes=["gkv"])
        cx.dma("sp", kbias[:], T["valid_tm"], writes=["kbias"])
        cx.op("dve", lambda e: e.tensor_scalar(out=kbias[:], in0=kbias[:], scalar1=-1.0, scalar2=30000.0, op0=ALU.add,
                                               op1=ALU.mult), reads=["kbias"], writes=["kbias"])
        cx.op("dve", lambda e: e.memset(ones[:], 1.0), writes=["ones"])
        cx.op("dve", lambda e: e.memset(epsc[:], EPS), writes=["epsc"])
        cx.op("dve", lambda e: e.memset(negpi[:], -float(np.pi)), writes=["negpi"])
        wuq = load_w_bf16(cx, nc, es, "wuq", T["w_uq"], 512, 3072, stg, "mstg")
        wukv = load_w_bf16(cx, nc, es, "wukv", T["w_ukv"], 512, 4096, stg, "mstg")
        wqr = sb("wqrot", [128, 4, 16, 64], BF16)
        wuq4 = wuq[:].rearrange("p c (h e) -> p c h e", h=16)
        cx.op("pool", lambda e: e.tensor_scalar(out=wqr[:, :, :, 0:32], in0=wuq4[:, :, :, 160:192], scalar1=-1.0,
                                                scalar2=None, op0=ALU.mult), reads=["wuq"], writes=["wqr"])
        cx.op("pool", lambda e: e.tensor_copy(out=wqr[:, :, :, 32:64], in_=wuq4[:, :, :, 128:160]), reads=["wuq"],
              writes=["wqr"])
        cx.barrier()
        kf = stg[0][0:64, 0:1056]
        ki = stg[1][0:64, 0:1056].bitcast(I32)
        TWO_PI = float(2 * np.pi)
        for (tab, off, nm) in [(sin2, 0.0, "sin2"), (cos2, float(0.5 * np.pi), "cos2")]:
            for hb in range(4):
                cs = slice(hb * 1056, hb * 1056 + 1056)
                cx.dma("sp", tab[:, cs], T["pos64"][:, cs], writes=[nm])
                cx.op("dve", lambda e: e.tensor_scalar(out=tab[:, cs], in0=tab[:, cs], scalar1=invf[:, 0:1], scalar2=off,
                                                       op0=ALU.mult, op1=ALU.add), reads=[nm, "invf"], writes=[nm])
                cx.op("dve", lambda e: e.tensor_scalar(out=kf, in0=tab[:, cs], scalar1=1.0 / TWO_PI, scalar2=None,
                                                       op0=ALU.mult), reads=[nm], writes=["kf"])
                cx.op("dve", lambda e: e.tensor_copy(out=ki, in_=kf), reads=["kf"], writes=["ki"])
                cx.op("dve", lambda e: e.tensor_copy(out=kf, in_=ki), reads=["ki"], writes=["kf"])
                cx.op("dve", lambda e: e.scalar_tensor_tensor(out=tab[:, cs], in0=kf, scalar=-TWO_PI, in1=tab[:, cs],
                                                              op0=ALU.mult, op1=ALU.add), reads=["kf", nm], writes=[nm])
                cx.op("dve", lambda e: e.tensor_scalar(out=kf, in0=tab[:, cs], scalar1=float(np.pi), scalar2=None,
                                                       op0=ALU.is_gt), reads=[nm], writes=["kf"])
                cx.op("dve", lambda e: e.scalar_tensor_tensor(out=tab[:, cs], in0=kf, scalar=-TWO_PI, in1=tab[:, cs],
                                                              op0=ALU.mult, op1=ALU.add), reads=["kf", nm], writes=[nm])
                cx.op("dve", lambda e: e.tensor_scalar(out=kf, in0=tab[:, cs], scalar1=-float(np.pi), scalar2=None,
                                                       op0=ALU.is_lt), reads=[nm], writes=["kf"])
                cx.op("dve", lambda e: e.scalar_tensor_tensor(out=tab[:, cs], in0=kf, scalar=TWO_PI, in1=tab[:, cs],
                                                              op0=ALU.mult, op1=ALU.add), reads=["kf", nm], writes=[nm])
                cx.op("act", lambda e: e.activation(out=tab[:, cs], in_=tab[:, cs], func=AF.Sin),
                      reads=[nm], writes=[nm])
        cx.barrier()
        def latent_norm(row0, tok0, ntok, dst, g, nm):
            for b0 in range(0, ntok, 512):
                n = min(512, ntok - b0)
                xs_ = []
                pq, pk = PQ[0], ("pq", 0)
                for c in range(4):
                    st_ = stg[c % 2]
                    half = (c // 2) * 1024
                    cx.dma("sp", st_[:, half:half + n], T["PRT"][row0 + c * 128:row0 + c * 128 + 128,
                                                                  1 + tok0 + b0:1 + tok0 + b0 + n],
                           writes=[("mstg", c % 2, c // 2)])
                    cx.op("act", lambda e: e.activation(out=st_[:, half + 512:half + 512 + n], in_=st_[:, half:half + n],
                                                        func=AF.Square),
                          reads=[("mstg", c % 2, c // 2)], writes=[("msq", c % 2, c // 2)])
                    cx.op("pe", lambda e: e.matmul(out=pq[:, 0:n], lhsT=ones[:, :], rhs=st_[:, half + 512:half + 512 + n],
                                                   start=(c == 0), stop=(c == 3)),
                          reads=[("msq", c % 2, c // 2), "ones"], writes=[pk], last=(c == 3))
                cx.op("act", lambda e: e.activation(out=pq[:, 512:512 + n], in_=pq[:, 0:n], func=AF.Sqrt, scale=1.0 / 512,
                                                    bias=epsc[:, 0:1]), reads=[pk, "epsc"], writes=[("pqb", 0)])
                cx.op("dve", lambda e: e.reciprocal(out=pq[:, 512:512 + n], in_=pq[:, 512:512 + n]),
                      reads=[("pqb", 0)], writes=[("pqb", 0)])
                for c in range(4):
                    st_ = stg[c % 2]
                    half = (c // 2) * 1024
                    cx.op("dve", lambda e: e.scalar_tensor_tensor(out=dst[:, c, b0:b0 + n], in0=st_[:, half:half + n],
                                                                  scalar=g[:, c:c + 1], in1=pq[:, 512:512 + n],
                                                                  op0=ALU.mult, op1=ALU.mult),
                          reads=[("mstg", c % 2, c // 2), ("pqb", 0), nm], writes=[("dst", nm)])
                    cx.buf[("msq", c % 2, c // 2)] = cx.buf.get(("msq", c % 2, c // 2), {"w": None, "r": {}})
        latent_norm(R_KVD, 0, NTOK, kvn, gkv, "gkv")
        latent_norm(R_QD, OWN0, NOWN, qn, gq, "gq")
        for b0 in range(0, NTOK, 1024):
            n = min(1024, NTOK - b0)
            cx.dma("sp", stg[0][0:64, 0:n], T["PRT"][R_KR:R_KR + 64, 1 + b0:1 + b0 + n], writes=[("mstg", 0, 0), ("mstg", 0, 1)])
            cx.dma("sp", stg[1][0:64, 0:n], T["PRT"][R_KRR:R_KRR + 64, 1 + b0:1 + b0 + n], writes=[("mstg", 1, 0), ("mstg", 1, 1)])
            cx.op("dve", lambda e: e.tensor_tensor(out=stg[0][0:64, 0:n], in0=stg[0][0:64, 0:n], in1=cos2[:, b0:b0 + n],
                                                   op=ALU.mult), reads=[("mstg", 0, 0), ("mstg", 0, 1), "cos2"],
                  writes=[("mstg", 0, 0), ("mstg", 0, 1)])
            cx.op("dve", lambda e: e.tensor_tensor(out=stg[1][0:64, 0:n], in0=stg[1][0:64, 0:n], in1=sin2[:, b0:b0 + n],
                                                   op=ALU.mult), reads=[("mstg", 1, 0), ("mstg", 1, 1), "sin2"],
                  writes=[("mstg", 1, 0), ("mstg", 1, 1)])
            cx.op("dve", lambda e: e.tensor_tensor(out=kr[0:64, b0:b0 + n], in0=stg[0][0:64, 0:n], in1=stg[1][0:64, 0:n],
                                                   op=ALU.add), reads=[("mstg", 0, 0), ("mstg", 0, 1), ("mstg", 1, 0), ("mstg", 1, 1)],
                  writes=["kr"])
        cx.barrier()
        kT = sb("kTh", [128, NTOK], BF16)
        Vh = sb("Vh", [128, 33, 132], BF16)
        qT = sb("qTh", [128, NOWN], BF16)
        qr = sb("qrh", [128, NOWN], BF16)
        qa = sb("qra", [128, 512])
        qb_ = sb("qrb", [64, 512])
        PT = [sb("PT%d" % i, [128, 512], BF16) for i in range(2)]
        ymt = [sb("ymt%d" % i, [128, 512], BF16) for i in range(2)]
        rs = sb("rsum", [1, 512])
        bcs = qa
        ones1 = sb("ones1", [1, 128])
        onesb = sb("onesb", [128, 128], BF16)
        cx.op("dve", lambda e: e.memset(ones1[:], 1.0), writes=["ones1"])
        cx.op("dve", lambda e: e.memset(onesb[:], 1.0), writes=["onesb"])
        qi_box = [0]
        cx.op("dve", lambda e: e.memset(Vh[:, :, 128:129], 1.0), writes=["Vh1"])
        cx.op("pool", lambda e: e.memset(kr[64:128, :], 0.0), writes=["kr0"])
        cx.op("pool", lambda e: e.memset(qr[64:128, :], 0.0), writes=["qr0"])
        scale = float(192 ** -0.5)
        kk = 0
        kk_box = [0]
        for h in range(16):
            for b0 in range(0, NTOK, 512):
                n = min(512, NTOK - b0)
                pb, pbk = PS[kk % 2], ("ps", kk % 2)
                kk += 1
                for c in range(4):
                    cx.op("pe", lambda e: e.matmul(out=pb[:, 0:n], lhsT=wukv[:, c, h * 256:h * 256 + 128],
                                                   rhs=kvn[:, c, b0:b0 + n], start=(c == 0), stop=(c == 3)),
                          reads=["wukv", ("dst", "gkv")], writes=[pbk], last=(c == 3))
                cx.op("act", lambda e: e.copy(out=kT[:, b0:b0 + n], in_=pb[:, 0:n]), reads=[pbk], writes=["kT"])
            for kt in range(33):
                pb, pbk = PS[kk % 2], ("ps", kk % 2)
                kk += 1
                for c in range(4):
                    cx.op("pe", lambda e: e.matmul(out=pb[:, 0:128], lhsT=kvn[:, c, kt * 128:kt * 128 + 128],
                                                   rhs=wukv[:, c, h * 256 + 128:h * 256 + 256], start=(c == 0), stop=(c == 3)),
                          reads=["wukv", ("dst", "gkv")], writes=[pbk], last=(c == 3))
                cx.op("dve", lambda e: e.tensor_copy(out=Vh[:, kt, 0:128], in_=pb[:, 0:128]), reads=[pbk], writes=["Vh"])
            for b0 in range(0, NOWN, 512):
                pb, pbk = PS[kk % 2], ("ps", kk % 2)
                kk += 1
                for c in range(4):
                    cx.op("pe", lambda e: e.matmul(out=pb[:, 0:512], lhsT=wuq[:, c, h * 192:h * 192 + 128],
                                                   rhs=qn[:, c, b0:b0 + 512], start=(c == 0), stop=(c == 3)),
                          reads=["wuq", ("dst", "gq")], writes=[pbk], last=(c == 3))
                cx.op("act", lambda e: e.copy(out=qT[:, b0:b0 + 512], in_=pb[:, 0:512]), reads=[pbk], writes=["qT"])
                pb, pbk = PS[kk % 2], ("ps", kk % 2)
                kk += 1
                for c in range(4):
                    cx.op("pe", lambda e: e.matmul(out=pb[0:64, 0:512], lhsT=wuq[:, c, h * 192 + 128:h * 192 + 192],
                                                   rhs=qn[:, c, b0:b0 + 512], start=(c == 0), stop=(c == 3)),
                          reads=["wuq", ("dst", "gq")], writes=[pbk], last=(c == 3))
                cx.op("dve", lambda e: e.tensor_tensor(out=qa[0:64, :], in0=pb[0:64, 0:512], in1=cos2[:, OWN0 + b0:OWN0 + b0 + 512],
                                                       op=ALU.mult), reads=[pbk, "cos2"], writes=["qa"])
                pb, pbk = PS[kk % 2], ("ps", kk % 2)
                kk += 1
                for c in range(4):
                    cx.op("pe", lambda e: e.matmul(out=pb[0:64, 0:512], lhsT=wqr[:, c, h, :],
                                                   rhs=qn[:, c, b0:b0 + 512], start=(c == 0), stop=(c == 3)),
                          reads=["wqr", ("dst", "gq")], writes=[pbk], last=(c == 3))
                cx.op("dve", lambda e: e.tensor_tensor(out=qb_[:], in0=pb[0:64, 0:512], in1=sin2[:, OWN0 + b0:OWN0 + b0 + 512],
                                                       op=ALU.mult), reads=[pbk, "sin2"], writes=["qb"])
                cx.op("dve", lambda e: e.tensor_tensor(out=qr[0:64, b0:b0 + 512], in0=qa[0:64, :], in1=qb_[:], op=ALU.add),
                      reads=["qa", "qb"], writes=["qr"])
            for qblk in range(4):
                q0 = qblk * 512
                sbank = {}

                def emit_S(kt):
                    i = kk_box[0] % 2
                    kk_box[0] += 1
                    pb_, pbk_ = PS[i], ("ps", i)
                    cx.op("pe", lambda e: e.matmul(out=pb_[:, 0:512], lhsT=kT[:, kt * 128:kt * 128 + 128],
                                                   rhs=qT[:, q0:q0 + 512], start=True, stop=False),
                          reads=["kT", "qT"], writes=[pbk_], last=False)
                    cx.op("pe", lambda e: e.matmul(out=pb_[:, 0:512], lhsT=kr[:, kt * 128:kt * 128 + 128],
                                                   rhs=qr[:, q0:q0 + 512], start=False, stop=True),
                          reads=["kr", "kr0", "qr", "qr0"], writes=[pbk_])
                    sbank[kt] = (pb_, pbk_)

                emit_S(0)
                for kt in range(33):
                    if kt + 1 < 33:
                        emit_S(kt + 1)
                    pb, pbk = sbank.pop(kt)
                    pt = PT[kt % 2]
                    cx.op("act", lambda e: e.activation(out=pt[:], in_=pb[:, 0:512], func=AF.Exp, scale=scale,
                                                        bias=kbias[:, kt:kt + 1]),
                          reads=[pbk, "kbias"], writes=[("PT", kt % 2)])
                    ob_, sb_ = 4 + 2 * (qi_box[0] % 2), 5 + 2 * (qi_box[0] % 2)
                    cx.op("pe", lambda e: e.matmul(out=PS[ob_][:, 0:512], lhsT=Vh[:, kt, 0:128], rhs=pt[:, 0:512],
                                                   start=(kt == 0), stop=(kt == 32)),
                          reads=[("PT", kt % 2), "Vh"], writes=[("ps", ob_)], last=False)
                    cx.op("pe", lambda e: e.matmul(out=PS[sb_][:, 0:512], lhsT=onesb[:, :], rhs=pt[:, 0:512],
                                                   start=(kt == 0), stop=(kt == 32)),
                          reads=[("PT", kt % 2), "onesb"], writes=[("ps", sb_)])
                cx.op("dve", lambda e: e.reciprocal(out=bcs[:], in_=PS[sb_][:, 0:512]), reads=[("ps", sb_)], writes=["qa"])
                ym = ymt[qi_box[0] % 2]
                cx.op("dve", lambda e: e.tensor_tensor(out=ym[:], in0=PS[ob_][:, 0:512], in1=bcs[:], op=ALU.mult),
                      reads=[("ps", ob_), "qa"], writes=[("ymt", qi_box[0] % 2)])
                cx.dma("sp", T["YMT"][h * 128:h * 128 + 128, q0:q0 + 512], ym[:], reads=[("ymt", qi_box[0] % 2)])
                qi_box[0] += 1
    cx.barrier()


def stage_merge(cx, nc, T, PQ, PS):
    with ExitStack() as es:
        def sb(name, shape, dt=F32):
            return es.enter_context(nc.sbuf_tensor(name, shape, dt))
        stg = [sb("gstg%d" % i, [128, 2048]) for i in range(2)]
        prw = load_w_bf16(cx, nc, es, "prw", T["p_rwkv"], 1024, 2048, stg, "gstg")
        pml = load_w_bf16(cx, nc, es, "pml", T["p_mla"], 2048, 2048, stg, "gstg")
        bg = sb("bgate", [128, 32])
        cx.dma("sp", bg[:], T["bgate_fm"], writes=["bg"])
        yr = sb("yrblk", [128, 8, 512], BF16)
        ym = sb("ymblk", [128, 16, 512], BF16)
        gr = sb("grblk", [128, 512])
        gm = sb("gmblk", [128, 512])
        mg = [sb("mgblk%d" % i, [128, 512], BF16) for i in range(2)]
        cx.barrier()
        kk = 0
        for tb in range(4):
            t0 = tb * 512
            cx.dma("sp", yr[:], T["YRT"][:, t0:t0 + 512].rearrange("(c p) t -> p c t", p=128), writes=["yr"])
            cx.dma("sp", ym[:], T["YMT"][:, t0:t0 + 512].rearrange("(c p) t -> p c t", p=128), writes=["ym"])
            for dc in range(16):
                cx.dma("sp", gr[:], T["PRT"][R_GR + dc * 128:R_GR + dc * 128 + 128, 1 + OWN0 + t0:1 + OWN0 + t0 + 512],
                       writes=["gr"])
                cx.dma("sp", gm[:], T["PRT"][R_GM + dc * 128:R_GM + dc * 128 + 128, 1 + OWN0 + t0:1 + OWN0 + t0 + 512],
                       writes=["gm"])
                cx.op("act", lambda e: e.activation(out=gr[:], in_=gr[:], func=AF.Sigmoid, bias=bg[:, dc:dc + 1]),
                      reads=["gr", "bg"], writes=["gr"])
                cx.op("act", lambda e: e.activation(out=gm[:], in_=gm[:], func=AF.Sigmoid, bias=bg[:, 16 + dc:17 + dc]),
                      reads=["gm", "bg"], writes=["gm"])
                pa, pak = PS[kk % 4], ("ps", kk % 4)
                pb, pbk = PS[4 + kk % 4], ("ps", 4 + kk % 4)
                kk += 1
                for c in range(8):
                    cx.op("pe", lambda e: e.matmul(out=pa[:, 0:512], lhsT=prw[:, c, dc * 128:dc * 128 + 128], rhs=yr[:, c, :],
                                                   start=(c == 0), stop=(c == 7)), reads=["prw", "yr"], writes=[pak], last=(c == 7))
                for c in range(16):
                    cx.op("pe", lambda e: e.matmul(out=pb[:, 0:512], lhsT=pml[:, c, dc * 128:dc * 128 + 128], rhs=ym[:, c, :],
                                                   start=(c == 0), stop=(c == 15)), reads=["pml", "ym"], writes=[pbk], last=(c == 15))
                cx.op("dve", lambda e: e.tensor_tensor(out=gr[:], in0=gr[:], in1=pa[:, 0:512], op=ALU.mult),
                      reads=["gr", pak], writes=["gr"])
                cx.op("dve", lambda e: e.tensor_tensor(out=gm[:], in0=gm[:], in1=pb[:, 0:512], op=ALU.mult),
                      reads=["gm", pbk], writes=["gm"])
                m_ = mg[dc % 2]
                cx.op("dve", lambda e: e.tensor_tensor(out=m_[:], in0=gr[:], in1=gm[:], op=ALU.add),
                      reads=["gr", "gm"], writes=[("mg", dc % 2)])
                cx.dma("sp", T["MGT"][dc * 128:dc * 128 + 128, t0:t0 + 512], m_[:], reads=[("mg", dc % 2)])
    cx.barrier()
    with ExitStack() as es:
        def sb(name, shape, dt=F32):
            return es.enter_context(nc.sbuf_tensor(name, shape, dt))
        stg = [sb("hstg%d" % i, [128, 2048]) for i in range(2)]
        wo = load_w_bf16(cx, nc, es, "wo", T["w_o"], 2048, 2048, stg, "hstg")
        mt = sb("mgt", [128, 16, 128], BF16)
        xt = sb("xres", [128, 2048])
        cx.barrier()
        for ti in range(16):
            t0 = ti * 128
            cx.dma("sp", mt[:], T["MGT"][:, t0:t0 + 128].rearrange("(c p) t -> p c t", p=128), writes=["mt"])
            cx.dma("sp", xt[:], T["xp"][OWN0 + t0:OWN0 + t0 + 128, :], writes=["xt"])
            for nb in range(4):
                pb, pbk = PS[(ti * 4 + nb) % 8], ("ps", (ti * 4 + nb) % 8)
                for c in range(16):
                    cx.op("pe", lambda e: e.matmul(out=pb[:, 0:512], lhsT=mt[:, c, :], rhs=wo[:, c, nb * 512:nb * 512 + 512],
                                                   start=(c == 0), stop=(c == 15)), reads=["mt", "wo"], writes=[pbk], last=(c == 15))
                cx.op("dve", lambda e: e.tensor_tensor(out=xt[:, nb * 512:nb * 512 + 512], in0=xt[:, nb * 512:nb * 512 + 512],
                                                       in1=pb[:, 0:512], op=ALU.add), reads=["xt", pbk], writes=["xt"])
            cx.dma("sp", T["H2"][t0:t0 + 128, :], xt[:], reads=["xt"])
    cx.barrier()


def stage_peer(cx, nc, T, PQ, PS):
    with ExitStack() as es0:
        def sb0(name, shape, dt=F32):
            return es0.enter_context(nc.sbuf_tensor(name, shape, dt))
        eidi_all = sb0("eidi_all", [128, 16, 128], I32)
        gate_all = sb0("gate_all", [128, 16, 128])
        ident = sb0("identE", [128, 128])
        gf = sb0("gffn", [128, 2048])
        epsc = sb0("epscE", [128, 1])
        cx.dma("sp", ident[:], T["ident"], writes=["ident"])
        cx.dma("sp", gf[:], T["gffn_b"], writes=["gf"])
        cx.op("dve", lambda e: e.memset(epsc[:], EPS), writes=["epsc"])
        with ExitStack() as es:
            def sb(name, shape, dt=F32):
                return es.enter_context(nc.sbuf_tensor(name, shape, dt))
            stg = [sb("estg%d" % i, [128, 2048]) for i in range(2)]
            wq = load_w_bf16(cx, nc, es, "wqp", T["peer_wq"], 2048, 2048, stg, "estg")
            cx.barrier()
            keysT = sb("keysT", [128, 16, 128])
            iota = sb("iotaE", [128, 256])
            cx.dma("sp", iota[:], T["iota256"], writes=["iota"])
            for g in range(16):
                cx.dma("sp", stg[0][:, g * 128:g * 128 + 128], T["peer_keys"][g], writes=["estg0"])
            for g in range(16):
                pb, pbk = PS[g % 2], ("ps", g % 2)
                cx.op("pe", lambda e: e.transpose(out=pb[:, 0:128], in_=stg[0][:, g * 128:g * 128 + 128], identity=ident[:, :]),
                      reads=["estg0", "ident"], writes=[pbk])
                cx.op("act", lambda e: e.copy(out=keysT[:, g, :], in_=pb[:, 0:128]), reads=[pbk], writes=["keysT"])
            cx.barrier()
            junk = stg[1][:]
            JK = "junkA"
            h2 = sb("h2", [128, 2048])
            hn = sb("hn", [128, 2048])
            hnT = sb("hnT", [128, 16, 128], BF16)
            qT = sb("qTp", [128, 16, 128])
            sc = sb("scp", [128, 16, 128])
            sc2 = sb("scp2", [128, 128])
            tops = sb("tops", [128, 16, 16])
            topi = sb("topi", [128, 16, 16])
            tiu_all = sb("tiu_all", [128, 16, 16], U32)
            piu = sb("piu", [128, 8, 16], U32)
            sc2_all = sb("sc2_all", [128, 16, 128])
            cand = sb("cand", [128, 8, 16, 16])
            cidx = sb("cidx", [128, 8, 16, 16])
            best = sb("best", [128, 8, 16])
            pos = sb("pos", [128, 8, 16])
            eid = sb("eid", [128, 8, 16])
            gate = sb("gate", [128, 8, 16])
            gsum = sb("gsum", [128, 8])
            ssq = sb("ssqE", [128, 2])
            NEG = -1e30
            cand2 = sc[:].rearrange("p (h s) n -> p h (s n)", s=2)
            eqb = junk.rearrange("p (h n) -> p h n", h=8)
            for ti in range(16):
                t0 = ti * 128
                cx.dma("sp", h2[:], T["H2"][t0:t0 + 128, :], writes=["h2"])
                cx.op("act", lambda e: e.activation(out=junk, in_=h2[:], func=AF.Square, accum_out=ssq[:, 0:1]),
                      reads=["h2"], writes=[JK, "ssq0"] + [("jk", i_) for i_ in range(8)])
                cx.op("act", lambda e: e.activation(out=ssq[:, 0:1], in_=ssq[:, 0:1], func=AF.Sqrt, scale=1.0 / D,
                                                    bias=epsc[:, 0:1]), reads=["ssq0", "epsc"], writes=["ssq0"])
                cx.op("dve", lambda e: e.reciprocal(out=ssq[:, 0:1], in_=ssq[:, 0:1]), reads=["ssq0"], writes=["ssq0"])
                cx.op("dve", lambda e: e.scalar_tensor_tensor(out=hn[:], in0=h2[:], scalar=ssq[:, 0:1], in1=gf[:],
                                                              op0=ALU.mult, op1=ALU.mult), reads=["h2", "ssq0", "gf"], writes=["hn"])
                for cg in range(4):
                    pb, pbk = PS[cg % 2], ("ps", cg % 2)
                    for ci in range(4):
                        c = cg * 4 + ci
                        cx.op("pe", lambda e: e.transpose(out=pb[:, ci * 128:ci * 128 + 128], in_=hn[:, c * 128:(c + 1) * 128],
                                                          identity=ident[:, :]), reads=["hn", "ident"], writes=[pbk], last=(ci == 3))
                    cx.op("act", lambda e: e.copy(out=hnT[:, cg * 4:cg * 4 + 4, :],
                                                  in_=pb[:, 0:512].rearrange("p (c t) -> p c t", c=4)), reads=[pbk], writes=["hnT"])
                for g in range(16):
                    pb, pbk = PS[2 + g % 2], ("ps", 2 + g % 2)
                    for c in range(16):
                        cx.op("pe", lambda e: e.matmul(out=pb[:, 0:128], lhsT=wq[:, c, g * 128:g * 128 + 128], rhs=hnT[:, c, :],
                                                       start=(c == 0), stop=(c == 15)), reads=["wqp", "hnT"], writes=[pbk], last=(c == 15))
                    cx.op("act", lambda e: e.copy(out=qT[:, g, :], in_=pb[:, 0:128]), reads=[pbk], writes=[("qT", g)])
                for g in range(16):
                    pb, pbk = PS[g % 2], ("ps", g % 2)
                    cx.op("pe", lambda e: e.matmul(out=pb[:, 0:128], lhsT=qT[:, g, :], rhs=keysT[:, g, :], start=True, stop=True),
                          reads=[("qT", g), "keysT"], writes=[pbk])
                    cx.op("act", lambda e: e.copy(out=sc[:, g, :], in_=pb[:, 0:128]), reads=[pbk], writes=[("sc", g)])
                for g in range(16):
                    cx.op("dve", lambda e: e.max(out=tops[:, g, 0:8], in_=sc[:, g, :]), reads=[("sc", g)], writes=[("tops", g)])
                for g in range(16):
                    cx.op("dve", lambda e: e.max_index(out=tiu_all[:, g, 0:8], in_max=tops[:, g, 0:8], in_values=sc[:, g, :]),
                          reads=[("sc", g), ("tops", g)], writes=[("tiu", g)])
                for g in range(16):
                    cx.op("dve", lambda e: e.match_replace(out=sc2_all[:, g, :], in_to_replace=tops[:, g, 0:8], in_values=sc[:, g, :],
                                                           imm_value=NEG), reads=[("sc", g), ("tops", g)], writes=[("sc2", g)])
                for g in range(16):
                    cx.op("dve", lambda e: e.max(out=tops[:, g, 8:16], in_=sc2_all[:, g, :]), reads=[("sc2", g)], writes=[("tops", g)])
                for g in range(16):
                    cx.op("dve", lambda e: e.max_index(out=tiu_all[:, g, 8:16], in_max=tops[:, g, 8:16], in_values=sc2_all[:, g, :]),
                          reads=[("sc2", g), ("tops", g)], writes=[("tiu", g)])
                cx.op("dve", lambda e: e.tensor_copy(out=topi[:], in_=tiu_all[:]), reads=[("tiu", g) for g in range(16)],
                      writes=[("topi", g) for g in range(16)])
                tkeys = [("tops", g) for g in range(16)]
                ikeys = [("topi", g) for g in range(16)]
                ts4 = tops[:].rearrange("p (h s) k -> p h s k", s=2)
                ti4 = topi[:].rearrange("p (h s) k -> p h s k", s=2)
                cx.op("dve", lambda e: e.tensor_tensor(out=cand[:], in0=ts4[:, :, 0, :].unsqueeze(3).to_broadcast([128, 8, 16, 16]),
                                                       in1=ts4[:, :, 1, :].unsqueeze(2).to_broadcast([128, 8, 16, 16]), op=ALU.add),
                      reads=tkeys, writes=["cand"])
                cx.op("dve", lambda e: e.tensor_scalar(out=eid[:], in0=ti4[:, :, 0, :], scalar1=128.0, scalar2=None, op0=ALU.mult),
                      reads=ikeys, writes=["eid"] + [("eid", hh, k) for hh in range(8) for k in range(16)])
                cx.op("dve", lambda e: e.tensor_tensor(out=cidx[:], in0=eid[:].unsqueeze(3).to_broadcast([128, 8, 16, 16]),
                                                       in1=ti4[:, :, 1, :].unsqueeze(2).to_broadcast([128, 8, 16, 16]),
                                                       op=ALU.add), reads=ikeys + ["eid"], writes=["cidx"])
                c3 = cand[:].rearrange("p h a b -> p h (a b)")
                i3 = cidx[:].rearrange("p h a b -> p h (a b)")
                for hh in range(8):
                    cx.op("dve", lambda e: e.max(out=best[:, hh, 0:8], in_=c3[:, hh, :]), reads=["cand"], writes=[("best", hh)])
                for hh in range(8):
                    cx.op("dve", lambda e: e.max_index(out=piu[:, hh, 0:8], in_max=best[:, hh, 0:8], in_values=c3[:, hh, :]),
                          reads=["cand", ("best", hh)], writes=[("piu", hh)])
                for hh in range(8):
                    cx.op("dve", lambda e: e.match_replace(out=cand2[:, hh, :], in_to_replace=best[:, hh, 0:8], in_values=c3[:, hh, :],
                                                           imm_value=NEG), reads=["cand", ("best", hh)],
                          writes=[("sc", 2 * hh), ("sc", 2 * hh + 1)])
                for hh in range(8):
                    cx.op("dve", lambda e: e.max(out=best[:, hh, 8:16], in_=cand2[:, hh, :]),
                          reads=[("sc", 2 * hh), ("sc", 2 * hh + 1)], writes=[("best", hh)])
                for hh in range(8):
                    cx.op("dve", lambda e: e.max_index(out=piu[:, hh, 8:16], in_max=best[:, hh, 8:16], in_values=cand2[:, hh, :]),
                          reads=[("sc", 2 * hh), ("sc", 2 * hh + 1), ("best", hh)], writes=[("piu", hh)])
                cx.op("dve", lambda e: e.tensor_copy(out=pos[:], in_=piu[:]), reads=[("piu", hh) for hh in range(8)],
                      writes=[("pos", hh) for hh in range(8)])
                bkeys = [("best", hh) for hh in range(8)]
                pkeys = [("pos", hh) for hh in range(8)]
                for hh in range(8):
                    for k in range(16):
                        js_ = (hh * 16 + k) % 8
                        cx.op("dve", lambda e: e.scalar_tensor_tensor(out=junk[:, js_ * 256:js_ * 256 + 256], in0=iota[:, :],
                                                                      scalar=pos[:, hh, k:k + 1],
                                                                      in1=i3[:, hh, :], op0=ALU.is_equal, op1=ALU.mult,
                                                                      accum_out=eid[:, hh, k:k + 1]),
                              reads=["iota", "cidx", ("pos", hh)], writes=[("jk", js_), ("eid", hh, k)])
                cx.op("dve", lambda e: e.tensor_copy(out=eidi_all[:, ti, :], in_=eid[:].rearrange("p h k -> p (h k)")),
                      reads=[("eid", hh, k) for hh in range(8) for k in range(16)], writes=[("eidi", ti)])
                cx.op("dve", lambda e: e.tensor_tensor(out=gate[:], in0=best[:], in1=best[:, :, 0:1].to_broadcast([128, 8, 16]),
                                                       op=ALU.subtract), reads=bkeys, writes=["gate"])
                cx.op("act", lambda e: e.activation(out=gate[:], in_=gate[:], func=AF.Exp), reads=["gate"], writes=["gate"])
                cx.op("dve", lambda e: e.tensor_reduce(out=gsum[:], in_=gate[:], axis=AX.X, op=ALU.add), reads=["gate"], writes=["gsum"])
                cx.op("dve", lambda e: e.reciprocal(out=gsum[:], in_=gsum[:]), reads=["gsum"], writes=["gsum"])
                cx.op("dve", lambda e: e.tensor_tensor(out=gate_all[:, ti, :].rearrange("p (h k) -> p h k", h=8), in0=gate[:],
                                                       in1=gsum[:].unsqueeze(2).to_broadcast([128, 8, 16]),
                                                       op=ALU.mult), reads=["gate", "gsum"], writes=[("gate_all", ti)])

        cx.barrier()
        with ExitStack() as es:
            def sb(name, shape, dt=F32):
                return es.enter_context(nc.sbuf_tensor(name, shape, dt))
            NR = 14
            rows = [sb("rows%d" % i, [128, 4096], BF16)[:] for i in range(NR)]
            gfin = sb("gfin", [128, 2048])
            identb = sb("identEb", [128, 128], BF16)
            h2 = sb("h2b", [128, 2048])
            hnb = sb("hnb", [128, 2048], BF16)
            junkb = sb("junkb", [128, 2048], BF16)
            score = sb("score", [128, 128])
            coef = sb("coef", [128, 128])
            ssq = sb("ssqB", [128, 2])
            dg = [sb("dg%d" % i, [128, 128], BF16) for i in range(8)]
            cx.dma("sp", gfin[:], T["gfin_b"], writes=["gfin"])
            cx.op("dve", lambda e: e.tensor_copy(out=identb[:], in_=ident[:]), reads=["ident"], writes=["identb"])
            junk = rows[NR - 1].bitcast(F32)
            JK = ("rows", NR - 1)
            NG = NR - 1
            for ti in range(16):
                t0 = ti * 128
                cx.dma("sp", h2[:], T["H2"][t0:t0 + 128, :], writes=["h2"])
                cx.op("act", lambda e: e.activation(out=junk, in_=h2[:], func=AF.Square, accum_out=ssq[:, 0:1]),
                      reads=["h2"], writes=[JK, "ssq0"])
                cx.op("act", lambda e: e.activation(out=ssq[:, 0:1], in_=ssq[:, 0:1], func=AF.Sqrt, scale=1.0 / D,
                                                    bias=epsc[:, 0:1]), reads=["ssq0", "epsc"], writes=["ssq0"])
                cx.op("dve", lambda e: e.reciprocal(out=ssq[:, 0:1], in_=ssq[:, 0:1]), reads=["ssq0"], writes=["ssq0"])
                cx.op("dve", lambda e: e.scalar_tensor_tensor(out=hnb[:], in0=h2[:], scalar=ssq[:, 0:1], in1=gf[:],
                                                              op0=ALU.mult, op1=ALU.mult), reads=["h2", "ssq0", "gf"], writes=["hnb"])
                for g in range(32):
                    js = list(range(g * 4, g * 4 + 4))
                    for j in js:
                        rb = rows[j % NG]
                        cx.dma("pool", rb, T["UVB"], indirect=bass.IndirectOffsetOnAxis(ap=eidi_all[:, ti, j:j + 1], axis=0),
                               writes=[("rows", j % NG)])
                        cx.op("dve", lambda e: e.scalar_tensor_tensor(out=junkb[:], in0=rb[:, 0:2048], scalar=1.0, in1=hnb[:],
                                                                      op0=ALU.mult, op1=ALU.mult, accum_out=score[:, j:j + 1]),
                              reads=[("rows", j % NG), "hnb"], writes=["junkb", ("score", g)])
                    cx.op("act", lambda e: e.activation(out=coef[:, g * 4:g * 4 + 4], in_=score[:, g * 4:g * 4 + 4], func=AF.Gelu),
                          reads=[("score", g)], writes=[("coef", g)])
                    cx.op("dve", lambda e: e.tensor_tensor(out=coef[:, g * 4:g * 4 + 4], in0=coef[:, g * 4:g * 4 + 4],
                                                           in1=gate_all[:, ti, g * 4:g * 4 + 4], op=ALU.mult),
                          reads=[("coef", g)], writes=[("coef", g)])
                    for j in js:
                        rb = rows[j % NG]
                        d_ = dg[j % 8]
                        cx.op("act", lambda e: e.activation(out=d_[:], in_=identb[:], func=AF.Copy, scale=coef[:, j:j + 1]),
                              reads=[("coef", g), "identb"], writes=[("dg", j % 8)])
                        for nb in range(4):
                            cx.op("pe", lambda e: e.matmul(out=PS[4 + nb][:, 0:512], lhsT=d_[:],
                                                           rhs=rb[:, 2048 + nb * 512:2048 + nb * 512 + 512],
                                                           start=(j == 0), stop=(j == 127)),
                                  reads=[("dg", j % 8), ("rows", j % NG)], writes=[("ps", 4 + nb)], last=(nb == 3))
                for nb in range(4):
                    cx.op("dve", lambda e: e.tensor_tensor(out=h2[:, nb * 512:nb * 512 + 512], in0=h2[:, nb * 512:nb * 512 + 512],
                                                           in1=PS[4 + nb][:, 0:512], op=ALU.add), reads=["h2", ("ps", 4 + nb)], writes=["h2"])
                cx.op("act", lambda e: e.activation(out=junk, in_=h2[:], func=AF.Square, accum_out=ssq[:, 1:2]),
                      reads=["h2"], writes=[JK, "ssq1"])
                cx.op("act", lambda e: e.activation(out=ssq[:, 1:2], in_=ssq[:, 1:2], func=AF.Sqrt, scale=1.0 / D,
                                                    bias=epsc[:, 0:1]), reads=["ssq1", "epsc"], writes=["ssq1"])
                cx.op("dve", lambda e: e.reciprocal(out=ssq[:, 1:2], in_=ssq[:, 1:2]), reads=["ssq1"], writes=["ssq1"])
                cx.op("dve", lambda e: e.scalar_tensor_tensor(out=junk, in0=h2[:], scalar=ssq[:, 1:2], in1=gfin[:],
                                                              op0=ALU.mult, op1=ALU.mult), reads=["h2", "ssq1", "gfin"], writes=[JK])
                cx.dma("sp", T["out"][t0:t0 + 128, :], junk, reads=[JK])
    cx.barrier()


_NC = None


def kernel(**inputs):
    global _NC
    inputs = {k: np.asarray(v) for k, v in inputs.items()}
    if _NC is None:
        _NC = build()
    in_maps = []
    for b in range(4):
        for s_ in range(2):
            m = prep_core(inputs, b, s_)
            in_maps.append({k: np.ascontiguousarray(v, dtype=np.float32) for k, v in m.items()})
    res = run_bass_kernel_spmd(_NC, in_maps, core_ids=list(range(8)))
    out = np.zeros((4, 4096, D), np.float32)
    for b in range(4):
        for s_ in range(2):
            o = np.asarray(res.results[b * 2 + s_]["out"])
            if s_ == 0:
                out[b, 0:2048] = o
            else:
                out[b, 2048:4096] = o[::-1]
    return out
```

```python
import numpy as np
from contextlib import ExitStack
import concourse.bass as bass
import concourse.mybir as mybir
from concourse.bass_utils import run_bass_kernel_spmd

F32 = mybir.dt.float32
BF16 = mybir.dt.bfloat16
I32 = mybir.dt.int32
U32 = mybir.dt.uint32
AF = mybir.ActivationFunctionType
ALU = mybir.AluOpType
AX = mybir.AxisListType

D = 2048
NTOK = 4224
OWN0, OWN1 = 64, 2112
NOWN = 2048
HALF = 2112
NCH = 66
C = 64
RW = 3488
EPS = 1e-6
C0 = float(np.exp(-0.5))

R_R, R_K, R_V, R_GD, R_WDF, R_WDB, R_ADF, R_ADB = 0, 1024, 2048, 3072, 3232, 3296, 3360, 3424
R_QD, R_KVD, R_KR, R_KRR, R_GR, R_GM = 3488, 4000, 4512, 4576, 4640, 6688
NROWS = 8736


class Ctx:
    def __init__(self, nc, es):
        self.nc = nc
        self.E = {"pe": nc.tensor, "dve": nc.vector, "act": nc.scalar, "pool": nc.gpsimd, "sp": nc.sync}
        self.psem = {k: es.enter_context(nc.semaphore("prog_" + k)) for k in ["pe", "dve", "act", "pool"]}
        self.pcnt = {k: 0 for k in self.psem}
        self.seen = {}
        self.buf = {}
        self.dq = {}
        for q in ["sp", "pool", "act"]:
            sems = [es.enter_context(nc.semaphore("dq_%s_%d" % (q, i))) for i in range(20)]
            self.dq[q] = {"sems": sems, "cnt": [0] * len(sems), "i": 0}
        self.ninst = 0
        self.pend = {}

    def wait(self, eng, tok):
        key, sem, val = tok
        k = (eng, key)
        if self.seen.get(k, 0) >= val:
            return
        self.E[eng].wait_ge(sem, val)
        self.seen[k] = val

    def _deps(self, reads, writes):
        deps = []
        for k in reads:
            st = self.buf.get(k)
            if st and st["w"]:
                deps.append(st["w"])
        for k in writes:
            st = self.buf.get(k)
            if st:
                if st["w"]:
                    deps.append(st["w"])
                deps.extend(st["r"].values())
        return deps

    def _commit(self, tok, reads, writes):
        for k in reads:
            st = self.buf.setdefault(k, {"w": None, "r": {}})
            old = st["r"].get(tok[0])
            if old is None or old[2] < tok[2]:
                st["r"][tok[0]] = tok
        for k in writes:
            self.buf[k] = {"w": tok, "r": {}}

    def op(self, eng, fn, reads=(), writes=(), last=True):
        for d in self._deps(reads, writes):
            self.wait(eng, d)
        ins = fn(self.E[eng])
        self.ninst += 1
        pend = self.pend.setdefault(eng, [])
        pend.append((list(reads), list(writes)))
        if not last:
            return None
        self.pcnt[eng] += 1
        ins.then_inc(self.psem[eng], 1)
        tok = (eng, self.psem[eng], self.pcnt[eng])
        for r, w in pend:
            self._commit(tok, r, w)
        del pend[:]
        return tok

    def dma(self, q, out, in_, reads=(), writes=(), indirect=None, slow=False):
        dq = self.dq[q]
        i = dq["i"]
        dq["i"] = (i + 1) % len(dq["sems"])
        sem = dq["sems"][i]
        key = "dq_%s_%d" % (q, i)
        if dq["cnt"][i] > 0:
            self.wait(q, (key, sem, dq["cnt"][i]))
        for d in self._deps(reads, writes):
            self.wait(q, d)
        if indirect is None:
            ins = self.E[q].dma_start(out=out, in_=in_, allow_slow_non_contiguous=slow)
        else:
            ins = self.E[q].indirect_dma_start(out=out, out_offset=None, in_=in_, in_offset=indirect)
        dq["cnt"][i] += 16
        ins.then_inc(sem, 16)
        tok = (key, sem, dq["cnt"][i])
        self._commit(tok, reads, writes)
        self.ninst += 1
        return tok

    def barrier(self):
        toks = [(k, self.psem[k], self.pcnt[k]) for k in self.psem if self.pcnt[k] > 0]
        for q, dq in self.dq.items():
            for i, sem in enumerate(dq["sems"]):
                if dq["cnt"][i] > 0:
                    toks.append(("dq_%s_%d" % (q, i), sem, dq["cnt"][i]))
        for eng in ["pe", "dve", "act", "pool", "sp"]:
            for t in toks:
                self.wait(eng, t)
        self.buf = {}


def stage_inproj(cx, nc, T, PS):
    with ExitStack() as es:
        def sb(name, shape, dt):
            return es.enter_context(nc.sbuf_tensor(name, shape, dt))
        nT = sb("nT", [128, 16, HALF], BF16)
        xt = [sb("xt%d" % i, [128, D], F32) for i in range(2)]
        xs = [sb("xs%d" % i, [128, D], F32) for i in range(2)]
        ssq = sb("ssq", [128, 40], F32)
        rstd = sb("rstd", [128, 40], F32)
        gS = sb("gS", [128, 16], F32)
        ident = sb("identA", [128, 128], F32)
        wb = [sb("wb%d" % i, [128, 16, 256], BF16) for i in range(2)]
        wrot = sb("wrot", [128, 16, 64], BF16)
        stg = [sb("stg%d" % i, [128, 512], F32) for i in range(4)]
        zero = sb("zeroA", [128, 2], F32)

        cx.dma("sp", ident[:], T["ident"], writes=["ident"])
        cx.dma("sp", gS[:], T["norm_mix_g"], writes=["gS"])
        epsc = sb("epsc", [128, 1], F32)
        cx.op("dve", lambda e: e.memset(epsc[:], EPS), writes=["epsc"])
        cx.op("dve", lambda e: e.memset(zero[:], 0.0), writes=["zero"])
        for r0 in range(0, NROWS, 128):
            m = min(128, NROWS - r0)
            cx.dma("sp", T["PRT"][r0:r0 + m, 0:1], zero[0:m, 0:1], reads=["zero"], slow=True)
            cx.dma("sp", T["PRT"][r0:r0 + m, NTOK + 1:NTOK + 2], zero[0:m, 1:2], reads=["zero"], slow=True)

        units = []
        for u0 in range(0, 3072, 256):
            units.append((T["w_in"][:, u0:u0 + 256], 256, [(0, 128, u0, False), (128, 128, u0 + 128, False)], False))
        units.append((T["w_lora"][:, 0:160], 160, [(0, 128, R_GD, False), (128, 32, R_GD + 128, False)], False))
        units.append((T["w_lora"][:, 160:416], 256, [(0, 64, R_WDF, False), (64, 64, R_WDB, False),
                                                      (128, 64, R_ADF, False), (192, 64, R_ADB, False)], False))
        for u0 in range(0, 512, 256):
            units.append((T["w_in"][:, 4000 + u0:4000 + u0 + 256], 256,
                          [(0, 128, R_KVD + u0, False), (128, 128, R_KVD + u0 + 128, False)], False))
        units.append((T["w_in"][:, 4512:4576], 64, [(0, 64, R_KR, False), (0, 64, R_KRR, True)], False))
        for u0 in range(0, 512, 256):
            units.append((T["w_in"][:, 3488 + u0:3488 + u0 + 256], 256,
                          [(0, 128, R_QD + u0, False), (128, 128, R_QD + u0 + 128, False)], True))
        for u0 in range(0, 4096, 256):
            units.append((T["w_in"][:, 4576 + u0:4576 + u0 + 256], 256,
                          [(0, 128, R_GR + u0, False), (128, 128, R_GR + u0 + 128, False)], True))

        tcb = [sb("tcb%d" % i, [128, 2048], BF16) for i in range(4)]

        def tabconv_gen():
            k = 0
            for ti in range(128):
                for tab, src in enumerate(["peer_u", "peer_v"]):
                    b_ = k % 4
                    cx.dma("pool", tcb[b_][:], T[src][ti * 128:(ti + 1) * 128, :], writes=[("tcb", b_)])
                    cx.dma("act", T["UVB"][ti * 128:(ti + 1) * 128, tab * 2048:(tab + 1) * 2048], tcb[b_][:],
                           reads=[("tcb", b_)])
                    k += 1
                    yield

        tcg = tabconv_gen()
        ev = [0]
        for half in range(2):
            tbase = half * HALF
            for ti in range(HALF // 128 + 1):
                t0 = tbase + ti * 128
                n = min(128, tbase + HALF - t0)
                if n <= 0:
                    continue
                b = ti % 2
                tt = half * 17 + ti
                cx.dma("sp", xt[b][0:n, :], T["xp"][t0:t0 + n, :], writes=[("xt", b)])
                cx.op("act", lambda e: e.activation(out=xs[b][0:n, :], in_=xt[b][0:n, :], func=AF.Square,
                                                    accum_out=ssq[0:n, tt:tt + 1]),
                      reads=[("xt", b)], writes=[("xs", b), ("ssq", tt)])
                cx.op("act", lambda e: e.activation(out=ssq[0:n, tt:tt + 1], in_=ssq[0:n, tt:tt + 1], func=AF.Sqrt,
                                                    scale=1.0 / D, bias=epsc[0:n, 0:1]),
                      reads=[("ssq", tt), "epsc"], writes=[("ssq", tt)])
                cx.op("dve", lambda e: e.reciprocal(out=rstd[0:n, tt:tt + 1], in_=ssq[0:n, tt:tt + 1]),
                      reads=[("ssq", tt)], writes=[("rstd", tt)])
                cx.op("act", lambda e: e.activation(out=xs[b][0:n, :], in_=xt[b][0:n, :], func=AF.Copy,
                                                    scale=rstd[0:n, tt:tt + 1]),
                      reads=[("xt", b), ("rstd", tt)], writes=[("xs", b)])
                for cg in range(4):
                    pb = PS[(tt * 4 + cg) % 2]
                    for ci in range(4):
                        c = cg * 4 + ci
                        cx.op("pe", lambda e: e.transpose(out=pb[:, ci * 128:ci * 128 + n],
                                                          in_=xs[b][0:n, c * 128:(c + 1) * 128], identity=ident[0:n, 0:n]),
                              reads=[("xs", b), "ident"], writes=[("ps", (tt * 4 + cg) % 2, ci)], last=(ci == 3))
                    tl = t0 - tbase
                    cx.op("dve", lambda e: e.tensor_tensor(
                        out=nT[:, cg * 4:cg * 4 + 4, tl:tl + n],
                        in0=pb[:, 0:512].rearrange("p (c t) -> p c t", c=4)[:, :, 0:n],
                        in1=gS[:, cg * 4:cg * 4 + 4].unsqueeze(2).to_broadcast([128, 4, n]), op=ALU.mult),
                        reads=[("ps", (tt * 4 + cg) % 2, i) for i in range(4)] + ["gS"],
                        writes=[("nT", ti)])
            if half == 0:
                blocks = [(0, 64, False)] + [(64 + 512 * i, 512, True) for i in range(4)]
            else:
                blocks = [(HALF + 512 * i, 512, False) for i in range(4)] + [(4160, 64, False)]
            nTreads = [("nT", i) for i in range(17)]
            active = [u for u in units if not (u[3] and half == 1)]

            def issue_load(idx):
                src_, U_, segs_, _ = active[idx]
                wb_ = wb[idx % 2]
                cx.dma("pool", wb_[:, :, 0:U_], src_.rearrange("(c p) u -> p c u", p=128), writes=[("wb", idx % 2)])
                if any(s_[3] for s_ in segs_):
                    cx.op("pool", lambda e: e.tensor_scalar(out=wrot[:, :, 0:32], in0=wb_[:, :, 32:64], scalar1=-1.0,
                                                            scalar2=None, op0=ALU.mult),
                          reads=[("wb", idx % 2)], writes=["wrot"])
                    cx.op("pool", lambda e: e.tensor_copy(out=wrot[:, :, 32:64], in_=wb_[:, :, 0:32]),
                          reads=[("wb", idx % 2)], writes=["wrot"])

            issue_load(0)
            for ui, (src, U, segs, own_only) in enumerate(active):
                if ui + 1 < len(active):
                    issue_load(ui + 1)
                wbuf = ui % 2
                for _ in range(5):
                    next(tcg, None)
                for (off, M, drow, rot) in segs:
                    for (t0, n, isown) in blocks:
                        if own_only and not isown:
                            continue
                        k = ev[0]
                        ev[0] += 1
                        pbk = 2 + (k % 4)
                        pb = PS[pbk]
                        tl = t0 - tbase
                        for c in range(16):
                            lhsT = wrot[:, c, 0:64] if rot else wb[wbuf][:, c, off:off + M]
                            cx.op("pe", lambda e: e.matmul(out=pb[0:M, 0:n], lhsT=lhsT, rhs=nT[:, c, tl:tl + n],
                                                           start=(c == 0), stop=(c == 15)),
                                  reads=[("wb", wbuf), "wrot"] + (nTreads if c == 0 else []),
                                  writes=[("psb", pbk)], last=(c == 15))
                        sg = stg[k % 4]
                        if k % 2 == 0:
                            cx.op("act", lambda e: e.copy(out=sg[0:M, 0:n], in_=pb[0:M, 0:n]),
                                  reads=[("psb", pbk)], writes=[("stg", k % 4)])
                        else:
                            cx.op("dve", lambda e: e.tensor_copy(out=sg[0:M, 0:n], in_=pb[0:M, 0:n]),
                                  reads=[("psb", pbk)], writes=[("stg", k % 4)])
                        cx.dma("sp", T["PRT"][drow:drow + M, 1 + t0:1 + t0 + n], sg[0:M, 0:n],
                               reads=[("stg", k % 4)])
        for _ in tcg:
            pass
    cx.barrier()


IN_SPECS = [
    ("xp", [NTOK, D]), ("ident", [128, 128]),
    ("norm_mix_g", [128, 16]), ("w_in", [D, 8672]), ("w_lora", [D, 416]), ("shiftc_fm", [128, 28, 3]), ("ones64", [64, 64]), ("rmask", [64, 16, 64]),
    ("maskG0", [128, 128]), ("maskG1", [128, 128]), ("maskN0", [64, 64]), ("maskN1", [64, 64]),
    ("wup0", [65, 1024]), ("wup1", [65, 1024]), ("aup0", [65, 1024]), ("aup1", [65, 1024]),
    ("kk_fm", [64, 16]), ("ka_fm", [64, 16]), ("rk_fm", [64, 16]),
    ("lng_b", [128, 1024]), ("lnb_b", [128, 1024]), ("g_up", [160, 1024]), ("invf", [64, 1]),
    ("gq_fm", [128, 4]), ("gkv_fm", [128, 4]), ("valid_tm", [128, 33]), ("pos64", [64, NTOK]),
    ("w_uq", [512, 3072]), ("w_ukv", [512, 4096]), ("p_rwkv", [1024, 2048]), ("p_mla", [2048, 2048]),
    ("w_o", [2048, 2048]), ("bgate_fm", [128, 32]), ("peer_wq", [2048, 2048]), ("peer_keys", [16, 128, 128]),
    ("peer_u", [16384, 2048]), ("peer_v", [16384, 2048]), ("gffn_b", [128, 2048]), ("gfin_b", [128, 2048]), ("iota256", [128, 256]),
]


def build(upto="all", debug_out=()):
    nc = bass.Bass("TRN2", target_bir_lowering=False)
    T = {}
    for name, shape in IN_SPECS:
        T[name] = nc.dram_tensor(name, shape, F32, kind="ExternalInput").ap()
    def scratch(name, shape, dt=F32):
        kind = "ExternalOutput" if name in debug_out else "Internal"
        T[name] = nc.dram_tensor(name, shape, dt, kind=kind).ap()
    scratch("PRT", [NROWS, NTOK + 2])
    scratch("XSQ", [3, NCH, 64, 16, 64])
    scratch("XSL", [416, NTOK])
    scratch("YF", [NOWN, 1040])
    scratch("YB", [NOWN, 1040])
    scratch("VTM", [NOWN, 1024])
    scratch("YRT", [1024, NOWN], BF16)
    scratch("YMT", [2048, NOWN], BF16)
    scratch("MGT", [2048, NOWN], BF16)
    scratch("H2", [NOWN, D])
    scratch("UVB", [16384, 4096], BF16)
    T["out"] = nc.dram_tensor("out", [NOWN, D], F32, kind="ExternalOutput").ap()
    with ExitStack() as es:
        PQ = [es.enter_context(nc.psum_tensor("pq%d" % i, [128, 1024], F32)) for i in range(4)]
        PS = [PQ[i // 2][:, (i % 2) * 512:(i % 2) * 512 + 512] for i in range(8)]
        cx = Ctx(nc, es)
        stage_inproj(cx, nc, T, PS)
        if upto == "inproj":
            print("instructions:", cx.ninst)
            return nc
        stage_shift(cx, nc, T)
        stage_rwkv(cx, nc, T, PQ, PS)
        if upto == "rwkv":
            print("instructions:", cx.ninst)
            return nc
        stage_post(cx, nc, T, PQ)
        stage_mla(cx, nc, T, PQ, PS)
        if upto == "mla":
            print("instructions:", cx.ninst)
            return nc
        stage_merge(cx, nc, T, PQ, PS)
        stage_peer(cx, nc, T, PQ, PS)
        print("instructions:", cx.ninst)
    return nc


def prep_core(inputs, b, s):
    x = inputs["x"]
    meta = inputs["meta_tokens"]
    xp = np.zeros((NTOK, D), np.float32)
    posv = np.zeros((1, NTOK), np.float32)
    valid = np.zeros((1, NTOK), np.float32)
    if s == 0:
        xp[48:64] = meta
        xp[64:64 + 4096] = x[b]
        posv[0, 48:64] = np.arange(16)
        posv[0, 64:64 + 4096] = 16 + np.arange(4096)
        valid[0, 48:64 + 4096] = 1
    else:
        xp[64:64 + 4096] = x[b, ::-1]
        xp[4160:4176] = meta[::-1]
        posv[0, 64:64 + 4096] = 16 + np.arange(4095, -1, -1)
        posv[0, 4160:4176] = np.arange(15, -1, -1)
        valid[0, 64:4176] = 1
    w_in = inputs["w_in"][0]
    sc = inputs["shift_c"][0]
    lo = R_GD
    if s == 0:
        order = [(R_GD, 160), (R_WDF, 64), (R_WDB, 64), (R_ADF, 64), (R_ADB, 64)]
    else:
        order = [(R_GD, 160), (R_WDB, 64), (R_WDF, 64), (R_ADB, 64), (R_ADF, 64)]
    cols = np.concatenate([np.arange(a, a + n) for a, n in order])
    w_lora = np.ascontiguousarray(w_in[:, cols])
    shiftc = sc.copy()
    shiftc[:, lo:RW] = sc[:, cols]
    if s == 1:
        shiftc = shiftc[::-1]
    m = {
        "xp": xp, "posv": posv, "valid": valid, "ident": np.eye(128, dtype=np.float32),
        "norm_mix_g": np.ascontiguousarray(inputs["norm_mix_g"][0].reshape(16, 128).T), "w_in": w_in, "w_lora": w_lora,
    }
    scp = np.zeros((3, 28 * 128), np.float32)
    scp[:, :RW] = shiftc
    m["shiftc_fm"] = np.ascontiguousarray(scp.reshape(3, 28, 128).transpose(2, 1, 0))
    m["ones64"] = np.ones((64, 64), np.float32)
    rm = np.ones((64, 16, 64), np.float32)
    rm[:, :, 0] = 0
    m["rmask"] = rm
    tt = np.arange(64)
    for di in range(2):
        if di == 0:
            strict = (tt[:, None] < tt[None, :]); incl = (tt[:, None] <= tt[None, :])
        else:
            strict = (tt[:, None] > tt[None, :]); incl = (tt[:, None] >= tt[None, :])
        m["maskG%d" % di] = np.block([[strict, incl], [strict, incl]]).astype(np.float32)
        m["maskN%d" % di] = np.ascontiguousarray(strict.T).astype(np.float32)
        d = di if s == 0 else 1 - di
        m["wup%d" % di] = np.concatenate([inputs["w_up"][0, d], inputs["w0"][0, d][None]], 0)
        m["aup%d" % di] = np.concatenate([inputs["a_up"][0, d], inputs["a0"][0, d][None]], 0)
    m["kk_fm"] = np.ascontiguousarray(inputs["k_k"][0].reshape(16, 64).T)
    m["ka_fm"] = np.ascontiguousarray(inputs["k_a"][0].reshape(16, 64).T)
    m["rk_fm"] = np.ascontiguousarray(inputs["r_k"][0].T)
    rep = lambda v, n: np.ascontiguousarray(np.broadcast_to(v[None, :], (n, v.shape[0])))
    m["lng_b"] = rep(inputs["ln_x_g"][0], 128)
    m["lnb_b"] = rep(inputs["ln_x_b"][0], 128)
    m["g_up"] = inputs["g_up"][0]
    invf = (10000.0 ** (-np.arange(0, 64, 2, dtype=np.float32) / 64)).astype(np.float32)
    m["invf"] = np.concatenate([invf, invf])[:, None]
    m["gq_fm"] = np.ascontiguousarray(inputs["q_norm_g"][0].reshape(4, 128).T)
    m["gkv_fm"] = np.ascontiguousarray(inputs["kv_norm_g"][0].reshape(4, 128).T)
    m["valid_tm"] = np.ascontiguousarray(valid[0].reshape(33, 128).T)
    m["pos64"] = rep(posv[0], 64)
    for k in ["w_uq", "w_ukv", "p_rwkv", "p_mla", "w_o", "peer_wq", "peer_u", "peer_v"]:
        m[k] = inputs[k][0]
    m["bgate_fm"] = np.ascontiguousarray(inputs["b_gate"][0].reshape(32, 128).T)
    m["peer_keys"] = inputs["peer_keys"][0].reshape(16, 128, 128)
    m["gffn_b"] = rep(inputs["norm_ffn_g"][0], 128)
    m["gfin_b"] = rep(inputs["final_norm_g"], 128)
    m["iota256"] = np.ascontiguousarray(np.broadcast_to(np.arange(256, dtype=np.float32)[None, :], (128, 256)))
    del m["posv"], m["valid"]
    return m


def stage_shift(cx, nc, T):
    with ExitStack() as es:
        def sb(name, shape, dt):
            return es.enter_context(nc.sbuf_tensor(name, shape, dt))
        sc = sb("shc", [128, 28, 3], F32)
        raw = [sb("shraw%d" % i, [128, 514], F32) for i in range(4)]
        xo = [sb("shxo%d" % i, [128, 512], F32) for i in range(4)]
        cx.dma("sp", sc[:], T["shiftc_fm"], writes=["sc"])
        k = 0
        for rt in range(28):
            r0 = rt * 128
            m = min(128, RW - r0)
            for t0 in range(0, NTOK, 512):
                n = min(512, NTOK - t0)
                b = k % 4
                k += 1
                cx.dma("sp", raw[b][0:m, 0:n + 2], T["PRT"][r0:r0 + m, t0:t0 + n + 2], writes=[("raw", b)])
                cx.op("act", lambda e: e.activation(out=xo[b][0:m, 0:n], in_=raw[b][0:m, 1:n + 1], func=AF.Copy,
                                                    scale=sc[0:m, rt, 1:2]),
                      reads=[("raw", b), "sc"], writes=[("xo", b)])
                cx.op("dve", lambda e: e.scalar_tensor_tensor(out=xo[b][0:m, 0:n], in0=raw[b][0:m, 0:n],
                                                              scalar=sc[0:m, rt, 0:1], in1=xo[b][0:m, 0:n],
                                                              op0=ALU.mult, op1=ALU.add),
                      reads=[("raw", b), ("xo", b), "sc"], writes=[("xo", b)])
                cx.op("dve", lambda e: e.scalar_tensor_tensor(out=xo[b][0:m, 0:n], in0=raw[b][0:m, 2:n + 2],
                                                              scalar=sc[0:m, rt, 2:3], in1=xo[b][0:m, 0:n],
                                                              op0=ALU.mult, op1=ALU.add),
                      reads=[("raw", b), ("xo", b), "sc"], writes=[("xo", b)])
                if rt < 24:
                    q, hp = rt // 8, rt % 8
                    c0 = t0 // 64
                    ncs = n // 64
                    for hh in range(2):
                        h = 2 * hp + hh
                        cx.dma("pool", T["XSQ"][q, c0:c0 + ncs, :, h, :].rearrange("c j t -> j c t"),
                               xo[b][hh * 64:hh * 64 + 64, 0:n].rearrange("j (c t) -> j c t", t=64),
                               reads=[("xo", b)])
                else:
                    cx.dma("pool", T["XSL"][r0 - 3072:r0 - 3072 + m, t0:t0 + n], xo[b][0:m, 0:n], reads=[("xo", b)])
    cx.barrier()


def stage_rwkv(cx, nc, T, PQ, PS):
    F32R = mybir.dt.float32r
    HG = 4
    with ExitStack() as es:
        def sb(name, shape, dt=F32):
            return es.enter_context(nc.sbuf_tensor(name, shape, dt))
        identf = sb("identRf", [128, 128])
        ident = sb("identR", [128, 128], F32R)
        ones64 = sb("ones64s", [64, 64], F32R)
        rmask = sb("rmasks", [64, HG, 64])
        maskG = [sb("maskGs%d" % i, [128, 128]) for i in range(2)]
        maskN = [sb("maskNs%d" % i, [64, 64]) for i in range(2)]
        wup = [sb("wups%d" % i, [65, 1024], F32R) for i in range(2)]
        aup = [sb("aups%d" % i, [65, 1024], F32R) for i in range(2)]
        kkp = sb("kkp", [64, 16])
        kap = sb("kap", [64, 16])
        omka = sb("omka", [64, 16])
        rkp = sb("rkp", [64, 16])
        wstg = sb("wstg", [65, 1024])
        tinyb = sb("tinyb", [64, 1])
        cx.op("dve", lambda e: e.memset(tinyb[:], 1e-24), writes=["tinyb"])
        cx.dma("sp", identf[:], T["ident"], writes=["identf"])
        cx.op("dve", lambda e: e.tensor_copy(out=ident[:], in_=identf[:]), reads=["identf"], writes=["ident"])
        onesf = sb("onesf", [65, 64])
        cx.op("dve", lambda e: e.memset(onesf[:], 1.0), writes=["onesf"])
        cx.op("dve", lambda e: e.tensor_copy(out=ones64[:], in_=onesf[0:64, :]), reads=["onesf"], writes=["ones64"])
        cx.dma("sp", rmask[:], T["rmask"][:, 0:HG, :], writes=["rmask"])
        for i in range(2):
            cx.dma("sp", maskG[i][:], T["maskG%d" % i], writes=["maskG%d" % i])
            cx.dma("sp", maskN[i][:], T["maskN%d" % i], writes=["maskN%d" % i])
            for (dst, src, nm) in [(wup[i], "wup%d" % i, "wup%d" % i), (aup[i], "aup%d" % i, "aup%d" % i)]:
                cx.dma("sp", wstg[:], T[src], writes=["wstg"])
                cx.op("dve", lambda e: e.tensor_copy(out=dst[:], in_=wstg[:]), reads=["wstg"], writes=[nm])
        cx.dma("sp", kkp[:], T["kk_fm"], writes=["kkp"])
        cx.dma("sp", kap[:], T["ka_fm"], writes=["kap"])
        cx.dma("sp", rkp[:], T["rk_fm"], writes=["rkp"])
        cx.op("dve", lambda e: e.tensor_scalar(out=omka[:], in0=kap[:], scalar1=-1.0, scalar2=1.0, op0=ALU.mult,
                                               op1=ALU.add), reads=["kap"], writes=["omka"])

        bank = [0]

        def newbank():
            i = bank[0] % 8
            bank[0] += 1
            return PS[i], ("ps", i)

        def make_chain(cid):
            h0 = cid * HG
            A = {}
            for n in ["rT", "kT", "sg", "ic", "PF", "Pex", "Pin", "ea", "er", "ei", "em", "kx", "t1", "kk", "kt", "kki",
                      "Q0", "YO", "ST"]:
                A[n] = sb("c%d_%s" % (cid, n), [64, HG, 64])
            for n in ["t2", "Nk0", "Nk1", "Mk0", "Mk1", "TT0", "TT1", "X", "STr"]:
                A[n] = sb("c%d_%s" % (cid, n), [64, HG, 64], F32R)
            A["vpad"] = sb("c%d_vpad" % cid, [64, HG, 128])
            A["LBp"] = sb("c%d_LBp" % cid, [64, HG, 128])
            A["LB"] = sb("c%d_LB" % cid, [64, HG, 128], F32R)
            A["RA"] = sb("c%d_RA" % cid, [64, HG, 128], F32R)
            A["GM"] = sb("c%d_GM" % cid, [128, HG, 128], F32R)
            A["UV"] = sb("c%d_UV" % cid, [128, HG, 64], F32R)
            A["BK"] = sb("c%d_BK" % cid, [128, HG, 64], F32R)
            A["wdraw"] = sb("c%d_wdraw" % cid, [64, 64])
            A["adraw"] = sb("c%d_adraw" % cid, [64, 64])
            A["wd"] = sb("c%d_wd" % cid, [65, 64], F32R)
            A["ad"] = sb("c%d_ad" % cid, [65, 64], F32R)
            A["tot"] = sb("c%d_tot" % cid, [64, HG])
            A["etot"] = sb("c%d_etot" % cid, [64, HG])
            A["rkr"] = sb("c%d_rkr" % cid, [64, HG])
            return A

        chains = [make_chain(cid) for cid in range(16 // HG)]

        def run_chain(cid, di, chunks, ychunks, ydst):
            A = chains[cid]
            h0 = cid * HG

            def K(*names):
                return [(cid, n) for n in names]

            def bc(p):
                return p[:, h0:h0 + HG].unsqueeze(2).to_broadcast([64, HG, 64])

            def bcl(p):
                return p[:].unsqueeze(2).to_broadcast([64, HG, 64])

            def tt(eng, out, a, b, op, reads, writes):
                return cx.op(eng, lambda e: e.tensor_tensor(out=out, in0=a, in1=b, op=op), reads=reads, writes=writes)

            def act(out, in_, func, reads, writes, **kw):
                return cx.op("act", lambda e: e.activation(out=out, in_=in_, func=func, **kw), reads=reads, writes=writes)

            def hmm(pb, pk, lhsf, rhsf, reads, w=64):
                for h in range(HG):
                    cx.op("pe", lambda e: e.matmul(out=pb[0:64, h * w:h * w + w], lhsT=lhsf(h), rhs=rhsf(h), start=True,
                                                   stop=True), reads=reads, writes=[pk], last=(h == HG - 1))

            def p3(pb, rows=64, w=64):
                return pb[0:rows, 0:HG * w].rearrange("p (h t) -> p h t", h=HG)

            def loads(c):
                t0 = c * 64
                cx.dma("sp", A["rT"][:], T["XSQ"][0, c, :, h0:h0 + HG, :], writes=K("rT"))
                cx.dma("sp", A["kT"][:], T["XSQ"][1, c, :, h0:h0 + HG, :], writes=K("kT"))
                cx.dma("sp", A["vpad"][:, :, 64:128], T["XSQ"][2, c, :, h0:h0 + HG, :], writes=K("vpad"))
                lw0 = (R_WDF if di == 0 else R_WDB) - 3072
                la0 = (R_ADF if di == 0 else R_ADB) - 3072
                cx.dma("sp", A["wdraw"][:], T["XSL"][lw0:lw0 + 64, t0:t0 + 64], writes=K("wdraw"))
                cx.dma("sp", A["adraw"][:], T["XSL"][la0:la0 + 64, t0:t0 + 64], writes=K("adraw"))

            cx.op("dve", lambda e: e.memset(A["ST"][:], 0.0), writes=K("ST"))
            cx.op("dve", lambda e: e.tensor_copy(out=A["STr"][:], in_=A["ST"][:]), reads=K("ST"), writes=K("STr"))
            cx.op("dve", lambda e: e.tensor_copy(out=A["wd"][64:65, :], in_=onesf[64:65, :]), reads=["onesf"], writes=K("wd1"))
            cx.op("dve", lambda e: e.tensor_copy(out=A["ad"][64:65, :], in_=onesf[64:65, :]), reads=["onesf"], writes=K("ad1"))
            cx.op("dve", lambda e: e.memset(A["vpad"][:], 0.0), writes=K("vpad"))
            loads(chunks[0])
            yield
            for ci, c in enumerate(chunks):
                t0 = c * 64
                want_y = c in ychunks
                act(A["wd"][0:64, :], A["wdraw"][:], AF.Tanh, K("wdraw"), K("wd"))
                act(A["ad"][0:64, :], A["adraw"][:], AF.Copy, K("adraw"), K("ad"))
                pb, pk = newbank()
                hmm(pb, pk, lambda h: wup[di][0:65, (h0 + h) * 64:(h0 + h) * 64 + 64], lambda h: A["wd"][0:65, :],
                    K("wd", "wd1") + ["wup%d" % di])
                act(A["sg"][:], p3(pb), AF.Sigmoid, [pk], K("sg"))
                pb, pk = newbank()
                hmm(pb, pk, lambda h: aup[di][0:65, (h0 + h) * 64:(h0 + h) * 64 + 64], lambda h: A["ad"][0:65, :],
                    K("ad", "ad1") + ["aup%d" % di])
                act(A["ic"][:], p3(pb), AF.Sigmoid, [pk], K("ic"))
                yield
                cx.op("dve", lambda e: e.tensor_tensor_scan(out=A["PF"][:].rearrange("p h t -> p (h t)"),
                                                            data0=rmask[:].rearrange("p h t -> p (h t)"),
                                                            data1=A["sg"][:].rearrange("p h t -> p (h t)"),
                                                            initial=0.0, op0=ALU.mult, op1=ALU.add),
                      reads=["rmask"] + K("sg"), writes=K("PF"))
                cx.op("dve", lambda e: e.tensor_copy(out=A["tot"][:], in_=A["PF"][:, :, 63]), reads=K("PF"), writes=K("tot"))
                if di == 0:
                    tt("dve", A["Pex"][:], A["PF"][:], A["sg"][:], ALU.subtract, K("PF", "sg"), K("Pex"))
                    pin = "PF"
                else:
                    tt("dve", A["Pex"][:], bcl(A["tot"]), A["PF"][:], ALU.subtract, K("PF", "tot"), K("Pex"))
                    tt("dve", A["Pin"][:], A["Pex"][:], A["sg"][:], ALU.add, K("Pex", "sg"), K("Pin"))
                    pin = "Pin"
                tt("pool", A["kx"][:], A["kT"][:], bc(kkp), ALU.mult, K("kT") + ["kkp"], K("kx"))
                tt("pool", A["t2"][:], A["kx"][:], A["kx"][:], ALU.mult, K("kx"), K("t2"))
                yield
                act(A["ea"][:], A["Pex"][:], AF.Exp, K("Pex"), K("ea"), scale=-C0)
                act(A["er"][:], A[pin][:], AF.Exp, K(pin), K("er"), scale=-C0)
                act(A["ei"][:], A[pin][:], AF.Exp, K(pin), K("ei"), scale=C0)
                act(A["etot"][:], A["tot"][:], AF.Exp, K("tot"), K("etot"), scale=-C0)
                pb, pk = newbank()
                cx.op("pe", lambda e: e.matmul(out=pb[0:64, 0:HG * 64], lhsT=ones64[:, :], rhs=A["t2"][:, :, :], start=True,
                                               stop=True), reads=K("t2") + ["ones64"], writes=[pk])
                act(A["kk"][:], p3(pb), AF.Sqrt, [pk, "tinyb"], K("kk"), bias=tinyb[:, 0:1])
                tt("pool", A["t1"][:], A["ic"][:], bc(kap), ALU.mult, K("ic") + ["kap"], K("t1"))
                tt("pool", A["t1"][:], A["t1"][:], bc(omka), ALU.add, K("t1") + ["omka"], K("t1"))
                tt("pool", A["kt"][:], A["kT"][:], A["t1"][:], ALU.mult, K("kT", "t1"), K("kt"))
                yield
                cx.op("dve", lambda e: e.reciprocal(out=A["kk"][:], in_=A["kk"][:]), reads=K("kk"), writes=K("kk"))
                tt("dve", A["kk"][:], A["kk"][:], A["kx"][:], ALU.mult, K("kk", "kx"), K("kk"))
                tt("pool", A["kki"][:], A["kk"][:], A["ic"][:], ALU.mult, K("kk", "ic"), K("kki"))
                LB4 = A["LB"][:].rearrange("p h (s t) -> p h s t", s=2)
                RA4 = A["RA"][:].rearrange("p h (s t) -> p h s t", s=2)
                LP4 = A["LBp"][:].rearrange("p h (s t) -> p h s t", s=2)
                tt("pool", LB4[:, :, 0, :], A["kki"][:], A["ei"][:], ALU.mult, K("kki", "ei"), K("LB"))
                tt("pool", LB4[:, :, 1, :], A["kt"][:], A["ei"][:], ALU.mult, K("kt", "ei", "LB"), K("LB"))
                cx.op("dve", lambda e: e.scalar_tensor_tensor(out=RA4[:, :, 0, :], in0=A["kk"][:], scalar=-1.0,
                                                              in1=A["ea"][:], op0=ALU.mult, op1=ALU.mult),
                      reads=K("kk", "ea"), writes=K("RA"))
                tt("dve", RA4[:, :, 1, :], A["rT"][:], A["er"][:], ALU.mult, K("rT", "er", "RA"), K("RA"))
                tt("pool", A["LBp"][:], A["LB"][:], A["etot"][:].unsqueeze(2).to_broadcast([64, HG, 128]), ALU.mult,
                   K("LB", "etot"), K("LBp"))
                if want_y:
                    tt("pool", A["t1"][:], A["rT"][:], A["kt"][:], ALU.mult, K("rT", "kt"), K("t1"))
                    tt("pool", A["t2"][:], A["t1"][:], bc(rkp), ALU.mult, K("t1") + ["rkp"], K("t2"))
                yield
                if want_y:
                    pb, pk = newbank()
                    hmm(pb, pk, lambda h: A["t2"][:, h, :], lambda h: ones64[:, 0:2], K("t2") + ["ones64"], w=2)
                    cx.op("act", lambda e: e.copy(out=A["rkr"][:], in_=pb[0:64, 0:2 * HG].rearrange("p (h t) -> p h t", t=2)[:, :, 0]),
                          reads=[pk], writes=K("rkr"))
                pb, pk = newbank()
                for h in range(HG):
                    cx.op("pe", lambda e: e.matmul(out=pb[:, h * 128:h * 128 + 128], lhsT=A["LB"][:, h, :], rhs=A["RA"][:, h, :],
                                                   start=True, stop=True), reads=K("LB", "RA"), writes=[pk], last=(h == HG - 1))
                tt("dve", A["GM"][:], pb[:, 0:HG * 128].rearrange("p (h t) -> p h t", h=HG),
                   maskG[di][:].unsqueeze(1).to_broadcast([128, HG, 128]), ALU.mult, [pk, "maskG%d" % di], K("GM"))
                pb, pk = newbank()
                hmm(pb, pk, lambda h: A["RA"][:, h, 0:64], lambda h: A["LB"][:, h, 0:64], K("RA", "LB"))
                tt("dve", A["Nk0"][:], p3(pb), maskN[di][:].unsqueeze(1).to_broadcast([64, HG, 64]), ALU.mult,
                   [pk, "maskN%d" % di], K("Nk0"))
                yield
                cx.op("act", lambda e: e.copy(out=A["Mk0"][:], in_=A["GM"][0:64, :, 0:64]), reads=K("GM"), writes=K("Mk0"))
                tt("pool", A["TT0"][:], A["GM"][0:64, :, 0:64], identf[0:64, 0:64].unsqueeze(1).to_broadcast([64, HG, 64]),
                   ALU.add, K("GM") + ["identf"], K("TT0"))
                pb, pk = newbank()
                for h in range(HG):
                    cx.op("pe", lambda e: e.transpose(out=pb[:, h * 64:h * 64 + 64], in_=A["vpad"][:, h, :],
                                                      identity=identf[0:64, 0:64]), reads=K("vpad") + ["identf"], writes=[pk], last=(h == HG - 1))
                cx.op("act", lambda e: e.copy(out=A["UV"][64:128, :, :], in_=pb[64:128, 0:HG * 64].rearrange("p (h t) -> p h t", h=HG)),
                      reads=[pk], writes=K("UVv"))
                pb, pk = newbank()
                for h in range(HG):
                    cx.op("pe", lambda e: e.transpose(out=pb[:, h * 64:h * 64 + 64], in_=A["LBp"][:, h, :],
                                                      identity=identf[0:64, 0:64]), reads=K("LBp") + ["identf"], writes=[pk], last=(h == HG - 1))
                cx.op("act", lambda e: e.copy(out=A["BK"][:], in_=pb[:, 0:HG * 64].rearrange("p (h t) -> p h t", h=HG)),
                      reads=[pk], writes=K("BK"))
                if ci + 1 < len(chunks):
                    loads(chunks[ci + 1])
                yield
                cur = 0
                for lvl in range(5):
                    nxt = 1 - cur
                    Nc, Mc, Nn, Mn = "Nk%d" % cur, "Mk%d" % cur, "Nk%d" % nxt, "Mk%d" % nxt
                    pb, pk = newbank()
                    hmm(pb, pk, lambda h: A[Mc][:, h, :], lambda h: A[Nc][:, h, :], K(Mc, Nc))
                    cx.op("act", lambda e: e.copy(out=A[Nn][:], in_=p3(pb)), reads=[pk], writes=K(Nn))
                    if lvl < 4:
                        pb, pk = newbank()
                        hmm(pb, pk, lambda h: A[Nc][:, h, :], lambda h: A[Mc][:, h, :], K(Mc, Nc))
                        cx.op("act", lambda e: e.copy(out=A[Mn][:], in_=p3(pb)), reads=[pk], writes=K(Mn))
                    yield
                    Tc, Tn = "TT%d" % cur, "TT%d" % nxt
                    pb, pk = newbank()
                    hmm(pb, pk, lambda h: A[Nn][:, h, :], lambda h: A[Tc][:, h, :], K(Nn, Tc))
                    tt("dve", A[Tn][:], p3(pb), A[Tc][:], ALU.add, [pk] + K(Tc), K(Tn))
                    cur = nxt
                    yield
                TTf = "TT%d" % cur
                pb, pk = newbank()
                hmm(pb, pk, lambda h: A["GM"][64:128, h, 0:64], lambda h: A["UV"][64:128, h, :], K("GM", "UVv"))
                cx.op("act", lambda e: e.copy(out=A["Q0"][:], in_=p3(pb)), reads=[pk], writes=K("Q0"))
                yield
                pb, pk = newbank()
                hmm(pb, pk, lambda h: A["RA"][:, h, 0:64], lambda h: A["STr"][:, h, :], K("RA", "STr"))
                tt("dve", A["X"][:], p3(pb), A["Q0"][:], ALU.add, [pk] + K("Q0"), K("X"))
                yield
                pb, pk = newbank()
                hmm(pb, pk, lambda h: A[TTf][:, h, :], lambda h: A["X"][:, h, :], K(TTf, "X"))
                cx.op("act", lambda e: e.copy(out=A["UV"][0:64, :, :], in_=p3(pb)), reads=[pk], writes=K("UVu"))
                yield
                if want_y:
                    pb, pk = newbank()
                    for h in range(HG):
                        cx.op("pe", lambda e: e.matmul(out=pb[0:64, h * 64:h * 64 + 64], lhsT=A["RA"][:, h, 64:128],
                                                       rhs=A["STr"][:, h, :], start=True, stop=False),
                              reads=K("RA", "STr"), writes=[pk], last=False)
                        cx.op("pe", lambda e: e.matmul(out=pb[0:64, h * 64:h * 64 + 64], lhsT=A["GM"][:, h, 64:128],
                                                       rhs=A["UV"][:, h, :], start=False, stop=True),
                              reads=K("GM", "UVu", "UVv"), writes=[pk], last=(h == HG - 1))
                    cx.op("act", lambda e: e.copy(out=A["YO"][:], in_=p3(pb)), reads=[pk], writes=K("YO"))
                    cx.dma("act", T[ydst][t0 - 64:t0, h0 * 64:(h0 + HG) * 64], A["YO"][:].rearrange("p h t -> p (h t)"),
                           reads=K("YO"))
                    cx.dma("act", T[ydst][t0 - 64:t0, 1024 + h0:1024 + h0 + HG], A["rkr"][:], reads=K("rkr"))
                    if di == 0:
                        cx.dma("act", T["VTM"][t0 - 64:t0, h0 * 64:(h0 + HG) * 64],
                               A["UV"][64:128, :, :].bitcast(F32).rearrange("p h t -> p (h t)"), reads=K("UVv"))
                pb, pk = newbank()
                hmm(pb, pk, lambda h: A["BK"][:, h, :], lambda h: A["UV"][:, h, :], K("BK", "UVu", "UVv"))
                tt("dve", A["ST"][:], A["ST"][:], bcl(A["etot"]), ALU.mult, K("ST", "etot"), K("ST"))
                tt("dve", A["ST"][:], A["ST"][:], p3(pb), ALU.add, K("ST") + [pk], K("ST"))
                cx.op("act", lambda e: e.copy(out=A["STr"][:], in_=A["ST"][:]), reads=K("ST"), writes=K("STr"))
                yield

        own = set(range(1, 33))
        for (di, chunks, ydst) in [(0, list(range(0, 33)), "YF"), (1, list(range(65, 0, -1)), "YB")]:
            gens = [run_chain(cid, di, chunks, own, ydst) for cid in range(16 // HG)]
            alive = list(gens)
            while alive:
                for g in list(alive):
                    try:
                        next(g)
                    except StopIteration:
                        alive.remove(g)
    cx.barrier()


def load_w_bf16(cx, nc, es, name, src, K, N, stg, stgkey):
    w = es.enter_context(nc.sbuf_tensor(name, [128, K // 128, N], BF16))
    for c in range(K // 128):
        for n0 in range(0, N, 2048):
            n = min(2048, N - n0)
            cx.dma("pool", w[:, c, n0:n0 + n], src[c * 128:(c + 1) * 128, n0:n0 + n], writes=[name])
    return w


def stage_post(cx, nc, T, PQ):
    with ExitStack() as es:
        def sb(name, shape, dt=F32):
            return es.enter_context(nc.sbuf_tensor(name, shape, dt))
        H = 16
        ident = sb("identP", [128, 128])
        lng = sb("lng", [128, 1024])
        lnb = sb("lnb", [128, 1024])
        gup0 = sb("gup0", [128, 1024])
        gup1 = sb("gup1", [32, 1024])
        gne = sb("gne", [128, 1])
        yf = sb("yf", [128, 1040])
        yb = sb("yb", [128, 1040])
        vt = sb("vt", [128, H, 64])
        y = sb("ypost", [128, H, 64])
        sq = sb("sqpost", [128, H, 64])
        mu = sb("mu", [128, H])
        var = sb("var", [128, H])
        bon = sb("bon", [128, H])
        sg0 = sb("sg0", [128, 128])
        sg1 = sb("sg1", [32, 128])
        yrt = sb("yrt", [128, 8, 128], BF16)
        cx.dma("sp", ident[:], T["ident"], writes=["ident"])
        cx.dma("sp", lng[:], T["lng_b"], writes=["lng"])
        cx.dma("sp", lnb[:], T["lnb_b"], writes=["lnb"])
        cx.dma("sp", gup0[:], T["g_up"][0:128, :], writes=["gup0"])
        cx.dma("sp", gup1[:], T["g_up"][128:160, :], writes=["gup1"])
        cx.op("dve", lambda e: e.memset(gne[:], 64e-5), writes=["gne"])
        for ti in range(16):
            t0 = ti * 128
            cx.dma("sp", yf[:], T["YF"][t0:t0 + 128, :], writes=["yf"])
            cx.dma("sp", yb[:], T["YB"][t0:t0 + 128, :], writes=["yb"])
            cx.dma("sp", vt[:].rearrange("p h t -> p (h t)"), T["VTM"][t0:t0 + 128, :], writes=["vt"])
            cx.dma("sp", sg0[:], T["XSL"][0:128, 64 + t0:64 + t0 + 128], writes=["sg0"])
            cx.dma("sp", sg1[:], T["XSL"][128:160, 64 + t0:64 + t0 + 128], writes=["sg1"])
            cx.op("act", lambda e: e.activation(out=sg0[:], in_=sg0[:], func=AF.Sigmoid), reads=["sg0"], writes=["sg0"])
            cx.op("act", lambda e: e.activation(out=sg1[:], in_=sg1[:], func=AF.Sigmoid), reads=["sg1"], writes=["sg1"])
            pq = PQ[ti % 2]
            pk = ("pq", ti % 2)
            for nb in range(2):
                cx.op("pe", lambda e: e.matmul(out=pq[:, nb * 512:nb * 512 + 512], lhsT=sg0[:, :],
                                               rhs=gup0[:, nb * 512:nb * 512 + 512], start=True, stop=False),
                      reads=["sg0", "gup0"], writes=[pk], last=False)
                cx.op("pe", lambda e: e.matmul(out=pq[:, nb * 512:nb * 512 + 512], lhsT=sg1[:, :],
                                               rhs=gup1[:, nb * 512:nb * 512 + 512], start=False, stop=True),
                      reads=["sg1", "gup1"], writes=[pk], last=(nb == 1))
            y3f = yf[:, 0:1024].rearrange("p (h t) -> p h t", h=H)
            y3b = yb[:, 0:1024].rearrange("p (h t) -> p h t", h=H)
            cx.op("dve", lambda e: e.tensor_tensor(out=y[:], in0=y3f, in1=y3b, op=ALU.add), reads=["yf", "yb"], writes=["y"])
            cx.op("dve", lambda e: e.tensor_reduce(out=mu[:], in_=y[:], axis=AX.X, op=ALU.add), reads=["y"], writes=["mu"])
            cx.op("dve", lambda e: e.tensor_scalar(out=mu[:], in0=mu[:], scalar1=1.0 / 64, scalar2=None, op0=ALU.mult),
                  reads=["mu"], writes=["mu"])
            cx.op("dve", lambda e: e.tensor_tensor(out=y[:], in0=y[:], in1=mu[:].unsqueeze(2).to_broadcast([128, H, 64]),
                                                   op=ALU.subtract), reads=["y", "mu"], writes=["y"])
            cx.op("dve", lambda e: e.tensor_tensor(out=sq[:], in0=y[:], in1=y[:], op=ALU.mult), reads=["y"], writes=["sq"])
            cx.op("dve", lambda e: e.tensor_reduce(out=var[:], in_=sq[:], axis=AX.X, op=ALU.add), reads=["sq"], writes=["var"])
            cx.op("act", lambda e: e.activation(out=var[:], in_=var[:], func=AF.Sqrt, scale=1.0 / 64, bias=gne[:, 0:1]),
                  reads=["var", "gne"], writes=["var"])
            cx.op("dve", lambda e: e.reciprocal(out=var[:], in_=var[:]), reads=["var"], writes=["var"])
            cx.op("dve", lambda e: e.tensor_tensor(out=y[:], in0=y[:], in1=var[:].unsqueeze(2).to_broadcast([128, H, 64]),
                                                   op=ALU.mult), reads=["y", "var"], writes=["y"])
            yfl = y[:].rearrange("p h t -> p (h t)")
            cx.op("dve", lambda e: e.tensor_tensor(out=yfl, in0=yfl, in1=lng[:], op=ALU.mult), reads=["y", "lng"], writes=["y"])
            cx.op("dve", lambda e: e.tensor_tensor(out=yfl, in0=yfl, in1=lnb[:], op=ALU.add), reads=["y", "lnb"], writes=["y"])
            cx.op("dve", lambda e: e.tensor_tensor(out=bon[:], in0=yf[:, 1024:1040], in1=yb[:, 1024:1040], op=ALU.add),
                  reads=["yf", "yb"], writes=["bon"])
            cx.op("dve", lambda e: e.scalar_tensor_tensor(out=sq[:], in0=vt[:], scalar=0.5,
                                                          in1=bon[:].unsqueeze(2).to_broadcast([128, H, 64]),
                                                          op0=ALU.mult, op1=ALU.mult), reads=["vt", "bon"], writes=["sq"])
            cx.op("dve", lambda e: e.tensor_tensor(out=y[:], in0=y[:], in1=sq[:], op=ALU.add), reads=["y", "sq"], writes=["y"])
            cx.op("dve", lambda e: e.tensor_tensor(out=yfl, in0=yfl, in1=pq[:, :], op=ALU.mult), reads=["y", pk], writes=["y"])
            pq2 = PQ[2 + ti % 2]
            pk2 = ("pq", 2 + ti % 2)
            for c in range(8):
                cx.op("pe", lambda e: e.transpose(out=pq2[:, c * 128:(c + 1) * 128], in_=yfl[:, c * 128:(c + 1) * 128],
                                                  identity=ident[:, :]), reads=["y", "ident"], writes=[pk2], last=(c == 7))
            cx.op("act", lambda e: e.copy(out=yrt[:], in_=pq2[:, :].rearrange("p (c t) -> p c t", c=8)),
                  reads=[pk2], writes=["yrt"])
            cx.dma("sp", T["YRT"][:, t0:t0 + 128].rearrange("(c p) t -> p c t", p=128), yrt[:], reads=["yrt"])
    cx.barrier()


def stage_mla(cx, nc, T, PQ, PS):
    with ExitStack() as es:
        def sb(name, shape, dt=F32):
            return es.enter_context(nc.sbuf_tensor(name, shape, dt))
        ident = sb("identM", [128, 128])
        ones = sb("onesM", [128, 128])
        stg = [sb("mstg%d" % i, [128, 2048]) for i in range(2)]
        epsc = sb("epscM", [128, 1])
        negpi = sb("negpi", [64, 1])
        invf = sb("invfs", [64, 1])
        gq = sb("gq", [128, 4])
        gkv = sb("gkv", [128, 4])
        kbias = sb("kbias", [128, 33])
        kvn = sb("kvnT", [128, 4, NTOK], BF16)
        qn = sb("qnT", [128, 4, NOWN], BF16)
        cos2 = sb("cos2", [64, NTOK])
        sin2 = sb("sin2", [64, NTOK])
        kr = sb("krT", [128, NTOK], BF16)
        cx.dma("sp", ident[:], T["ident"], writes=["ident"])
        cx.dma("sp", invf[:], T["invf"], writes=["invf"])
        cx.dma("sp", gq[:], T["gq_fm"], writes=["gq"])
        cx.dma("sp", gkv[:], T["gkv_fm"], writes=["gkv"])
        cx.dma("sp", kbias[:], T["valid_tm"], writes=["kbias"])
        cx.op("dve", lambda e: e.tensor_scalar(out=kbias[:], in0=kbias[:], scalar1=-1.0, scalar2=30000.0, op0=ALU.add,
                                               op1=ALU.mult), reads=["kbias"], writes=["kbias"])
        cx.op("dve", lambda e: e.memset(ones[:], 1.0), writes=["ones"])
        cx.op("dve", lambda e: e.memset(epsc[:], EPS), writes=["epsc"])
        cx.op("dve", lambda e: e.memset(negpi[:], -float(np.pi)), writes=["negpi"])
        wuq = load_w_bf16(cx, nc, es, "wuq", T["w_uq"], 512, 3072, stg, "mstg")
        wukv = load_w_bf16(cx, nc, es, "wukv", T["w_ukv"], 512, 4096, stg, "mstg")
        wqr = sb("wqrot", [128, 4, 16, 64], BF16)
        wuq4 = wuq[:].rearrange("p c (h e) -> p c h e", h=16)
        cx.op("pool", lambda e: e.tensor_scalar(out=wqr[:, :, :, 0:32], in0=wuq4[:, :, :, 160:192], scalar1=-1.0,
                                                scalar2=None, op0=ALU.mult), reads=["wuq"], writes=["wqr"])
        cx.op("pool", lambda e: e.tensor_copy(out=wqr[:, :, :, 32:64], in_=wuq4[:, :, :, 128:160]), reads=["wuq"],
              writes=["wqr"])
        cx.barrier()
        kf = stg[0][0:64, 0:1056]
        ki = stg[1][0:64, 0:1056].bitcast(I32)
        TWO_PI = float(2 * np.pi)
        for (tab, off, nm) in [(sin2, 0.0, "sin2"), (cos2, float(0.5 * np.pi), "cos2")]:
            for hb in range(4):
                cs = slice(hb * 1056, hb * 1056 + 1056)
                cx.dma("sp", tab[:, cs], T["pos64"][:, cs], writes=[nm])
                cx.op("dve", lambda e: e.tensor_scalar(out=tab[:, cs], in0=tab[:, cs], scalar1=invf[:, 0:1], scalar2=off,
                                                       op0=ALU.mult, op1=ALU.add), reads=[nm, "invf"], writes=[nm])
                cx.op("dve", lambda e: e.tensor_scalar(out=kf, in0=tab[:, cs], scalar1=1.0 / TWO_PI, scalar2=None,
                                                       op0=ALU.mult), reads=[nm], writes=["kf"])
                cx.op("dve", lambda e: e.tensor_copy(out=ki, in_=kf), reads=["kf"], writes=["ki"])
                cx.op("dve", lambda e: e.tensor_copy(out=kf, in_=ki), reads=["ki"], writes=["kf"])
                cx.op("dve", lambda e: e.scalar_tensor_tensor(out=tab[:, cs], in0=kf, scalar=-TWO_PI, in1=tab[:, cs],
                                                              op0=ALU.mult, op1=ALU.add), reads=["kf", nm], writes=[nm])
                cx.op("dve", lambda e: e.tensor_scalar(out=kf, in0=tab[:, cs], scalar1=float(np.pi), scalar2=None,
                                                       op0=ALU.is_gt), reads=[nm], writes=["kf"])
                cx.op("dve", lambda e: e.scalar_tensor_tensor(out=tab[:, cs], in0=kf, scalar=-TWO_PI, in1=tab[:, cs],
                                                              op0=ALU.mult, op1=ALU.add), reads=["kf", nm], writes=[nm])
                cx.op("dve", lambda e: e.tensor_scalar(out=kf, in0=tab[:, cs], scalar1=-float(np.pi), scalar2=None,
                                                       op0=ALU.is_lt), reads=[nm], writes=["kf"])
                cx.op("dve", lambda e: e.scalar_tensor_tensor(out=tab[:, cs], in0=kf, scalar=TWO_PI, in1=tab[:, cs],
                                                              op0=ALU.mult, op1=ALU.add), reads=["kf", nm], writes=[nm])
                cx.op("act", lambda e: e.activation(out=tab[:, cs], in_=tab[:, cs], func=AF.Sin),
                      reads=[nm], writes=[nm])
        cx.barrier()
        def latent_norm(row0, tok0, ntok, dst, g, nm):
            for b0 in range(0, ntok, 512):
                n = min(512, ntok - b0)
                xs_ = []
                pq, pk = PQ[0], ("pq", 0)
                for c in range(4):
                    st_ = stg[c % 2]
                    half = (c // 2) * 1024
                    cx.dma("sp", st_[:, half:half + n], T["PRT"][row0 + c * 128:row0 + c * 128 + 128,
                                                                  1 + tok0 + b0:1 + tok0 + b0 + n],
                           writes=[("mstg", c % 2, c // 2)])
                    cx.op("act", lambda e: e.activation(out=st_[:, half + 512:half + 512 + n], in_=st_[:, half:half + n],
                                                        func=AF.Square),
                          reads=[("mstg", c % 2, c // 2)], writes=[("msq", c % 2, c // 2)])
                    cx.op("pe", lambda e: e.matmul(out=pq[:, 0:n], lhsT=ones[:, :], rhs=st_[:, half + 512:half + 512 + n],
                                                   start=(c == 0), stop=(c == 3)),
                          reads=[("msq", c % 2, c // 2), "ones"], writes=[pk], last=(c == 3))
                cx.op("act", lambda e: e.activation(out=pq[:, 512:512 + n], in_=pq[:, 0:n], func=AF.Sqrt, scale=1.0 / 512,
                                                    bias=epsc[:, 0:1]), reads=[pk, "epsc"], writes=[("pqb", 0)])
                cx.op("dve", lambda e: e.reciprocal(out=pq[:, 512:512 + n], in_=pq[:, 512:512 + n]),
                      reads=[("pqb", 0)], writes=[("pqb", 0)])
                for c in range(4):
                    st_ = stg[c % 2]
                    half = (c // 2) * 1024
                    cx.op("dve", lambda e: e.scalar_tensor_tensor(out=dst[:, c, b0:b0 + n], in0=st_[:, half:half + n],
                                                                  scalar=g[:, c:c + 1], in1=pq[:, 512:512 + n],
                                                                  op0=ALU.mult, op1=ALU.mult),
                          reads=[("mstg", c % 2, c // 2), ("pqb", 0), nm], writes=[("dst", nm)])
                    cx.buf[("msq", c % 2, c // 2)] = cx.buf.get(("msq", c % 2, c // 2), {"w": None, "r": {}})
        latent_norm(R_KVD, 0, NTOK, kvn, gkv, "gkv")
        latent_norm(R_QD, OWN0, NOWN, qn, gq, "gq")
        for b0 in range(0, NTOK, 1024):
            n = min(1024, NTOK - b0)
            cx.dma("sp", stg[0][0:64, 0:n], T["PRT"][R_KR:R_KR + 64, 1 + b0:1 + b0 + n], writes=[("mstg", 0, 0), ("mstg", 0, 1)])
            cx.dma("sp", stg[1][0:64, 0:n], T["PRT"][R_KRR:R_KRR + 64, 1 + b0:1 + b0 + n], writes=[("mstg", 1, 0), ("mstg", 1, 1)])
            cx.op("dve", lambda e: e.tensor_tensor(out=stg[0][0:64, 0:n], in0=stg[0][0:64, 0:n], in1=cos2[:, b0:b0 + n],
                                                   op=ALU.mult), reads=[("mstg", 0, 0), ("mstg", 0, 1), "cos2"],
                  writes=[("mstg", 0, 0), ("mstg", 0, 1)])
            cx.op("dve", lambda e: e.tensor_tensor(out=stg[1][0:64, 0:n], in0=stg[1][0:64, 0:n], in1=sin2[:, b0:b0 + n],
                                                   op=ALU.mult), reads=[("mstg", 1, 0), ("mstg", 1, 1), "sin2"],
                  writes=[("mstg", 1, 0), ("mstg", 1, 1)])
            cx.op("dve", lambda e: e.tensor_tensor(out=kr[0:64, b0:b0 + n], in0=stg[0][0:64, 0:n], in1=stg[1][0:64, 0:n],
                                                   op=ALU.add), reads=[("mstg", 0, 0), ("mstg", 0, 1), ("mstg", 1, 0), ("mstg", 1, 1)],
                  writes=["kr"])
        cx.barrier()
        kT = sb("kTh", [128, NTOK], BF16)
        Vh = sb("Vh", [128, 33, 132], BF16)
        qT = sb("qTh", [128, NOWN], BF16)
        qr = sb("qrh", [128, NOWN], BF16)
        qa = sb("qra", [128, 512])
        qb_ = sb("qrb", [64, 512])
        PT = [sb("PT%d" % i, [128, 512], BF16) for i in range(2)]
        ymt = [sb("ymt%d" % i, [128, 512], BF16) for i in range(2)]
        rs = sb("rsum", [1, 512])
        bcs = qa
        ones1 = sb("ones1", [1, 128])
        onesb = sb("onesb", [128, 128], BF16)
        cx.op("dve", lambda e: e.memset(ones1[:], 1.0), writes=["ones1"])
        cx.op("dve", lambda e: e.memset(onesb[:], 1.0), writes=["onesb"])
        qi_box = [0]
        cx.op("dve", lambda e: e.memset(Vh[:, :, 128:129], 1.0), writes=["Vh1"])
        cx.op("pool", lambda e: e.memset(kr[64:128, :], 0.0), writes=["kr0"])
        cx.op("pool", lambda e: e.memset(qr[64:128, :], 0.0), writes=["qr0"])
        scale = float(192 ** -0.5)
        kk = 0
        kk_box = [0]
        for h in range(16):
            for b0 in range(0, NTOK, 512):
                n = min(512, NTOK - b0)
                pb, pbk = PS[kk % 2], ("ps", kk % 2)
                kk += 1
                for c in range(4):
                    cx.op("pe", lambda e: e.matmul(out=pb[:, 0:n], lhsT=wukv[:, c, h * 256:h * 256 + 128],
                                                   rhs=kvn[:, c, b0:b0 + n], start=(c == 0), stop=(c == 3)),
                          reads=["wukv", ("dst", "gkv")], writes=[pbk], last=(c == 3))
                cx.op("act", lambda e: e.copy(out=kT[:, b0:b0 + n], in_=pb[:, 0:n]), reads=[pbk], writes=["kT"])
            for kt in range(33):
                pb, pbk = PS[kk % 2], ("ps", kk % 2)
                kk += 1
                for c in range(4):
                    cx.op("pe", lambda e: e.matmul(out=pb[:, 0:128], lhsT=kvn[:, c, kt * 128:kt * 128 + 128],
                                                   rhs=wukv[:, c, h * 256 + 128:h * 256 + 256], start=(c == 0), stop=(c == 3)),
                          reads=["wukv", ("dst", "gkv")], writes=[pbk], last=(c == 3))
                cx.op("dve", lambda e: e.tensor_copy(out=Vh[:, kt, 0:128], in_=pb[:, 0:128]), reads=[pbk], writes=["Vh"])
            for b0 in range(0, NOWN, 512):
                pb, pbk = PS[kk % 2], ("ps", kk % 2)
                kk += 1
                for c in range(4):
                    cx.op("pe", lambda e: e.matmul(out=pb[:, 0:512], lhsT=wuq[:, c, h * 192:h * 192 + 128],
                                                   rhs=qn[:, c, b0:b0 + 512], start=(c == 0), stop=(c == 3)),
                          reads=["wuq", ("dst", "gq")], writes=[pbk], last=(c == 3))
                cx.op("act", lambda e: e.copy(out=qT[:, b0:b0 + 512], in_=pb[:, 0:512]), reads=[pbk], writes=["qT"])
                pb, pbk = PS[kk % 2], ("ps", kk % 2)
                kk += 1
                for c in range(4):
                    cx.op("pe", lambda e: e.matmul(out=pb[0:64, 0:512], lhsT=wuq[:, c, h * 192 + 128:h * 192 + 192],
                                                   rhs=qn[:, c, b0:b0 + 512], start=(c == 0), stop=(c == 3)),
                          reads=["wuq", ("dst", "gq")], writes=[pbk], last=(c == 3))
                cx.op("dve", lambda e: e.tensor_tensor(out=qa[0:64, :], in0=pb[0:64, 0:512], in1=cos2[:, OWN0 + b0:OWN0 + b0 + 512],
                                                       op=ALU.mult), reads=[pbk, "cos2"], writes=["qa"])
                pb, pbk = PS[kk % 2], ("ps", kk % 2)
                kk += 1
                for c in range(4):
                    cx.op("pe", lambda e: e.matmul(out=pb[0:64, 0:512], lhsT=wqr[:, c, h, :],
                                                   rhs=qn[:, c, b0:b0 + 512], start=(c == 0), stop=(c == 3)),
                          reads=["wqr", ("dst", "gq")], writes=[pbk], last=(c == 3))
                cx.op("dve", lambda e: e.tensor_tensor(out=qb_[:], in0=pb[0:64, 0:512], in1=sin2[:, OWN0 + b0:OWN0 + b0 + 512],
                                                       op=ALU.mult), reads=[pbk, "sin2"], writes=["qb"])
                cx.op("dve", lambda e: e.tensor_tensor(out=qr[0:64, b0:b0 + 512], in0=qa[0:64, :], in1=qb_[:], op=ALU.add),
                      reads=["qa", "qb"], writes=["qr"])
            for qblk in range(4):
                q0 = qblk * 512
                sbank = {}

                def emit_S(kt):
                    i = kk_box[0] % 2
                    kk_box[0] += 1
                    pb_, pbk_ = PS[i], ("ps", i)
                    cx.op("pe", lambda e: e.matmul(out=pb_[:, 0:512], lhsT=kT[:, kt * 128:kt * 128 + 128],
                                                   rhs=qT[:, q0:q0 + 512], start=True, stop=False),
                          reads=["kT", "qT"], writes=[pbk_], last=False)
                    cx.op("pe", lambda e: e.matmul(out=pb_[:, 0:512], lhsT=kr[:, kt * 128:kt * 128 + 128],
                                                   rhs=qr[:, q0:q0 + 512], start=False, stop=True),
                          reads=["kr", "kr0", "qr", "qr0"], writes=[pbk_])
                    sbank[kt] = (pb_, pbk_)

                emit_S(0)
                for kt in range(33):
                    if kt + 1 < 33:
                        emit_S(kt + 1)
                    pb, pbk = sbank.pop(kt)
                    pt = PT[kt % 2]
                    cx.op("act", lambda e: e.activation(out=pt[:], in_=pb[:, 0:512], func=AF.Exp, scale=scale,
                                                        bias=kbias[:, kt:kt + 1]),
                          reads=[pbk, "kbias"], writes=[("PT", kt % 2)])
                    ob_, sb_ = 4 + 2 * (qi_box[0] % 2), 5 + 2 * (qi_box[0] % 2)
                    cx.op("pe", lambda e: e.matmul(out=PS[ob_][:, 0:512], lhsT=Vh[:, kt, 0:128], rhs=pt[:, 0:512],
                                                   start=(kt == 0), stop=(kt == 32)),
                          reads=[("PT", kt % 2), "Vh"], writes=[("ps", ob_)], last=False)
                    cx.op("pe", lambda e: e.matmul(out=PS[sb_][:, 0:512], lhsT=onesb[:, :], rhs=pt[:, 0:512],
                                                   start=(kt == 0), stop=(kt == 32)),
                          reads=[("PT", kt % 2), "onesb"], writes=[("ps", sb_)])
                cx.op("dve", lambda e: e.reciprocal(out=bcs[:], in_=PS[sb_][:, 0:512]), reads=[("ps", sb_)], writes=["qa"])
                ym = ymt[qi_box[0] % 2]
                cx.op("dve", lambda e: e.tensor_tensor(out=ym[:], in0=PS[ob_][:, 0:512], in1=bcs[:], op=ALU.mult),
                      reads=[("ps", ob_), "qa"], writes=[("ymt", qi_box[0] % 2)])
                cx.dma("sp", T["YMT"][h * 128:h * 128 + 128, q0:q0 + 512], ym[:], reads=[("ymt", qi_box[0] % 2)])
                qi_box[0] += 1
    cx.barrier()


def stage_merge(cx, nc, T, PQ, PS):
    with ExitStack() as es:
        def sb(name, shape, dt=F32):
            return es.enter_context(nc.sbuf_tensor(name, shape, dt))
        stg = [sb("gstg%d" % i, [128, 2048]) for i in range(2)]
        prw = load_w_bf16(cx, nc, es, "prw", T["p_rwkv"], 1024, 2048, stg, "gstg")
        pml = load_w_bf16(cx, nc, es, "pml", T["p_mla"], 2048, 2048, stg, "gstg")
        bg = sb("bgate", [128, 32])
        cx.dma("sp", bg[:], T["bgate_fm"], writes=["bg"])
        yr = sb("yrblk", [128, 8, 512], BF16)
        ym = sb("ymblk", [128, 16, 512], BF16)
        gr = sb("grblk", [128, 512])
        gm = sb("gmblk", [128, 512])
        mg = [sb("mgblk%d" % i, [128, 512], BF16) for i in range(2)]
        cx.barrier()
        kk = 0
        for tb in range(4):
            t0 = tb * 512
            cx.dma("sp", yr[:], T["YRT"][:, t0:t0 + 512].rearrange("(c p) t -> p c t", p=128), writes=["yr"])
            cx.dma("sp", ym[:], T["YMT"][:, t0:t0 + 512].rearrange("(c p) t -> p c t", p=128), writes=["ym"])
            for dc in range(16):
                cx.dma("sp", gr[:], T["PRT"][R_GR + dc * 128:R_GR + dc * 128 + 128, 1 + OWN0 + t0:1 + OWN0 + t0 + 512],
                       writes=["gr"])
                cx.dma("sp", gm[:], T["PRT"][R_GM + dc * 128:R_GM + dc * 128 + 128, 1 + OWN0 + t0:1 + OWN0 + t0 + 512],
                       writes=["gm"])
                cx.op("act", lambda e: e.activation(out=gr[:], in_=gr[:], func=AF.Sigmoid, bias=bg[:, dc:dc + 1]),
                      reads=["gr", "bg"], writes=["gr"])
                cx.op("act", lambda e: e.activation(out=gm[:], in_=gm[:], func=AF.Sigmoid, bias=bg[:, 16 + dc:17 + dc]),
                      reads=["gm", "bg"], writes=["gm"])
                pa, pak = PS[kk % 4], ("ps", kk % 4)
                pb, pbk = PS[4 + kk % 4], ("ps", 4 + kk % 4)
                kk += 1
                for c in range(8):
                    cx.op("pe", lambda e: e.matmul(out=pa[:, 0:512], lhsT=prw[:, c, dc * 128:dc * 128 + 128], rhs=yr[:, c, :],
                                                   start=(c == 0), stop=(c == 7)), reads=["prw", "yr"], writes=[pak], last=(c == 7))
                for c in range(16):
                    cx.op("pe", lambda e: e.matmul(out=pb[:, 0:512], lhsT=pml[:, c, dc * 128:dc * 128 + 128], rhs=ym[:, c, :],
                                                   start=(c == 0), stop=(c == 15)), reads=["pml", "ym"], writes=[pbk], last=(c == 15))
                cx.op("dve", lambda e: e.tensor_tensor(out=gr[:], in0=gr[:], in1=pa[:, 0:512], op=ALU.mult),
                      reads=["gr", pak], writes=["gr"])
                cx.op("dve", lambda e: e.tensor_tensor(out=gm[:], in0=gm[:], in1=pb[:, 0:512], op=ALU.mult),
                      reads=["gm", pbk], writes=["gm"])
                m_ = mg[dc % 2]
                cx.op("dve", lambda e: e.tensor_tensor(out=m_[:], in0=gr[:], in1=gm[:], op=ALU.add),
                      reads=["gr", "gm"], writes=[("mg", dc % 2)])
                cx.dma("sp", T["MGT"][dc * 128:dc * 128 + 128, t0:t0 + 512], m_[:], reads=[("mg", dc % 2)])
    cx.barrier()
    with ExitStack() as es:
        def sb(name, shape, dt=F32):
            return es.enter_context(nc.sbuf_tensor(name, shape, dt))
        stg = [sb("hstg%d" % i, [128, 2048]) for i in range(2)]
        wo = load_w_bf16(cx, nc, es, "wo", T["w_o"], 2048, 2048, stg, "hstg")
        mt = sb("mgt", [128, 16, 128], BF16)
        xt = sb("xres", [128, 2048])
        cx.barrier()
        for ti in range(16):
            t0 = ti * 128
            cx.dma("sp", mt[:], T["MGT"][:, t0:t0 + 128].rearrange("(c p) t -> p c t", p=128), writes=["mt"])
            cx.dma("sp", xt[:], T["xp"][OWN0 + t0:OWN0 + t0 + 128, :], writes=["xt"])
            for nb in range(4):
                pb, pbk = PS[(ti * 4 + nb) % 8], ("ps", (ti * 4 + nb) % 8)
                for c in range(16):
                    cx.op("pe", lambda e: e.matmul(out=pb[:, 0:512], lhsT=mt[:, c, :], rhs=wo[:, c, nb * 512:nb * 512 + 512],
                                                   start=(c == 0), stop=(c == 15)), reads=["mt", "wo"], writes=[pbk], last=(c == 15))
                cx.op("dve", lambda e: e.tensor_tensor(out=xt[:, nb * 512:nb * 512 + 512], in0=xt[:, nb * 512:nb * 512 + 512],
                                                       in1=pb[:, 0:512], op=ALU.add), reads=["xt", pbk], writes=["xt"])
            cx.dma("sp", T["H2"][t0:t0 + 128, :], xt[:], reads=["xt"])
    cx.barrier()


def stage_peer(cx, nc, T, PQ, PS):
    with ExitStack() as es0:
        def sb0(name, shape, dt=F32):
            return es0.enter_context(nc.sbuf_tensor(name, shape, dt))
        eidi_all = sb0("eidi_all", [128, 16, 128], I32)
        gate_all = sb0("gate_all", [128, 16, 128])
        ident = sb0("identE", [128, 128])
        gf = sb0("gffn", [128, 2048])
        epsc = sb0("epscE", [128, 1])
        cx.dma("sp", ident[:], T["ident"], writes=["ident"])
        cx.dma("sp", gf[:], T["gffn_b"], writes=["gf"])
        cx.op("dve", lambda e: e.memset(epsc[:], EPS), writes=["epsc"])
        with ExitStack() as es:
            def sb(name, shape, dt=F32):
                return es.enter_context(nc.sbuf_tensor(name, shape, dt))
            stg = [sb("estg%d" % i, [128, 2048]) for i in range(2)]
            wq = load_w_bf16(cx, nc, es, "wqp", T["peer_wq"], 2048, 2048, stg, "estg")
            cx.barrier()
            keysT = sb("keysT", [128, 16, 128])
            iota = sb("iotaE", [128, 256])
            cx.dma("sp", iota[:], T["iota256"], writes=["iota"])
            for g in range(16):
                cx.dma("sp", stg[0][:, g * 128:g * 128 + 128], T["peer_keys"][g], writes=["estg0"])
            for g in range(16):
                pb, pbk = PS[g % 2], ("ps", g % 2)
                cx.op("pe", lambda e: e.transpose(out=pb[:, 0:128], in_=stg[0][:, g * 128:g * 128 + 128], identity=ident[:, :]),
                      reads=["estg0", "ident"], writes=[pbk])
                cx.op("act", lambda e: e.copy(out=keysT[:, g, :], in_=pb[:, 0:128]), reads=[pbk], writes=["keysT"])
            cx.barrier()
            junk = stg[1][:]
            JK = "junkA"
            h2 = sb("h2", [128, 2048])
            hn = sb("hn", [128, 2048])
            hnT = sb("hnT", [128, 16, 128], BF16)
            qT = sb("qTp", [128, 16, 128])
            sc = sb("scp", [128, 16, 128])
            sc2 = sb("scp2", [128, 128])
            tops = sb("tops", [128, 16, 16])
            topi = sb("topi", [128, 16, 16])
            tiu_all = sb("tiu_all", [128, 16, 16], U32)
            piu = sb("piu", [128, 8, 16], U32)
            sc2_all = sb("sc2_all", [128, 16, 128])
            cand = sb("cand", [128, 8, 16, 16])
            cidx = sb("cidx", [128, 8, 16, 16])
            best = sb("best", [128, 8, 16])
            pos = sb("pos", [128, 8, 16])
            eid = sb("eid", [128, 8, 16])
            gate = sb("gate", [128, 8, 16])
            gsum = sb("gsum", [128, 8])
            ssq = sb("ssqE", [128, 2])
            NEG = -1e30
            cand2 = sc[:].rearrange("p (h s) n -> p h (s n)", s=2)
            eqb = junk.rearrange("p (h n) -> p h n", h=8)
            for ti in range(16):
                t0 = ti * 128
                cx.dma("sp", h2[:], T["H2"][t0:t0 + 128, :], writes=["h2"])
                cx.op("act", lambda e: e.activation(out=junk, in_=h2[:], func=AF.Square, accum_out=ssq[:, 0:1]),
                      reads=["h2"], writes=[JK, "ssq0"] + [("jk", i_) for i_ in range(8)])
                cx.op("act", lambda e: e.activation(out=ssq[:, 0:1], in_=ssq[:, 0:1], func=AF.Sqrt, scale=1.0 / D,
                                                    bias=epsc[:, 0:1]), reads=["ssq0", "epsc"], writes=["ssq0"])
                cx.op("dve", lambda e: e.reciprocal(out=ssq[:, 0:1], in_=ssq[:, 0:1]), reads=["ssq0"], writes=["ssq0"])
                cx.op("dve", lambda e: e.scalar_tensor_tensor(out=hn[:], in0=h2[:], scalar=ssq[:, 0:1], in1=gf[:],
                                                              op0=ALU.mult, op1=ALU.mult), reads=["h2", "ssq0", "gf"], writes=["hn"])
                for cg in range(4):
                    pb, pbk = PS[cg % 2], ("ps", cg % 2)
                    for ci in range(4):
                        c = cg * 4 + ci
                        cx.op("pe", lambda e: e.transpose(out=pb[:, ci * 128:ci * 128 + 128], in_=hn[:, c * 128:(c + 1) * 128],
                                                          identity=ident[:, :]), reads=["hn", "ident"], writes=[pbk], last=(ci == 3))
                    cx.op("act", lambda e: e.copy(out=hnT[:, cg * 4:cg * 4 + 4, :],
                                                  in_=pb[:, 0:512].rearrange("p (c t) -> p c t", c=4)), reads=[pbk], writes=["hnT"])
                for g in range(16):
                    pb, pbk = PS[2 + g % 2], ("ps", 2 + g % 2)
                    for c in range(16):
                        cx.op("pe", lambda e: e.matmul(out=pb[:, 0:128], lhsT=wq[:, c, g * 128:g * 128 + 128], rhs=hnT[:, c, :],
                                                       start=(c == 0), stop=(c == 15)), reads=["wqp", "hnT"], writes=[pbk], last=(c == 15))
                    cx.op("act", lambda e: e.copy(out=qT[:, g, :], in_=pb[:, 0:128]), reads=[pbk], writes=[("qT", g)])
                for g in range(16):
                    pb, pbk = PS[g % 2], ("ps", g % 2)
                    cx.op("pe", lambda e: e.matmul(out=pb[:, 0:128], lhsT=qT[:, g, :], rhs=keysT[:, g, :], start=True, stop=True),
                          reads=[("qT", g), "keysT"], writes=[pbk])
                    cx.op("act", lambda e: e.copy(out=sc[:, g, :], in_=pb[:, 0:128]), reads=[pbk], writes=[("sc", g)])
                for g in range(16):
                    cx.op("dve", lambda e: e.max(out=tops[:, g, 0:8], in_=sc[:, g, :]), reads=[("sc", g)], writes=[("tops", g)])
                for g in range(16):
                    cx.op("dve", lambda e: e.max_index(out=tiu_all[:, g, 0:8], in_max=tops[:, g, 0:8], in_values=sc[:, g, :]),
                          reads=[("sc", g), ("tops", g)], writes=[("tiu", g)])
                for g in range(16):
                    cx.op("dve", lambda e: e.match_replace(out=sc2_all[:, g, :], in_to_replace=tops[:, g, 0:8], in_values=sc[:, g, :],
                                                           imm_value=NEG), reads=[("sc", g), ("tops", g)], writes=[("sc2", g)])
                for g in range(16):
                    cx.op("dve", lambda e: e.max(out=tops[:, g, 8:16], in_=sc2_all[:, g, :]), reads=[("sc2", g)], writes=[("tops", g)])
                for g in range(16):
                    cx.op("dve", lambda e: e.max_index(out=tiu_all[:, g, 8:16], in_max=tops[:, g, 8:16], in_values=sc2_all[:, g, :]),
                          reads=[("sc2", g), ("tops", g)], writes=[("tiu", g)])
                cx.op("dve", lambda e: e.tensor_copy(out=topi[:], in_=tiu_all[:]), reads=[("tiu", g) for g in range(16)],
                      writes=[("topi", g) for g in range(16)])
                tkeys = [("tops", g) for g in range(16)]
                ikeys = [("topi", g) for g in range(16)]
                ts4 = tops[:].rearrange("p (h s) k -> p h s k", s=2)
                ti4 = topi[:].rearrange("p (h s) k -> p h s k", s=2)
                cx.op("dve", lambda e: e.tensor_tensor(out=cand[:], in0=ts4[:, :, 0, :].unsqueeze(3).to_broadcast([128, 8, 16, 16]),
                                                       in1=ts4[:, :, 1, :].unsqueeze(2).to_broadcast([128, 8, 16, 16]), op=ALU.add),
                      reads=tkeys, writes=["cand"])
                cx.op("dve", lambda e: e.tensor_scalar(out=eid[:], in0=ti4[:, :, 0, :], scalar1=128.0, scalar2=None, op0=ALU.mult),
                      reads=ikeys, writes=["eid"] + [("eid", hh, k) for hh in range(8) for k in range(16)])
                cx.op("dve", lambda e: e.tensor_tensor(out=cidx[:], in0=eid[:].unsqueeze(3).to_broadcast([128, 8, 16, 16]),
                                                       in1=ti4[:, :, 1, :].unsqueeze(2).to_broadcast([128, 8, 16, 16]),
                                                       op=ALU.add), reads=ikeys + ["eid"], writes=["cidx"])
                c3 = cand[:].rearrange("p h a b -> p h (a b)")
                i3 = cidx[:].rearrange("p h a b -> p h (a b)")
                for hh in range(8):
                    cx.op("dve", lambda e: e.max(out=best[:, hh, 0:8], in_=c3[:, hh, :]), reads=["cand"], writes=[("best", hh)])
                for hh in range(8):
                    cx.op("dve", lambda e: e.max_index(out=piu[:, hh, 0:8], in_max=best[:, hh, 0:8], in_values=c3[:, hh, :]),
                          reads=["cand", ("best", hh)], writes=[("piu", hh)])
                for hh in range(8):
                    cx.op("dve", lambda e: e.match_replace(out=cand2[:, hh, :], in_to_replace=best[:, hh, 0:8], in_values=c3[:, hh, :],
                                                           imm_value=NEG), reads=["cand", ("best", hh)],
                          writes=[("sc", 2 * hh), ("sc", 2 * hh + 1)])
                for hh in range(8):
                    cx.op("dve", lambda e: e.max(out=best[:, hh, 8:16], in_=cand2[:, hh, :]),
                          reads=[("sc", 2 * hh), ("sc", 2 * hh + 1)], writes=[("best", hh)])
                for hh in range(8):
                    cx.op("dve", lambda e: e.max_index(out=piu[:, hh, 8:16], in_max=best[:, hh, 8:16], in_values=cand2[:, hh, :]),
                          reads=[("sc", 2 * hh), ("sc", 2 * hh + 1), ("best", hh)], writes=[("piu", hh)])
                cx.op("dve", lambda e: e.tensor_copy(out=pos[:], in_=piu[:]), reads=[("piu", hh) for hh in range(8)],
                      writes=[("pos", hh) for hh in range(8)])
                bkeys = [("best", hh) for hh in range(8)]
                pkeys = [("pos", hh) for hh in range(8)]
                for hh in range(8):
                    for k in range(16):
                        js_ = (hh * 16 + k) % 8
                        cx.op("dve", lambda e: e.scalar_tensor_tensor(out=junk[:, js_ * 256:js_ * 256 + 256], in0=iota[:, :],
                                                                      scalar=pos[:, hh, k:k + 1],
                                                                      in1=i3[:, hh, :], op0=ALU.is_equal, op1=ALU.mult,
                                                                      accum_out=eid[:, hh, k:k + 1]),
                              reads=["iota", "cidx", ("pos", hh)], writes=[("jk", js_), ("eid", hh, k)])
                cx.op("dve", lambda e: e.tensor_copy(out=eidi_all[:, ti, :], in_=eid[:].rearrange("p h k -> p (h k)")),
                      reads=[("eid", hh, k) for hh in range(8) for k in range(16)], writes=[("eidi", ti)])
                cx.op("dve", lambda e: e.tensor_tensor(out=gate[:], in0=best[:], in1=best[:, :, 0:1].to_broadcast([128, 8, 16]),
                                                       op=ALU.subtract), reads=bkeys, writes=["gate"])
                cx.op("act", lambda e: e.activation(out=gate[:], in_=gate[:], func=AF.Exp), reads=["gate"], writes=["gate"])
                cx.op("dve", lambda e: e.tensor_reduce(out=gsum[:], in_=gate[:], axis=AX.X, op=ALU.add), reads=["gate"], writes=["gsum"])
                cx.op("dve", lambda e: e.reciprocal(out=gsum[:], in_=gsum[:]), reads=["gsum"], writes=["gsum"])
                cx.op("dve", lambda e: e.tensor_tensor(out=gate_all[:, ti, :].rearrange("p (h k) -> p h k", h=8), in0=gate[:],
                                                       in1=gsum[:].unsqueeze(2).to_broadcast([128, 8, 16]),
                                                       op=ALU.mult), reads=["gate", "gsum"], writes=[("gate_all", ti)])

        cx.barrier()
        with ExitStack() as es:
            def sb(name, shape, dt=F32):
                return es.enter_context(nc.sbuf_tensor(name, shape, dt))
            NR = 14
            rows = [sb("rows%d" % i, [128, 4096], BF16)[:] for i in range(NR)]
            gfin = sb("gfin", [128, 2048])
            identb = sb("identEb", [128, 128], BF16)
            h2 = sb("h2b", [128, 2048])
            hnb = sb("hnb", [128, 2048], BF16)
            junkb = sb("junkb", [128, 2048], BF16)
            score = sb("score", [128, 128])
            coef = sb("coef", [128, 128])
            ssq = sb("ssqB", [128, 2])
            dg = [sb("dg%d" % i, [128, 128], BF16) for i in range(8)]
            cx.dma("sp", gfin[:], T["gfin_b"], writes=["gfin"])
            cx.op("dve", lambda e: e.tensor_copy(out=identb[:], in_=ident[:]), reads=["ident"], writes=["identb"])
            junk = rows[NR - 1].bitcast(F32)
            JK = ("rows", NR - 1)
            NG = NR - 1
            for ti in range(16):
                t0 = ti * 128
                cx.dma("sp", h2[:], T["H2"][t0:t0 + 128, :], writes=["h2"])
                cx.op("act", lambda e: e.activation(out=junk, in_=h2[:], func=AF.Square, accum_out=ssq[:, 0:1]),
                      reads=["h2"], writes=[JK, "ssq0"])
                cx.op("act", lambda e: e.activation(out=ssq[:, 0:1], in_=ssq[:, 0:1], func=AF.Sqrt, scale=1.0 / D,
                                                    bias=epsc[:, 0:1]), reads=["ssq0", "epsc"], writes=["ssq0"])
                cx.op("dve", lambda e: e.reciprocal(out=ssq[:, 0:1], in_=ssq[:, 0:1]), reads=["ssq0"], writes=["ssq0"])
                cx.op("dve", lambda e: e.scalar_tensor_tensor(out=hnb[:], in0=h2[:], scalar=ssq[:, 0:1], in1=gf[:],
                                                              op0=ALU.mult, op1=ALU.mult), reads=["h2", "ssq0", "gf"], writes=["hnb"])
                for g in range(32):
                    js = list(range(g * 4, g * 4 + 4))
                    for j in js:
                        rb = rows[j % NG]
                        cx.dma("pool", rb, T["UVB"], indirect=bass.IndirectOffsetOnAxis(ap=eidi_all[:, ti, j:j + 1], axis=0),
                               writes=[("rows", j % NG)])
                        cx.op("dve", lambda e: e.scalar_tensor_tensor(out=junkb[:], in0=rb[:, 0:2048], scalar=1.0, in1=hnb[:],
                                                                      op0=ALU.mult, op1=ALU.mult, accum_out=score[:, j:j + 1]),
                              reads=[("rows", j % NG), "hnb"], writes=["junkb", ("score", g)])
                    cx.op("act", lambda e: e.activation(out=coef[:, g * 4:g * 4 + 4], in_=score[:, g * 4:g * 4 + 4], func=AF.Gelu),
                          reads=[("score", g)], writes=[("coef", g)])
                    cx.op("dve", lambda e: e.tensor_tensor(out=coef[:, g * 4:g * 4 + 4], in0=coef[:, g * 4:g * 4 + 4],
                                                           in1=gate_all[:, ti, g * 4:g * 4 + 4], op=ALU.mult),
                          reads=[("coef", g)], writes=[("coef", g)])
                    for j in js:
                        rb = rows[j % NG]
                        d_ = dg[j % 8]
                        cx.op("act", lambda e: e.activation(out=d_[:], in_=identb[:], func=AF.Copy, scale=coef[:, j:j + 1]),
                              reads=[("coef", g), "identb"], writes=[("dg", j % 8)])
                        for nb in range(4):
                            cx.op("pe", lambda e: e.matmul(out=PS[4 + nb][:, 0:512], lhsT=d_[:],
                                                           rhs=rb[:, 2048 + nb * 512:2048 + nb * 512 + 512],
                                                           start=(j == 0), stop=(j == 127)),
                                  reads=[("dg", j % 8), ("rows", j % NG)], writes=[("ps", 4 + nb)], last=(nb == 3))
                for nb in range(4):
                    cx.op("dve", lambda e: e.tensor_tensor(out=h2[:, nb * 512:nb * 512 + 512], in0=h2[:, nb * 512:nb * 512 + 512],
                                                           in1=PS[4 + nb][:, 0:512], op=ALU.add), reads=["h2", ("ps", 4 + nb)], writes=["h2"])
                cx.op("act", lambda e: e.activation(out=junk, in_=h2[:], func=AF.Square, accum_out=ssq[:, 1:2]),
                      reads=["h2"], writes=[JK, "ssq1"])
                cx.op("act", lambda e: e.activation(out=ssq[:, 1:2], in_=ssq[:, 1:2], func=AF.Sqrt, scale=1.0 / D,
                                                    bias=epsc[:, 0:1]), reads=["ssq1", "epsc"], writes=["ssq1"])
                cx.op("dve", lambda e: e.reciprocal(out=ssq[:, 1:2], in_=ssq[:, 1:2]), reads=["ssq1"], writes=["ssq1"])
                cx.op("dve", lambda e: e.scalar_tensor_tensor(out=junk, in0=h2[:], scalar=ssq[:, 1:2], in1=gfin[:],
                                                              op0=ALU.mult, op1=ALU.mult), reads=["h2", "ssq1", "gfin"], writes=[JK])
                cx.dma("sp", T["out"][t0:t0 + 128, :], junk, reads=[JK])
    cx.barrier()


_NC = None


def kernel(**inputs):
    global _NC
    inputs = {k: np.asarray(v) for k, v in inputs.items()}
    if _NC is None:
        _NC = build()
    in_maps = []
    for b in range(4):
        for s_ in range(2):
            m = prep_core(inputs, b, s_)
            in_maps.append({k: np.ascontiguousarray(v, dtype=np.float32) for k, v in m.items()})
    res = run_bass_kernel_spmd(_NC, in_maps, core_ids=list(range(8)))
    out = np.zeros((4, 4096, D), np.float32)
    for b in range(4):
        for s_ in range(2):
            o = np.asarray(res.results[b * 2 + s_]["out"])
            if s_ == 0:
                out[b, 0:2048] = o
            else:
                out[b, 2048:4096] = o[::-1]
    return out
```
